# Optimizing a Trainium2 kernel written in Bass

```python
import math
import jax, jax.numpy as jnp
from jax import lax
import numpy as np


D_MODEL = 1024
BATCH = 4
SEQ = 8192
DEPTH = 1

PLE_DIM = 256
DN_HEADS = 4
DN_HEAD_DIM = 128
DN_WIDTH = DN_HEADS * DN_HEAD_DIM
CONV_WIDTH = 4
CHUNK = 64
DA_HEADS = 4
DA_HEAD_DIM = 64
DA_V_DIM = 2 * DA_HEAD_DIM
DA_QK_WIDTH = DA_HEADS * 2 * DA_HEAD_DIM
DA_WIDTH = DA_HEADS * DA_V_DIM
Q_BLOCK = 128
N_GROUPS = 4
EXPERTS_PER_GROUP = 4
N_EXPERTS = N_GROUPS * EXPERTS_PER_GROUP
TOP_K_IN_GROUP = 2
EXPERT_FF = 256
IN_SIZES = (3 * DN_WIDTH, DN_WIDTH, DN_HEADS, DN_HEADS, DA_QK_WIDTH, DA_QK_WIDTH, DA_WIDTH, 2 * D_MODEL)
IN_DIM = sum(IN_SIZES)
LN_EPS = 1e-5
RMS_EPS = 1e-6

kernel_name = 'hybrid_deltanet_diffattn_hmoe_deepnorm'


def layer_norm(x, g, b):
    xf = x.astype(jnp.float32)
    mu = jnp.mean(xf, axis=-1, keepdims=True)
    var = jnp.mean(jnp.square(xf - mu), axis=-1, keepdims=True)
    return ((xf - mu) * lax.rsqrt(var + LN_EPS) * g + b).astype(x.dtype)


def rms_norm(x, w):
    xf = x.astype(jnp.float32)
    return (xf * lax.rsqrt(jnp.mean(jnp.square(xf), axis=-1, keepdims=True) + RMS_EPS) * w).astype(x.dtype)


def l2_normalize(x):
    return x * lax.rsqrt(jnp.sum(jnp.square(x), axis=-1, keepdims=True) + RMS_EPS)


def causal_conv_silu(u, w):
    S = u.shape[1]
    K = w.shape[0]
    up = jnp.pad(u, ((0, 0), (K - 1, 0), (0, 0)))
    out = up[:, 0:S] * w[0]
    for j in range(1, K):
        out = out + up[:, j:j + S] * w[j]
    return jax.nn.silu(out)


def gated_delta_rule(q, k, v, beta, g):
    out_dtype = v.dtype
    f32 = jnp.float32
    B, S, H, dk = q.shape
    dv = v.shape[-1]
    n = S // CHUNK
    q = l2_normalize(q.astype(f32)) * (dk ** -0.5)
    k = l2_normalize(k.astype(f32))
    v = v.astype(f32)
    to_chunks = lambda t: jnp.moveaxis(t.reshape(B, n, CHUNK, H, -1), 3, 1)
    q, k, v = to_chunks(q), to_chunks(k), to_chunks(v)
    beta = jnp.moveaxis(beta.astype(f32).reshape(B, n, CHUNK, H), 3, 1)
    g = jnp.cumsum(jnp.moveaxis(g.astype(f32).reshape(B, n, CHUNK, H), 3, 1), axis=-1)
    idx = jnp.arange(CHUNK)
    causal = idx[:, None] >= idx[None, :]
    strict = idx[:, None] > idx[None, :]
    decay = jnp.exp(jnp.where(causal, g[..., :, None] - g[..., None, :], -jnp.inf))
    kb = k * beta[..., None]
    nmat = jnp.where(strict, -jnp.einsum('bhnid,bhnjd->bhnij', kb, k) * decay, 0.0)
    eye = jnp.eye(CHUNK, dtype=f32)
    T = eye + nmat
    power = nmat
    for _ in range(int(math.log2(CHUNK)) - 1):
        power = power @ power
        T = T @ (eye + power)
    u = T @ (v * beta[..., None])
    w = T @ (kb * jnp.exp(g)[..., None])
    a_intra = jnp.where(causal, jnp.einsum('bhnid,bhnjd->bhnij', q, k) * decay, 0.0)
    qg = q * jnp.exp(g)[..., None]
    g_last = g[..., -1]
    kg = k * jnp.exp(g_last[..., None] - g)[..., None]

    def step(state, xs):
        u_c, w_c, qg_c, kg_c, a_c, gl_c = xs
        v_new = u_c - w_c @ state
        o_c = qg_c @ state + a_c @ v_new
        state = state * jnp.exp(gl_c)[..., None, None] + jnp.swapaxes(kg_c, -1, -2) @ v_new
        return state, o_c

    xs = tuple(jnp.moveaxis(t, 2, 0) for t in (u, w, qg, kg, a_intra, g_last))
    state0 = jnp.zeros((B, H, dk, dv), f32)
    _, o = lax.scan(step, state0, xs)
    return jnp.transpose(o, (1, 0, 3, 2, 4)).reshape(B, S, H, dv).astype(out_dtype)


def diff_attention_alibi(q, k, v, lam):
    f32 = jnp.float32
    B, S, H, _, d = q.shape
    nb = S // Q_BLOCK
    slopes = jnp.asarray(2.0 ** (-8.0 * np.arange(1, H + 1) / H), dtype=f32)
    qb = jnp.moveaxis((q * (d ** -0.5)).reshape(B, nb, Q_BLOCK, H, 2, d), 1, 0)
    kpos = jnp.arange(S)

    def block(args):
        qi, bi = args
        qpos = bi * Q_BLOCK + jnp.arange(Q_BLOCK)
        dist = qpos[:, None] - kpos[None, :]
        s = jnp.einsum('bqhmd,bkhmd->bhmqk', qi, k, preferred_element_type=f32)
        s = s - slopes[:, None, None, None] * dist.astype(f32)
        s = jnp.where(dist >= 0, s, -jnp.inf)
        pr = jax.nn.softmax(s, axis=-1)
        a = pr[:, :, 0] - lam * pr[:, :, 1]
        return jnp.einsum('bhqk,bkhe->bqhe', a.astype(v.dtype), v,
                          preferred_element_type=f32).astype(v.dtype)

    o = lax.map(block, (qb, jnp.arange(nb)))
    return jnp.moveaxis(o, 0, 1).reshape(B, S, H, v.shape[-1])


def hierarchical_moe(h, w_rg, b_rg, w_re, b_re, w_gate, w_up, w_down):
    f32 = jnp.float32
    group_prob = jax.nn.softmax((h @ w_rg + b_rg).astype(f32), axis=-1)
    g_val, g_idx = lax.top_k(group_prob, 1)
    exp_logits = (h @ w_re + b_re).astype(f32)
    exp_logits = exp_logits.reshape(h.shape[:-1] + (N_GROUPS, EXPERTS_PER_GROUP))
    in_group = jnp.einsum('bsge,bsg->bse', exp_logits, jax.nn.one_hot(g_idx[..., 0], N_GROUPS, dtype=f32))
    e_prob = jax.nn.softmax(in_group, axis=-1)
    e_val, e_idx = lax.top_k(e_prob, TOP_K_IN_GROUP)
    e_w = e_val / jnp.sum(e_val, axis=-1, keepdims=True) * g_val
    global_idx = g_idx * EXPERTS_PER_GROUP + e_idx
    combine = jnp.einsum('bsk,bske->bse', e_w, jax.nn.one_hot(global_idx, N_EXPERTS, dtype=f32))
    hid = jax.nn.silu(jnp.einsum('bsd,edf->bsef', h, w_gate)) * jnp.einsum('bsd,edf->bsef', h, w_up)
    return jnp.einsum('bsef,efd->bsd', hid * combine[..., None].astype(h.dtype), w_down)


def setup_inputs(seed: int = 0) -> dict:
    key = jax.random.key(seed)
    ks = jax.random.split(key, 40)
    f32 = jnp.float32
    nrm = lambda kk, shape, scale: jax.random.normal(kk, shape, f32) * scale
    L = DEPTH
    dn_beta = (8.0 * DEPTH) ** -0.25
    dt = jnp.exp(jax.random.uniform(ks[8], (L, DN_HEADS), f32, math.log(1e-3), math.log(1e-1)))
    return {
        'x': nrm(ks[0], (BATCH, SEQ, D_MODEL), 1.0),
        'p': nrm(ks[1], (DEPTH, BATCH, SEQ, PLE_DIM), 1.0),
        'emb_ln_g': 1.0 + nrm(ks[2], (D_MODEL,), 0.02),
        'emb_ln_b': nrm(ks[3], (D_MODEL,), 0.02),
        'w_in': nrm(ks[4], (L, D_MODEL, IN_DIM), D_MODEL ** -0.5),
        'b_in': nrm(ks[5], (L, IN_DIM), 0.02),
        'conv_w': nrm(ks[6], (L, CONV_WIDTH, 3 * DN_WIDTH), CONV_WIDTH ** -0.5),
        'dn_a_log': jnp.log(jax.random.uniform(ks[7], (L, DN_HEADS), f32, 1.0, 16.0)),
        'dn_dt_bias': dt + jnp.log(-jnp.expm1(-dt)),
        'dn_norm_w': 1.0 + nrm(ks[9], (L, DN_HEAD_DIM), 0.02),
        'w_dn_o': nrm(ks[10], (L, DN_WIDTH, D_MODEL), DN_WIDTH ** -0.5 * dn_beta),
        'da_lq1': nrm(ks[11], (L, DA_HEAD_DIM), 0.1),
        'da_lk1': nrm(ks[12], (L, DA_HEAD_DIM), 0.1),
        'da_lq2': nrm(ks[13], (L, DA_HEAD_DIM), 0.1),
        'da_lk2': nrm(ks[14], (L, DA_HEAD_DIM), 0.1),
        'da_subln_w': 1.0 + nrm(ks[15], (L, DA_V_DIM), 0.02),
        'w_da_o': nrm(ks[16], (L, DA_WIDTH, D_MODEL), DA_WIDTH ** -0.5 * dn_beta),
        'w_out': nrm(ks[17], (L, D_MODEL, D_MODEL), D_MODEL ** -0.5 * dn_beta),
        'ln1_g': 1.0 + nrm(ks[18], (L, D_MODEL), 0.02),
        'ln1_b': nrm(ks[19], (L, D_MODEL), 0.02),
        'w_router_group': nrm(ks[20], (L, D_MODEL, N_GROUPS), D_MODEL ** -0.5),
        'b_router_group': nrm(ks[21], (L, N_GROUPS), 0.01),
        'w_router_expert': nrm(ks[22], (L, D_MODEL, N_EXPERTS), D_MODEL ** -0.5),
        'b_router_expert': nrm(ks[23], (L, N_EXPERTS), 0.01),
        'w_exp_gate': nrm(ks[24], (L, N_EXPERTS, D_MODEL, EXPERT_FF), D_MODEL ** -0.5),
        'w_exp_up': nrm(ks[25], (L, N_EXPERTS, D_MODEL, EXPERT_FF), D_MODEL ** -0.5),
        'w_exp_down': nrm(ks[26], (L, N_EXPERTS, EXPERT_FF, D_MODEL), EXPERT_FF ** -0.5 * dn_beta),
        'w_ple_gate': nrm(ks[27], (L, D_MODEL, D_MODEL), D_MODEL ** -0.5),
        'b_ple_gate': nrm(ks[28], (L, D_MODEL), 0.02),
        'w_ple_proj': nrm(ks[29], (L, PLE_DIM, D_MODEL), PLE_DIM ** -0.5 * dn_beta),
        'ln2_g': 1.0 + nrm(ks[30], (L, D_MODEL), 0.02),
        'ln2_b': nrm(ks[31], (L, D_MODEL), 0.02),
    }


def reference(x, p, emb_ln_g, emb_ln_b, w_in, b_in, conv_w, dn_a_log, dn_dt_bias, dn_norm_w, w_dn_o,
              da_lq1, da_lk1, da_lq2, da_lk2, da_subln_w, w_da_o, w_out, ln1_g, ln1_b,
              w_router_group, b_router_group, w_router_expert, b_router_expert,
              w_exp_gate, w_exp_up, w_exp_down, w_ple_gate, b_ple_gate, w_ple_proj, ln2_g, ln2_b):
    f32 = jnp.float32
    alpha = (2.0 * DEPTH) ** 0.25
    B, S, _ = x.shape
    splits = [int(c) for c in np.cumsum(IN_SIZES)[:-1]]
    h = layer_norm(x, emb_ln_g, emb_ln_b)
    for i in range(DEPTH):
        proj = h @ w_in[i] + b_in[i]
        qkv_dn, z_dn, b_dn, a_dn, q_da, k_da, v_da, gate_logits = jnp.split(proj, splits, axis=-1)
        qkv_dn = causal_conv_silu(qkv_dn, conv_w[i])
        q_dn, k_dn, v_dn = jnp.split(qkv_dn, 3, axis=-1)
        beta_dn = jax.nn.sigmoid(b_dn.astype(f32))
        g_dn = -jnp.exp(dn_a_log[i].astype(f32)) * jax.nn.softplus(a_dn.astype(f32) + dn_dt_bias[i])
        o_dn = gated_delta_rule(q_dn.reshape(B, S, DN_HEADS, DN_HEAD_DIM),
                                k_dn.reshape(B, S, DN_HEADS, DN_HEAD_DIM),
                                v_dn.reshape(B, S, DN_HEADS, DN_HEAD_DIM), beta_dn, g_dn)
        o_dn = rms_norm(o_dn, dn_norm_w[i]) * jax.nn.silu(z_dn.reshape(B, S, DN_HEADS, DN_HEAD_DIM))
        y_dn = o_dn.reshape(B, S, DN_WIDTH) @ w_dn_o[i]
        lam_init = 0.8 - 0.6 * math.exp(-0.3 * i)
        lam = (jnp.exp(jnp.sum(da_lq1[i].astype(f32) * da_lk1[i].astype(f32)))
               - jnp.exp(jnp.sum(da_lq2[i].astype(f32) * da_lk2[i].astype(f32))) + lam_init)
        o_da = diff_attention_alibi(q_da.reshape(B, S, DA_HEADS, 2, DA_HEAD_DIM),
                                    k_da.reshape(B, S, DA_HEADS, 2, DA_HEAD_DIM),
                                    v_da.reshape(B, S, DA_HEADS, DA_V_DIM), lam)
        o_da = rms_norm(o_da, da_subln_w[i]) * (1.0 - lam_init)
        y_da = o_da.reshape(B, S, DA_WIDTH) @ w_da_o[i]
        gate_dn, gate_da = jnp.split(jax.nn.sigmoid(gate_logits), 2, axis=-1)
        mix = (gate_dn * y_dn + gate_da * y_da) @ w_out[i]
        h = layer_norm(alpha * h + mix, ln1_g[i], ln1_b[i])
        ffn = hierarchical_moe(h, w_router_group[i], b_router_group[i], w_router_expert[i],
                               b_router_expert[i], w_exp_gate[i], w_exp_up[i], w_exp_down[i])
        ple = jax.nn.sigmoid(h @ w_ple_gate[i] + b_ple_gate[i]) * (p[i] @ w_ple_proj[i])
        h = layer_norm(alpha * h + ffn + ple, ln2_g[i], ln2_b[i])
    return h
```

```python
import contextlib
import numpy as np
import concourse.bass as bass
import concourse.mybir as mybir

F32 = mybir.dt.float32
BF16 = mybir.dt.bfloat16
AF = mybir.ActivationFunctionType
ALU = mybir.AluOpType
AX = mybir.AxisListType

EPOCH = 30000


class Res:
    __slots__ = ("w", "r")

    def __init__(self):
        self.w = None
        self.r = {}


class KB:
    def __init__(self, nc, es):
        self.nc = nc
        self.es = es
        self.E = {"pe": nc.tensor, "act": nc.scalar, "dve": nc.vector, "pool": nc.gpsimd, "sp": nc.sync}
        self.sems = {}
        self.cur = {}
        self.epoch = {e: 0 for e in self.E}
        self.waited = {e: {} for e in self.E}
        self.res = {}
        self.dcount = {}
        self.dtarget = {}
        self.ringpos = {}
        self.RINGS = {"c": 6, "w0": 2, "w1": 2}
        self.ninstr = 0
        for e in self.E:
            self._new_epoch(e)

    def _sem(self, key):
        if key not in self.sems:
            self.sems[key] = self.es.enter_context(self.nc.semaphore(key))
        return self.sems[key]

    def _new_epoch(self, e):
        key = f"e_{e}_{self.epoch[e]}"
        self.epoch[e] += 1
        self._sem(key)
        self.cur[e] = [key, 0]

    def R(self, x):
        if isinstance(x, tuple):
            ap, key = x
            k = (ap.tensor.name, key)
        else:
            ap = x
            k = ap.tensor.name
        r = self.res.get(k)
        if r is None:
            r = self.res[k] = Res()
        return ap, r

    def _need(self, eng, ev):
        if ev is None:
            return
        key, val = ev
        if eng == "pe" and key.startswith("e_pe_"):
            return
        if key.startswith("d_"):
            val = self.dcount[key]
            if self.dtarget.get(key, 0) < val:
                self.dtarget[key] = val
        w = self.waited[eng]
        if w.get(key, 0) >= val:
            return
        self.E[eng].wait_ge(self.sems[key], val)
        w[key] = val

    def _pre(self, eng, reads, writes):
        for r in reads:
            self._need(eng, r.w)
        for w in writes:
            self._need(eng, w.w)
            for key, val in w.r.items():
                self._need(eng, (key, val))

    def _post(self, ev, reads, writes):
        key, val = ev
        for r in reads:
            if r.r.get(key, 0) < val:
                r.r[key] = val
        for w in writes:
            w.w = ev
            w.r = {}

    def op(self, eng, fn, outs, ins):
        rs = [self.R(x)[1] for x in ins if x is not None]
        ws = [self.R(x)[1] for x in outs]
        self._pre(eng, rs, ws)
        ins_ = fn()
        c = self.cur[eng]
        ins_.then_inc(self.sems[c[0]], 1)
        c[1] += 1
        ev = (c[0], c[1])
        self._post(ev, rs, ws)
        if c[1] >= EPOCH:
            self._new_epoch(eng)
        self.ninstr += 1
        return ins_

    def dma(self, q, out, in_, stream, **kw):
        oap, ow = self.R(out)
        iap, ir = self.R(in_)
        self._pre(q, [ir], [ow])
        ring = self.RINGS.get(stream, 1)
        if ring > 1:
            i = self.ringpos.get((q, stream), 0)
            self.ringpos[(q, stream)] = i + 1
            stream = "%s%d" % (stream, i % ring)
        key = "d_" + q + "_" + stream
        sem = self._sem(key)
        issued = self.dcount.get(key, 0)
        if issued and self.dtarget.get(key, 0) >= issued:
            self._need(q, (key, issued))
        self.E[q].dma_start(out=oap, in_=iap, **kw).then_inc(sem, 16)
        self.dcount[key] = self.dcount.get(key, 0) + 16
        ev = (key, self.dcount[key])
        self._post(ev, [ir], [ow])
        self.ninstr += 1

    def barrier(self):
        evs = [(c[0], c[1]) for c in self.cur.values() if c[1] > 0]
        evs += [(k, v) for k, v in self.dcount.items()]
        for e in self.E:
            for ev in evs:
                if ev[0].startswith("e_" + e + "_"):
                    continue
                self._need(e, ev)
        self.res = {}

    def finish(self, outs):
        for x in outs:
            _, r = self.R(x)
            self._need("sp", r.w)

    def mm(self, out, lhsT, rhs, start=True, stop=True, **kw):
        o = out[0] if isinstance(out, tuple) else out
        l = lhsT[0] if isinstance(lhsT, tuple) else lhsT
        r = rhs[0] if isinstance(rhs, tuple) else rhs
        return self.op("pe", lambda: self.nc.tensor.matmul(o, l, r, start=start, stop=stop, **kw), [out], [lhsT, rhs])

    def tr(self, out, in_, ident):
        o = out[0] if isinstance(out, tuple) else out
        i = in_[0] if isinstance(in_, tuple) else in_
        return self.op("pe", lambda: self.nc.tensor.transpose(o, i, ident), [out], [in_, ident])

    def act(self, out, in_, func, bias=None, scale=None, accum_out=None, eng="act"):
        o = out[0] if isinstance(out, tuple) else out
        i = in_[0] if isinstance(in_, tuple) else in_
        kw = {}
        ins = [in_]
        if bias is not None:
            kw["bias"] = bias[0] if isinstance(bias, tuple) else bias
            if not isinstance(bias, (int, float)):
                ins.append(bias)
        if scale is not None:
            kw["scale"] = scale[0] if isinstance(scale, tuple) else scale
            if not isinstance(scale, (int, float)):
                ins.append(scale)
        outs = [out]
        if accum_out is not None:
            kw["accum_out"] = accum_out[0] if isinstance(accum_out, tuple) else accum_out
            outs.append(accum_out)
        return self.op("act", lambda: self.nc.scalar.activation(out=o, in_=i, func=func, **kw), outs, ins)

    def tt(self, eng, out, in0, in1, op):
        o = out[0] if isinstance(out, tuple) else out
        a = in0[0] if isinstance(in0, tuple) else in0
        b = in1[0] if isinstance(in1, tuple) else in1
        return self.op(eng, lambda: self.E[eng].tensor_tensor(out=o, in0=a, in1=b, op=op), [out], [in0, in1])

    def ts(self, eng, out, in0, s1, s2=None, op0=ALU.mult, op1=None, accum_out=None):
        o = out[0] if isinstance(out, tuple) else out
        a = in0[0] if isinstance(in0, tuple) else in0
        ins = [in0]
        kw = {}
        if not isinstance(s1, (int, float)):
            ins.append(s1)
            s1 = s1[0] if isinstance(s1, tuple) else s1
        if s2 is not None and not isinstance(s2, (int, float)):
            ins.append(s2)
            s2 = s2[0] if isinstance(s2, tuple) else s2
        if op1 is not None:
            kw["op1"] = op1
        outs = [out]
        if accum_out is not None:
            kw["accum_out"] = accum_out[0] if isinstance(accum_out, tuple) else accum_out
            outs.append(accum_out)
        return self.op(eng, lambda: self.E[eng].tensor_scalar(out=o, in0=a, scalar1=s1, scalar2=s2, op0=op0, **kw), outs, ins)

    def stt(self, out, in0, scalar, in1, op0, op1):
        o = out[0] if isinstance(out, tuple) else out
        a = in0[0] if isinstance(in0, tuple) else in0
        b = in1[0] if isinstance(in1, tuple) else in1
        ins = [in0, in1]
        if not isinstance(scalar, (int, float)):
            ins.append(scalar)
            scalar = scalar[0] if isinstance(scalar, tuple) else scalar
        return self.op("dve", lambda: self.nc.vector.scalar_tensor_tensor(out=o, in0=a, scalar=scalar, in1=b, op0=op0, op1=op1), [out], ins)

    def copy(self, eng, out, in_):
        o = out[0] if isinstance(out, tuple) else out
        i = in_[0] if isinstance(in_, tuple) else in_
        if eng == "act":
            return self.op("act", lambda: self.nc.scalar.copy(out=o, in_=i), [out], [in_])
        return self.op(eng, lambda: self.E[eng].tensor_copy(out=o, in_=i), [out], [in_])

    def memset(self, eng, out, val):
        o = out[0] if isinstance(out, tuple) else out
        return self.op(eng, lambda: self.E[eng].memset(o, val), [out], [])

    def sb(self, name, shape, dt, es=None):
        es = es or self.es
        t = es.enter_context(self.nc.sbuf_tensor(name, shape, dt))
        nbytes = int(np.prod(shape[1:])) * (2 if dt == BF16 else 4)
        rem = (-nbytes) % 128
        if rem:
            es.enter_context(self.nc.sbuf_tensor(name + "_pad", [shape[0], rem // 2], BF16))
        return t


D = 1024
IN_DIM = 5640
C_Z, C_B, C_A, C_DQ, C_DK, C_DV, C_G = 1536, 2048, 2052, 2056, 2568, 3080, 3592
SLOPES = [2.0 ** (-8.0 * (h + 1) / 4) for h in range(4)]


def dram(nc, name, shape, dt, kind="Internal"):
    return nc.dram_tensor(name, list(shape), dt, kind=kind).ap()


class Ctx:
    pass


def make_consts(Tc, To):
    T = Tc + To
    c = {}
    c["ident"] = np.eye(128, dtype=np.float32)
    i = np.arange(128)
    c["U"] = (i[:, None] <= i[None, :]).astype(np.float32)
    c["negU"] = -c["U"]
    c["ones"] = np.ones((128, 128), np.float32)
    c["MLs"] = np.tile(np.where(i[:, None] > i[None, :], 0.0, -1e9).astype(np.float32), (1, 4))
    c["MUi"] = np.tile(np.where(i[None, :] >= i[:, None], 0.0, -1e9).astype(np.float32), (1, 4))
    c["SU01"] = np.tile((i[None, :] > i[:, None]).astype(np.float32), (1, 4))
    c["catri"] = np.where(i[:, None] <= i[None, :], 0.0, -30000.0).astype(np.float32)
    kpos = np.arange(T)
    qpos = Tc + np.arange(To)
    augk = np.zeros((4, 5, T), np.float32)
    augq = np.zeros((4, 4, To), np.float32)
    for h in range(4):
        s = SLOPES[h]
        augk[h, 0] = 1.0
        augk[h, 1] = 1.0
        augk[h, 2] = s * 128.0 * (kpos // 128)
        augk[h, 3] = 1.0
        augk[h, 4] = s * (kpos % 128)
        augq[h, 0] = -s * 128.0 * (qpos // 128)
        augq[h, 1] = 1.0
        augq[h, 2] = -s * (qpos % 128)
        augq[h, 3] = 1.0
    c["augk"] = augk
    c["augq"] = augq
    return c


INPUT_NAMES = ['emb_ln_g', 'emb_ln_b', 'w_in', 'b_in', 'conv_w', 'dn_a_log', 'dn_dt_bias', 'dn_norm_w', 'w_dn_o',
               'da_lq1', 'da_lk1', 'da_lq2', 'da_lk2', 'da_subln_w', 'w_da_o', 'w_out', 'ln1_g', 'ln1_b',
               'w_router_group', 'b_router_group', 'w_router_expert', 'b_router_expert',
               'w_exp_gate', 'w_exp_up', 'w_exp_down', 'w_ple_gate', 'b_ple_gate', 'w_ple_proj', 'ln2_g', 'ln2_b']
W_SHAPES = {
    'emb_ln_g': (1024,), 'emb_ln_b': (1024,), 'w_in': (1024, 5640), 'b_in': (5640,), 'conv_w': (4, 1536),
    'dn_a_log': (4,), 'dn_dt_bias': (4,), 'dn_norm_w': (128,), 'w_dn_o': (512, 1024),
    'da_lq1': (64,), 'da_lk1': (64,), 'da_lq2': (64,), 'da_lk2': (64,), 'da_subln_w': (128,),
    'w_da_o': (512, 1024), 'w_out': (1024, 1024), 'ln1_g': (1024,), 'ln1_b': (1024,),
    'w_router_group': (1024, 4), 'b_router_group': (4,), 'w_router_expert': (1024, 16), 'b_router_expert': (16,),
    'w_exp_gate': (16, 1024, 256), 'w_exp_up': (16, 1024, 256), 'w_exp_down': (16, 256, 1024),
    'w_ple_gate': (1024, 1024), 'b_ple_gate': (1024,), 'w_ple_proj': (256, 1024), 'ln2_g': (1024,), 'ln2_b': (1024,),
}


def colload(k, q, dst, src1d, n, M=128, stream="c"):
    k.dma(q, dst, src1d.rearrange("(c p) -> p c", p=M), stream, allow_slow_non_contiguous=True)


def phase_A(k, g):
    nc = k.nc
    d = g.d
    Tc, To, T = g.Tc, g.To, g.T
    nb = T // 512
    nbc = Tc // 512
    NW = C_G
    with contextlib.ExitStack() as es:
        sb = lambda name, shape, dt: k.sb(name, shape, dt, es)
        Wb = sb("A_Wb", [128, 8, NW], BF16)
        for kk in range(8):
            k.dma("pool", (Wb[:, kk, :], kk), d["w_in"][kk * 128:(kk + 1) * 128, 0:NW], "w%d" % (kk % 2))
        ident = sb("A_ident", [128, 128], BF16)
        k.dma("pool", ident[:], d["ident"][:, :], "c")
        onesb = sb("A_ones", [128, 128], BF16)
        k.dma("pool", onesb[:], d["ones"][:, :], "c")
        bcol = sb("A_bcol", [128, 12], F32)
        colload(k, "sp", bcol[:, :], d["b_in"][0:1536], 12)
        qcol = sb("A_qcol", [128, 4], F32)
        colload(k, "sp", qcol[:, :], d["b_in"][C_DQ:C_DQ + 512], 4)
        kcol = sb("A_kcol", [128, 4], F32)
        colload(k, "sp", kcol[:, :], d["b_in"][C_DK:C_DK + 512], 4)
        egc = sb("A_egc", [128, 8], F32)
        colload(k, "sp", egc[:, :], d["emb_ln_g"], 8)
        ebc = sb("A_ebc", [128, 8], F32)
        colload(k, "sp", ebc[:, :], d["emb_ln_b"], 8)
        cw = sb("A_cw", [128, 12, 4], F32)
        for j in range(4):
            k.dma("sp", cw[:, :, j], d["conv_w"][j, :].rearrange("(c p) -> p c", p=128), "c", allow_slow_non_contiguous=True)
        eg_bc = sb("A_eg_bc", [128, 1024], F32)
        k.dma("sp", eg_bc[:], d["emb_ln_g"].partition_broadcast(128), "c")
        eb_bc = sb("A_eb_bc", [128, 1024], F32)
        k.dma("sp", eb_bc[:], d["emb_ln_b"].partition_broadcast(128), "c")
        zb_bc = sb("A_zb_bc", [128, 512], F32)
        k.dma("sp", zb_bc[:], d["b_in"][C_Z:C_Z + 512].partition_broadcast(128), "c")
        vb_bc = sb("A_vb_bc", [128, 512], F32)
        k.dma("sp", vb_bc[:], d["b_in"][C_DV:C_DV + 512].partition_broadcast(128), "c")
        bab_bc = sb("A_bab_bc", [128, 8], F32)
        k.dma("sp", bab_bc[:], d["b_in"][C_B:C_B + 8].partition_broadcast(128), "c")
        nw_bc = sb("A_nw_bc", [128, 4, 128], F32)
        for h in range(4):
            k.dma("sp", nw_bc[:, h, :], d["dn_norm_w"].partition_broadcast(128), "c")
        dtb_bc = sb("A_dtb_bc", [128, 4], F32)
        k.dma("sp", dtb_bc[:], d["dn_dt_bias"].partition_broadcast(128), "c")
        negA = sb("A_negA", [128, 4], F32)
        k.dma("sp", negA[:], d["dn_a_log"].partition_broadcast(128), "c")
        k.act(negA[:], negA[:], AF.Exp)
        k.ts("dve", negA[:], negA[:], -1.0)
        tm_tok = sb("A_tm_tok", [128, T // 128], F32)
        k.dma("sp", tm_tok[:], d["tm_tok"][:, :], "c")
        epsc = sb("A_epsc", [128, 3], F32)
        k.memset("dve", epsc[:, 0:1], 1e-6)
        k.memset("dve", epsc[:, 1:2], 1e-5)
        k.memset("dve", epsc[:, 2:3], 1.0)
        kmx = sb("A_kmx", [128, 4, nb], F32)

        xblk = sb("A_x", [128, 4, 1024], F32)
        xn = sb("A_xn", [128, 4, 1024], BF16)
        xn32s = [sb("A_xn32_%d" % i, [128, 1024], F32) for i in range(2)]
        stats = sb("A_stats", [128, 4, 2, 6], F32)
        mv = sb("A_mv", [128, 4, 2], F32)
        rstd = sb("A_rstd", [128, 4], F32)
        hTs = [sb("A_hT%d" % i, [128, 8, 512], BF16) for i in range(2)]
        pre = sb("A_pre", [128, 12, 515], BF16)
        identf = sb("A_identf", [128, 128], F32)
        k.dma("sp", identf[:], d["ident"][:, :], "c")
        diagw = sb("A_diagw", [128, 12, 4, 128], BF16)
        for c_ in range(12):
            for j_ in range(4):
                k.ts("dve" if (c_ + j_) % 2 == 0 else "pool", diagw[:, c_, j_, :], identf[:], cw[:, c_, j_:j_ + 1])
        for c_ in range(12):
            k.memset("pool", (pre[:, c_, 0:3], c_), 0.0)
        tmbcs = [sb("A_tmbc%d" % i, [128, 512], F32) for i in range(2)]
        NR = 4
        qa = [sb("A_qa%d" % i, [128, 512], F32) for i in range(NR)]
        sq = [sb("A_sq%d" % i, [128, 512], BF16) for i in range(NR)]
        rinv = [sb("A_rinv%d" % i, [128, 512], F32) for i in range(NR)]
        qn = [sb("A_qn%d" % i, [128, 512], BF16) for i in range(NR)]
        vn = [sb("A_vn%d" % i, [128, 512], BF16) for i in range(3)]
        ktok = sb("A_ktok", [128, 4, 512], BF16)
        vtok = sb("A_vtok", [128, 4, 512], BF16)
        zzb = sb("A_zzb", [128, 4, 512], BF16)
        tmp32 = [sb("A_tmp32%d" % i, [128, 512], F32) for i in range(2)]
        vaug = sb("A_vaug", [128, 4, 4, 130], BF16)
        k.memset("pool", vaug[:], 0.0)
        ba = sb("A_ba", [128, 4, 8], F32)
        bgb = sb("A_bgb", [128, 4, 8], F32)
        spx = sb("A_spx", [128, 4, 4], F32)
        dq = [sb("A_dq%d" % i, [128, 512], BF16) for i in range(2)]
        dk = [sb("A_dk%d" % i, [128, 512], BF16) for i in range(2)]
        ps = g.ps
        psb = [p[:].bitcast(BF16) for p in ps]

        st = {"bank": 0, "cnt": 0}
        dfq = []

        def defer(n, fn):
            dfq.append([n, fn])

        def tick():
            for it in dfq:
                it[0] -= 1
            for it in [it for it in dfq if it[0] <= 0]:
                dfq.remove(it)
                it[1]()

        def flush():
            while dfq:
                it = dfq.pop(0)
                it[1]()

        def nextbank():
            st["bank"] = (st["bank"] + 1) % 6
            return 2 + st["bank"]

        def proj_f(hT, col0, M):
            b_ = nextbank()
            for kk in range(8):
                k.mm(ps[b_][0:M, :], (Wb[:, kk, col0:col0 + M], kk), hT[:, kk, :], start=(kk == 0), stop=(kk == 7))
            tick()
            return ps[b_]

        def proj_t(hT, t, col0, N):
            b_ = nextbank()
            for kk in range(8):
                k.mm(ps[b_][:, 0:N], hT[:, kk, t * 128:(t + 1) * 128], (Wb[:, kk, col0:col0 + N], kk), start=(kk == 0), stop=(kk == 7))
            tick()
            return ps[b_]

        def ln_stats(b):
            for t in range(4):
                for hh in range(2):
                    k.op("dve", lambda t=t, hh=hh: nc.vector.bn_stats(out=stats[:, t, hh, :], in_=xblk[:, t, hh * 512:(hh + 1) * 512]),
                         [stats[:]], [xblk[:]])
                k.op("dve", lambda t=t: nc.vector.bn_aggr(out=mv[:, t, :], in_=stats[:, t, :, :].rearrange("p a b -> p (a b)")),
                     [mv[:]], [stats[:]])
            k.act(rstd[:], mv[:, :, 1], AF.Sqrt, bias=epsc[:, 1:2])
            k.op("dve", lambda: nc.vector.reciprocal(out=rstd[:], in_=rstd[:]), [rstd[:]], [rstd[:]])

        def ln_apply(b, t):
            own = b >= nbc
            o0 = b * 512 - Tc
            xn32 = xn32s[t % 2]
            if own:
                k.ts("dve", xn32[:], xblk[:, t, :], mv[:, t, 0:1], rstd[:, t:t + 1], op0=ALU.subtract, op1=ALU.mult)
                k.copy("act", xn[:, t, :], xn32[:])
                k.tt("pool", xn32[:], xn32[:], eg_bc[:], ALU.mult)
                k.tt("pool", xn32[:], xn32[:], eb_bc[:], ALU.add)
                k.dma("pool", d["hres"][o0 + t * 128:o0 + (t + 1) * 128, :], xn32[:], "hres")
            else:
                k.ts("dve", xn[:, t, :], xblk[:, t, :], mv[:, t, 0:1], rstd[:, t:t + 1], op0=ALU.subtract, op1=ALU.mult)
            if t == 3 and b + 1 < nb:
                k.dma("sp", xblk[:], d["xin"][(b + 1) * 512:(b + 2) * 512, :].rearrange("(t p) f -> p t f", p=128), "x")

        def layer_norm(b):
            ln_stats(b)
            for t in range(4):
                ln_apply(b, t)

        def transposes(b):
            hT = hTs[b % 2]
            for kk in range(8):
                pb = psb[kk % 2]
                for t in range(4):
                    k.tr(pb[:, t * 128:(t + 1) * 128], xn[:, t, kk * 128:(kk + 1) * 128], ident[:])
                k.act(hT[:, kk, :], pb[:, 0:512], AF.Identity, bias=ebc[:, kk:kk + 1], scale=egc[:, kk:kk + 1])
            if b >= nbc:
                o0 = b * 512 - Tc
                k.dma("pool", d["hT_own"][:, :, o0:o0 + 512].rearrange("kk p t -> p kk t"), hT[:], "hT")

        k.dma("sp", xblk[:], d["xin"][0:512, :].rearrange("(t p) f -> p t f", p=128), "x")
        k.dma("sp", tmbcs[0][:], d["tmask"][0:512].partition_broadcast(128), "tm")
        layer_norm(0)
        transposes(0)
        for b in range(nb):
            own = b >= nbc
            t0 = b * 512
            o0 = t0 - Tc
            hT = hTs[b % 2]
            tmbc = tmbcs[b % 2]
            if b + 1 < nb:
                k.dma("sp", tmbcs[(b + 1) % 2][:], d["tmask"][t0 + 512:t0 + 1024].partition_broadcast(128), "tm")
            for c in range(12):
                isq = c < 4
                if isq and not (own or b == nbc - 1):
                    continue
                p_ = proj_f(hT, c * 128, 128)
                k.stt((pre[:, c, 3:515], c), p_[:, :], bcol[:, c:c + 1], tmbc[:], ALU.add, ALU.mult)
                if isq and not own:
                    k.copy("pool", (pre[:, c, 0:3], c), (pre[:, c, 512:515], c))
                    continue
                i_ = st["cnt"]
                st["cnt"] += 1
                q_, s_, r_, n_ = qa[i_ % NR], sq[i_ % NR], rinv[i_ % NR], qn[i_ % NR]
                if c >= 8:
                    n_ = vn[i_ % 3]
                h = c % 4

                def stage2(c=c, h=h, n_=n_):
                    dst = ktok if c < 8 else vtok
                    pb = psb[nextbank()]
                    for t in range(4):
                        k.tr(pb[:, t * 128:(t + 1) * 128], n_[:, t * 128:(t + 1) * 128], ident[:])
                    k.copy("act", dst[:, :, h * 128:(h + 1) * 128], pb[:, 0:512].rearrange("p (t d) -> p t d", t=4))

                def stage1b(c=c, h=h, q_=q_, r_=r_, n_=n_, isq=isq, stage2=stage2, o0=o0, t0=t0):
                    k.op("dve", lambda: nc.vector.reciprocal(out=r_[:], in_=r_[:]), [r_[:]], [r_[:]])
                    if isq:
                        k.stt(n_[:], q_[:], 128.0 ** -0.5, r_[:], ALU.mult, ALU.mult)
                        k.dma("pool", d["dn_qT"][h, :, o0:o0 + 512], n_[:], "o0")
                    else:
                        k.tt("dve", n_[:], q_[:], r_[:], ALU.mult)
                        k.dma("pool", d["dn_kT"][h, :, t0:t0 + 512], n_[:], "o1")
                        defer(2, stage2)

                def stage1(c=c, s_=s_, r_=r_, stage1b=stage1b):
                    b2 = nextbank()
                    k.mm(ps[b2][:, :], onesb[:], s_[:])
                    k.act(r_[:], ps[b2][:, :], AF.Sqrt, bias=epsc[:, 0:1])
                    defer(1, stage1b)

                def stage0(c=c, q_=q_, s_=s_, n_=n_, stage1=stage1, stage2=stage2):
                    b3 = nextbank()
                    for j in range(4):
                        k.mm(ps[b3][:, :], diagw[:, c, j, :], (pre[:, c, j:j + 512], c), start=(j == 0), stop=(j == 3))
                    k.copy("pool", (pre[:, c, 0:3], c), (pre[:, c, 512:515], c))
                    if c < 8:
                        k.act(q_[:], ps[b3][:, :], AF.Silu)
                        k.act(s_[:], q_[:], AF.Square)
                        defer(2, stage1)
                    else:
                        k.act(n_[:], ps[b3][:, :], AF.Silu)
                        defer(2, stage2)

                defer(2, stage0)
            if b + 1 < nb:
                ln_stats(b + 1)
            for t in range(4):
                if b + 1 < nb:
                    ln_apply(b + 1, t)
                if own:
                    p_ = proj_t(hT, t, C_Z, 512)
                    k.tt("dve", tmp32[0][:], p_[:, :], zb_bc[:], ALU.add)
                    k.act(tmp32[0][:], tmp32[0][:], AF.Silu)
                    k.tt("pool", zzb[:, t, :], tmp32[0][:], nw_bc[:].rearrange("p h d -> p (h d)"), ALU.mult)
                p_ = proj_t(hT, t, C_B, 8)
                k.tt("dve", ba[:, t, :], p_[:, 0:8], bab_bc[:], ALU.add)
                p_ = proj_t(hT, t, C_DV, 512)
                k.tt("dve", tmp32[1][:], p_[:, :], vb_bc[:], ALU.add)
                tmc = tm_tok[:, b * 4 + t:b * 4 + t + 1]
                k.act(vaug[:, t, :, 0:128], tmp32[1][:].rearrange("p (h d) -> p h d", h=4), AF.Copy, scale=tmc)
                k.copy("pool", vaug[:, t, :, 128:129], tmc.unsqueeze(1).broadcast_to([128, 4, 1]))
            if own:
                k.dma("pool", d["dn_zz"][o0:o0 + 512, :].rearrange("(t p) f -> p t f", p=128), zzb[:], "o4")
            k.dma("pool", d["da_v"][t0:t0 + 512, :, :].rearrange("(t p) h e -> p t h e", p=128), vaug[:], "o5")
            k.act(bgb[:, :, 0:4], ba[:, :, 0:4], AF.Sigmoid)
            k.tt("dve", spx[:], ba[:, :, 4:8], dtb_bc[:].unsqueeze(1).broadcast_to([128, 4, 4]), ALU.add)
            k.act(spx[:], spx[:], AF.Exp)
            k.act(spx[:], spx[:], AF.Ln, bias=epsc[:, 2:3])
            k.tt("dve", bgb[:, :, 4:8], spx[:], negA[:].unsqueeze(1).broadcast_to([128, 4, 4]), ALU.mult)
            k.dma("pool", d["dn_bg"][t0:t0 + 512, 0:8].rearrange("(t p) f -> p t f", p=128), bgb[:], "o6")
            if b + 1 < nb:
                transposes(b + 1)
            for h in range(4):
                if own:
                    p_ = proj_f(hT, C_DQ + h * 128, 128)
                    q_ = dq[h % 2]
                    k.ts("dve", q_[:], p_[:, :], qcol[:, h:h + 1], 0.125, op0=ALU.add, op1=ALU.mult)
                    k.dma("pool", d["da_qT"][h, :, :, o0:o0 + 512].rearrange("m d t -> (m d) t"), q_[:], "o7")
                p_ = proj_f(hT, C_DK + h * 128, 128)
                k_ = dk[h % 2]
                k.ts("dve", k_[:], p_[:, :], kcol[:, h:h + 1], None, op0=ALU.add)
                k.op("dve", lambda k_=k_, h=h, b=b: nc.vector.tensor_reduce(out=kmx[:, h, b:b + 1], in_=k_[:], axis=AX.X, op=ALU.max,
                                                                          apply_absolute_value=True), [kmx[:]], [k_[:]])
                k.dma("pool", d["da_kT"][h, :, :, t0:t0 + 512].rearrange("m d t -> (m d) t"), k_[:], "o8")
            flush()
            k.dma("pool", d["dn_ktok"][t0:t0 + 512, :].rearrange("(t p) f -> p t f", p=128), ktok[:], "o2")
            k.dma("pool", d["dn_vtok"][t0:t0 + 512, :].rearrange("(t p) f -> p t f", p=128), vtok[:], "o3")
        kmxf = sb("A_kmxf", [128, 4], F32)
        k.op("dve", lambda: nc.vector.tensor_reduce(out=kmxf[:], in_=kmx[:], axis=AX.X, op=ALU.max), [kmxf[:]], [kmx[:]])
        for m in range(2):
            k.dma("pool", d["kmaxabs"][:, 0:8].rearrange("d (h m) -> d h m", m=2)[:, :, m], kmxf[m * 64:(m + 1) * 64, :], "o9",
                  allow_slow_non_contiguous=True)
        k.barrier()


def declare_dram(nc, g, debug):
    Tc, To, T = g.Tc, g.To, g.T
    d = {}
    kin = "ExternalInput"
    d["xin"] = dram(nc, "xin", [T, D], F32, kin)
    d["pin"] = dram(nc, "pin", [To, 256], F32, kin)
    d["tm_tok"] = dram(nc, "tm_tok", [128, T // 128], F32, kin)
    d["tmask"] = dram(nc, "tmask", [T], F32, kin)
    for n in INPUT_NAMES:
        d[n] = dram(nc, n, W_SHAPES[n], F32, kin)
    for n, a in make_consts(Tc, To).items():
        d[n] = dram(nc, n, a.shape, F32, kin)
    sk = "ExternalOutput" if debug else "Internal"
    g.scratch = {}

    def scr(name, shape, dt):
        d[name] = dram(nc, name, shape, dt, sk)
        g.scratch[name] = (shape, dt)
    scr("hres", [To, D], F32)
    scr("dn_qT", [4, 128, To], BF16)
    scr("dn_kT", [4, 128, T], BF16)
    scr("dn_ktok", [T, 512], BF16)
    scr("dn_vtok", [T, 512], BF16)
    scr("dn_bg", [T, 16], F32)
    scr("dn_zz", [To, 512], BF16)
    scr("da_qT", [4, 2, 64, To], BF16)
    scr("da_kT", [4, 2, 64, T], BF16)
    scr("da_v", [T, 4, 130], BF16)
    scr("hT_own", [8, 128, To], BF16)
    scr("kmaxabs", [64, 16], F32)
    scr("oT_dn", [4, 128, To], BF16)
    scr("oT_da", [4, 128, To], BF16)
    scr("h1", [To, D], F32)
    scr("h1T", [8, 128, To], BF16)
    scr("comb", [To, 16], F32)
    scr("ffn0", [To, D], F32)
    d["out"] = dram(nc, "out", [To, D], F32, "ExternalOutput")
    return d


def build(Tc, To, debug=False, phases="A"):
    nc = bass.Bass("TRN2", target_bir_lowering=False)
    g = Ctx()
    g.Tc, g.To, g.T = Tc, To, Tc + To
    g.d = declare_dram(nc, g, debug)
    es = contextlib.ExitStack()
    with es:
        k = KB(nc, es)
        g.ps = [es.enter_context(nc.psum_tensor("ps%d" % i, [128, 512], F32)) for i in range(8)]
        if "A" in phases:
            phase_A(k, g)
        if "B" in phases and "C" in phases and "S" not in phases:
            with contextlib.ExitStack() as esb:
                gen = gen_B(k, g, esb, (4, 5, 7))
                state = {"done": False}
                next(gen)

                def stepper():
                    if not state["done"]:
                        try:
                            next(gen)
                        except StopIteration:
                            state["done"] = True
                phase_C(k, g, stepper)
                while not state["done"]:
                    stepper()
                k.barrier()
        else:
            if "B" in phases:
                phase_B(k, g)
            if "C" in phases:
                phase_C(k, g)
        if "D" in phases:
            phase_D(k, g)
        if "E" in phases:
            phase_E(k, g)
        k.barrier()
        print("instructions:", k.ninstr, "sems:", len(k.sems))
    return nc, g


def gen_B(k, g, es, bk):
    nc = k.nc
    d = g.d
    Tc, To, T = g.Tc, g.To, g.T
    nch = T // 128
    ncc = Tc // 128
    ps = g.ps
    psb = [p[:].bitcast(BF16) for p in ps]
    X, Y, Z = [ps[i] for i in bk]
    XB = X[:].bitcast(BF16)
    if True:
        sb = lambda name, shape, dt: k.sb(name, shape, dt, es)
        U = sb("B_U", [128, 128], F32); k.dma("sp", U[:], d["U"][:, :], "c")
        negU = sb("B_negU", [128, 128], F32); k.dma("sp", negU[:], d["negU"][:, :], "c")
        ones = sb("B_ones", [128, 128], F32); k.dma("sp", ones[:], d["ones"][:, :], "c")
        identb = sb("B_identb", [128, 128], BF16); k.dma("pool", identb[:], d["ident"][:, :], "c")
        MLs = sb("B_MLs", [128, 512], F32); k.dma("sp", MLs[:], d["MLs"][:, :], "c")
        MUi = sb("B_MUi", [128, 512], F32); k.dma("sp", MUi[:], d["MUi"][:, :], "c")
        SU01 = sb("B_SU01", [128, 512], F32); k.dma("sp", SU01[:], d["SU01"][:, :], "c")
        epsc = sb("B_epsc", [128, 1], F32); k.memset("dve", epsc[:], 1e-6)
        S32 = sb("B_S32", [128, 4, 128], F32); k.memset("dve", S32[:], 0.0)
        Sb = sb("B_Sb", [128, 4, 128], BF16); k.memset("pool", Sb[:], 0.0)
        KT = [sb("B_KT%d" % i, [128, 4, 128], BF16) for i in range(2)]
        QT = [sb("B_QT%d" % i, [128, 4, 128], BF16) for i in range(2)]
        ktok = [sb("B_ktok%d" % i, [128, 4, 128], BF16) for i in range(2)]
        vtok = [sb("B_vtok%d" % i, [128, 4, 128], BF16) for i in range(2)]
        zz = [sb("B_zz%d" % i, [128, 4, 128], BF16) for i in range(2)]
        bg = [sb("B_bg%d" % i, [128, 8], F32) for i in range(2)]
        gB = sb("B_gB", [128, 4, 128], F32)
        Gc = sb("B_Gc", [128, 4], F32)
        cb = sb("B_cb", [128, 4], F32)
        cg = sb("B_cg", [128, 4], F32)
        egl = sb("B_egl", [128, 4], F32)
        nbeta = sb("B_nbeta", [128, 4], F32)
        argL = sb("B_argL", [128, 4, 128], F32)
        argU = sb("B_argU", [128, 4, 128], F32)
        Dst = sb("B_Dst", [128, 4, 128], F32)
        DTi = sb("B_DTi", [128, 4, 128], F32)
        kbg = sb("B_kbg", [128, 4, 128], BF16)
        kg = sb("B_kg", [128, 4, 128], BF16)
        vb = sb("B_vb", [128, 4, 128], BF16)
        P = [sb("B_P%d" % i, [128, 4, 128], BF16) for i in range(2)]
        PT = [sb("B_PT%d" % i, [128, 4, 128], BF16) for i in range(2)]
        AT = [sb("B_AT%d" % i, [128, 4, 128], BF16) for i in range(2)]
        u = sb("B_u", [128, 4, 128], F32)
        wT = sb("B_wT", [128, 4, 128], BF16)
        aT = sb("B_aT", [128, 4, 128], BF16)
        eGrow = sb("B_eGrow", [128, 4, 128], F32)
        QgT = sb("B_QgT", [128, 4, 128], BF16)
        vnew = sb("B_vnew", [128, 4, 128], BF16)
        ssq = sb("B_ssq", [128, 4], F32)
        junk = sb("B_junk", [128, 128], F32)
        og = sb("B_og", [128, 4, 128], BF16)
        oT = [sb("B_oT%d" % i, [128, 4, 128], BF16) for i in range(2)]

        def bc4(ap):
            return ap.unsqueeze(2).broadcast_to([128, 4, 128])

        def load(c):
            s = c % 2
            t0 = c * 128
            k.dma("sp", KT[s][:], d["dn_kT"][:, :, t0:t0 + 128].rearrange("h d t -> d h t"), "bl%d" % s)
            k.dma("sp", ktok[s][:].rearrange("p h d -> p (h d)"), d["dn_ktok"][t0:t0 + 128, :], "bl%d" % s)
            k.dma("sp", vtok[s][:].rearrange("p h d -> p (h d)"), d["dn_vtok"][t0:t0 + 128, :], "bl%d" % s)
            k.dma("sp", bg[s][:], d["dn_bg"][t0:t0 + 128, 0:8], "bl%d" % s)
            if c >= ncc:
                o0 = t0 - Tc
                k.dma("sp", QT[s][:], d["dn_qT"][:, :, o0:o0 + 128].rearrange("h d t -> d h t"), "bl%d" % s)
                k.dma("sp", zz[s][:].rearrange("p h d -> p (h d)"), d["dn_zz"][o0:o0 + 128, :], "bl%d" % s)

        yield
        load(0)
        for c in range(nch):
            s = c % 2
            own = c >= ncc
            o0 = c * 128 - Tc
            if c + 1 < nch:
                load(c + 1)
            beta = bg[s][:, 0:4]
            gg = bg[s][:, 4:8]
            k.copy("dve", gB[:], bc4(gg))
            k.mm(X[:, 0:4], U[:], gg)
            k.mm(X[:, 8:12], ones[:], gg)
            for h in range(4):
                k.mm(Y[:, h * 128:(h + 1) * 128], U[:], gB[:, h, :], start=True, stop=False)
                k.mm(Y[:, h * 128:(h + 1) * 128], gB[:, h, :], negU[:], start=False, stop=True)
            if own:
                for h in range(4):
                    k.mm(Z[:, h * 128:(h + 1) * 128], gB[:, h, :], U[:])
            yield
            k.copy("dve", Gc[:], X[:, 0:4])
            k.tt("dve", cg[:], X[:, 8:12], Gc[:], ALU.subtract)
            k.ts("dve", nbeta[:], beta, -1.0)
            k.tt("dve", argL[:].rearrange("p h d -> p (h d)"), Y[:, :], MLs[:], ALU.add)
            if own:
                k.stt(argU[:].rearrange("p h d -> p (h d)"), Y[:, :], -1.0, MUi[:], ALU.mult, ALU.add)
            yield
            k.act(cb[:], Gc[:], AF.Exp)
            k.act(cg[:], cg[:], AF.Exp)
            k.act(egl[:], X[:, 8:12], AF.Exp)
            k.act(Dst[:], argL[:], AF.Exp)
            if own:
                k.act(DTi[:], argU[:], AF.Exp)
                k.act(eGrow[:].rearrange("p h d -> p (h d)"), Z[:, :], AF.Exp)
            yield
            k.tt("dve", cb[:], cb[:], beta, ALU.mult)
            if own:
                k.tt("pool", QgT[:], QT[s][:], eGrow[:], ALU.mult)
            k.tt("pool", kbg[:], ktok[s][:], bc4(cb[:, :]), ALU.mult)
            k.tt("pool", kg[:], ktok[s][:], bc4(cg[:, :]), ALU.mult)
            k.tt("pool", vb[:], vtok[s][:], bc4(beta), ALU.mult)
            for h in range(4):
                k.mm(X[:, h * 128:(h + 1) * 128], KT[s][:, h, :], KT[s][:, h, :])
            yield
            for h in range(4):
                k.stt(P[0][:, h, :], X[:, h * 128:(h + 1) * 128], nbeta[:, h:h + 1], Dst[:, h, :], ALU.mult, ALU.mult)
            yield
            for h in range(4):
                k.mm(Y[:, h * 128:(h + 1) * 128], P[0][:, h, :], identb[:])
            yield
            k.copy("dve", PT[0][:].rearrange("p h d -> p (h d)"), Y[:, :])
            k.tt("pool", AT[0][:], PT[0][:], identb[:, :].unsqueeze(1).broadcast_to([128, 4, 128]), ALU.add)
            yield
            ci = 0
            ai = 0
            for it in range(1, 7):
                ni = 1 - ci
                for h in range(4):
                    k.mm(X[:, h * 128:(h + 1) * 128], PT[ci][:, h, :], P[ci][:, h, :])
                if it < 6:
                    for h in range(4):
                        k.mm(Y[:, h * 128:(h + 1) * 128], P[ci][:, h, :], PT[ci][:, h, :])
                yield
                k.copy("dve", P[ni][:].rearrange("p h d -> p (h d)"), X[:, :])
                if it < 6:
                    k.copy("dve", PT[ni][:].rearrange("p h d -> p (h d)"), Y[:, :])
                yield
                yield
                for h in range(4):
                    k.mm(Z[:, h * 128:(h + 1) * 128], P[ni][:, h, :], AT[ai][:, h, :])
                yield
                k.tt("dve", AT[1 - ai][:].rearrange("p h d -> p (h d)"), Z[:, :], AT[ai][:].rearrange("p h d -> p (h d)"), ALU.add)
                ai = 1 - ai
                ci = ni
            TT = AT[ai]
            yield
            for h in range(4):
                k.mm(X[:, h * 128:(h + 1) * 128], TT[:, h, :], vb[:, h, :])
            for h in range(4):
                k.mm(Y[:, h * 128:(h + 1) * 128], kbg[:, h, :], TT[:, h, :])
            if own:
                for h in range(4):
                    k.mm(Z[:, h * 128:(h + 1) * 128], KT[s][:, h, :], QT[s][:, h, :])
            yield
            k.copy("dve", u[:].rearrange("p h d -> p (h d)"), X[:, :])
            k.copy("dve", wT[:].rearrange("p h d -> p (h d)"), Y[:, :])
            if own:
                k.tt("dve", aT[:].rearrange("p h d -> p (h d)"), Z[:, :], DTi[:].rearrange("p h d -> p (h d)"), ALU.mult)
            yield
            yield
            for h in range(4):
                k.mm(X[:, h * 128:(h + 1) * 128], wT[:, h, :], Sb[:, h, :])
            yield
            k.tt("dve", vnew[:].rearrange("p h d -> p (h d)"), u[:].rearrange("p h d -> p (h d)"), X[:, :], ALU.subtract)
            yield
            yield
            if own:
                for h in range(4):
                    k.mm(Y[:, h * 128:(h + 1) * 128], QgT[:, h, :], Sb[:, h, :], start=True, stop=False)
                    k.mm(Y[:, h * 128:(h + 1) * 128], aT[:, h, :], vnew[:, h, :], start=False, stop=True)
            for h in range(4):
                k.mm(Z[:, h * 128:(h + 1) * 128], kg[:, h, :], vnew[:, h, :])
            yield
            k.tt("dve", S32[:], S32[:], bc4(egl[:, :]), ALU.mult)
            k.tt("dve", S32[:].rearrange("p h d -> p (h d)"), S32[:].rearrange("p h d -> p (h d)"), Z[:, :], ALU.add)
            k.copy("dve", Sb[:], S32[:])
            if own:
                yield
                for h in range(4):
                    k.act(junk[:], Y[:, h * 128:(h + 1) * 128], AF.Square, accum_out=ssq[:, h:h + 1])
                k.act(ssq[:], ssq[:], AF.Sqrt, bias=epsc[:, 0:1], scale=1.0 / 128.0)
                yield
                k.op("dve", lambda: nc.vector.reciprocal(out=ssq[:], in_=ssq[:]), [ssq[:]], [ssq[:]])
                for h in range(4):
                    k.stt(og[:, h, :], Y[:, h * 128:(h + 1) * 128], ssq[:, h:h + 1], zz[s][:, h, :], ALU.mult, ALU.mult)
                yield
                for h in range(4):
                    k.tr(XB[:, h * 128:(h + 1) * 128], og[:, h, :], identb[:])
                yield
                o_ = oT[c % 2]
                k.copy("dve", o_[:].rearrange("p h d -> p (h d)"), XB[:, 0:512])
                k.dma("pool", d["oT_dn"][:, :, o0:o0 + 128].rearrange("h d t -> d h t"), o_[:], "bo%d" % (c % 2))


def phase_B(k, g):
    with contextlib.ExitStack() as es:
        for _ in gen_B(k, g, es, (4, 5, 7)):
            pass
        k.barrier()


def phase_C(k, g, stepper=None):
    nc = k.nc
    d = g.d
    Tc, To, T = g.Tc, g.To, g.T
    nkt = T // 128
    nqb = To // 512
    ps = g.ps
    psb = [p[:].bitcast(BF16) for p in ps]
    lam_init = 0.8 - 0.6
    with contextlib.ExitStack() as es:
        sb = lambda name, shape, dt: k.sb(name, shape, dt, es)
        identb = sb("C_identb", [128, 128], BF16); k.dma("pool", identb[:], d["ident"][:, :], "c")
        catrib = sb("C_catrib", [128, 128], BF16); k.dma("pool", catrib[:], d["catri"][:, :], "c")
        epsc = sb("C_epsc", [128, 1], F32); k.memset("dve", epsc[:], 1e-6)
        lv = sb("C_lv", [128, 4, 64], F32)
        for i, n in enumerate(["da_lq1", "da_lk1", "da_lq2", "da_lk2"]):
            k.dma("sp", lv[:, i, :], d[n].partition_broadcast(128), "c")
        lp = sb("C_lp", [128, 2, 64], F32)
        k.tt("dve", lp[:, 0, :], lv[:, 0, :], lv[:, 1, :], ALU.mult)
        k.tt("dve", lp[:, 1, :], lv[:, 2, :], lv[:, 3, :], ALU.mult)
        ls = sb("C_ls", [128, 2], F32)
        k.op("dve", lambda: nc.vector.tensor_reduce(out=ls[:], in_=lp[:], axis=AX.X, op=ALU.add), [ls[:]], [lp[:]])
        k.act(ls[:], ls[:], AF.Exp)
        neglam = sb("C_neglam", [128, 1], F32)
        k.tt("dve", neglam[:], ls[:, 1:2], ls[:, 0:1], ALU.subtract)
        k.ts("dve", neglam[:], neglam[:], -lam_init, op0=ALU.add)
        wbc = sb("C_wbc", [128, 128], F32)
        k.dma("sp", wbc[:], d["da_subln_w"].partition_broadcast(128), "c")
        k.ts("dve", wbc[:], wbc[:], 1.0 - lam_init)
        kmx = sb("C_kmx", [64, 8], F32); k.dma("sp", kmx[:], d["kmaxabs"][:, 0:8], "c")
        kmxL = sb("C_kmxL", [64, 8, 65], BF16)
        k.memset("dve", kmxL[:], 0.0)
        k.copy("dve", kmxL[:, :, 64:65], kmx[:, :].unsqueeze(2))

        Va = [sb("C_Va%d" % i, [128, nkt, 130], BF16) for i in range(2)]
        kTa = [[sb("C_kTa%d_%d" % (i, m), [69, T], BF16) for m in range(2)] for i in range(2)]
        qTa = [[sb("C_qTa%d_%d" % (i, m), [69, To], BF16) for m in range(2)] for i in range(2)]
        absqs = [sb("C_absq%d" % i, [64, 512], BF16) for i in range(2)]
        PT = [sb("C_PT%d" % i, [128, 512], BF16) for i in range(4)]
        o1 = sb("C_o1", [128, 4, 128], F32)
        rden = sb("C_rden", [128, 4], F32)
        ssqs = [sb("C_ssq%d" % i, [128, 4], F32) for i in range(2)]
        o1s = [sb("C_o1s%d" % i, [128, 4, 128], F32) for i in range(2)]
        junk = sb("C_junk", [128, 128], F32)
        ons = [sb("C_on%d" % i, [128, 4, 128], BF16) for i in range(2)]
        oT = [sb("C_oT%d" % i, [128, 512], BF16) for i in range(2)]

        def load(h):
            s = h % 2
            k.dma("sp", Va[s][:], d["da_v"][:, h, :].rearrange("(kt p) e -> p kt e", p=128), "cl%d" % s)
            for m in range(2):
                k.dma("sp", kTa[s][m][0:64, :], d["da_kT"][h, m, :, :], "cl%d" % s)
                k.dma("pool", kTa[s][m][64:69, :], d["augk"][h, :, :], "cl%d" % s)
                k.dma("sp", qTa[s][m][0:64, :], d["da_qT"][h, m, :, :], "cl%d" % s)
                k.dma("pool", qTa[s][m][65:69, :], d["augq"][h, :, :], "cl%d" % s)

        accs = sb("C_accs", [128, 4, 129], F32)

        def accv(j):
            return ps[2 + j // 2][:, (j % 2) * 256:(j % 2) * 256 + 129]

        load(0)
        st = {"pti": 0, "si": 0, "pend": [], "epi": 0, "ab": 0}
        for h in range(4):
            s = h % 2
            if h + 1 < 4:
                load(h + 1)
            def bound(hh, m, qb):
                s2 = hh % 2
                hm = hh * 2 + m
                a_ = absqs[st["ab"] % 2]
                st["ab"] += 1
                k.act(a_[:], qTa[s2][m][0:64, qb * 512:(qb + 1) * 512], AF.Abs)
                k.mm(ps[6][0:65, :], kmxL[:, hm, :], a_[:])
                k.act(qTa[s2][m][64:65, qb * 512:(qb + 1) * 512], ps[6][64:65, :], AF.Copy, scale=-1.0)
                return lambda: None
            if h == 0:
                for m in range(2):
                    for qb in range(nqb):
                        bound(0, m, qb)()
            for qb in range(nqb):
                Q0 = Tc // 128 + 4 * qb
                for m in range(2):
                    def emit_S(kt):
                        c = max(kt - Q0, 0)
                        lo = 128 * c
                        pS = ps[(0, 1, 6)[st["si"] % 3]]
                        st["si"] += 1
                        dg = kt >= Q0
                        k.mm(pS[:, lo:512], kTa[s][m][0:69, kt * 128:(kt + 1) * 128], qTa[s][m][0:69, qb * 512 + lo:(qb + 1) * 512],
                             start=True, stop=not dg)
                        if dg:
                            k.mm(pS[:, lo:lo + 128], identb[:], catrib[:], start=False, stop=True)
                        P_ = PT[st["pti"] % 4]
                        st["pti"] += 1
                        k.act(P_[:, lo:512], pS[:, lo:512], AF.Exp)
                        return P_, c
                    nkt = Q0 + 4
                    ahead = [emit_S(0)]
                    if nkt > 1:
                        ahead.append(emit_S(1))
                    for kt in range(nkt):
                        P_, c = ahead.pop(0)
                        if kt + 2 < nkt:
                            ahead.append(emit_S(kt + 2))
                        for j in range(c, 4):
                            k.mm(accv(j), P_[:, j * 128:(j + 1) * 128], Va[s][:, kt, 0:129],
                                 start=(kt == 0 and j % 2 == 0), stop=(kt == Q0 + j), skip_group_check=True)
                        if stepper is not None:
                            stepper()
                        if st["pend"] and kt in (1, 3, 5):
                            st["pend"].pop(0)()
                        if kt == min(6, Q0 + 3) - 1 and h + 1 < 4:
                            st["bnd"] = bound(h + 1, m, qb)
                        elif st.get("bnd") is not None:
                            st["bnd"]()
                            st["bnd"] = None
                    for j in range(4):
                        k.copy("dve", accs[:, j, :], accv(j))
                    k.op("dve", lambda: nc.vector.reciprocal(out=rden[:], in_=accs[:, :, 128]), [rden[:]], [accs[:]])
                    if m == 0:
                        for j in range(4):
                            k.ts("dve", o1[:, j, :], accs[:, j, 0:128], rden[:, j:j + 1])
                    else:
                        k.ts("dve", rden[:], rden[:], neglam[:, 0:1])
                        o1_ = o1s[st["epi"] % 2]
                        for j in range(4):
                            k.stt(o1_[:, j, :], accs[:, j, 0:128], rden[:, j:j + 1], o1[:, j, :], ALU.mult, ALU.add)
                on_ = ons[st["epi"] % 2]
                ssq_ = ssqs[st["epi"] % 2]
                st["epi"] += 1

                def e_act(o1_=o1_, ssq_=ssq_):
                    for j in range(4):
                        k.act(junk[:], o1_[:, j, :], AF.Square, accum_out=ssq_[:, j:j + 1])
                    k.act(ssq_[:], ssq_[:], AF.Sqrt, bias=epsc[:, 0:1], scale=1.0 / 128.0)

                def e_dve(o1_=o1_, ssq_=ssq_, on_=on_):
                    k.op("dve", lambda: nc.vector.reciprocal(out=ssq_[:], in_=ssq_[:]), [ssq_[:]], [ssq_[:]])
                    for j in range(4):
                        k.stt(on_[:, j, :], o1_[:, j, :], ssq_[:, j:j + 1], wbc[:], ALU.mult, ALU.mult)

                def e_pe(h=h, qb=qb, on_=on_):
                    for j in range(4):
                        k.tr(psb[6][:, j * 128:(j + 1) * 128], on_[:, j, :], identb[:])
                    o_ = oT[qb % 2]
                    k.copy("dve", o_[:], psb[6][:, 0:512])
                    k.dma("pool", d["oT_da"][h, :, qb * 512:(qb + 1) * 512], o_[:], "co%d" % (qb % 2))
                while st["pend"]:
                    st["pend"].pop(0)()
                st["pend"] = [e_act, e_dve, e_pe]
        while st["pend"]:
            st["pend"].pop(0)()
        k.barrier()


ALPHA = 2.0 ** 0.25


def layer_norm_tile(k, nc, r, gbc, bbc, out, stats, mv, rstd, epsc5):
    for hh in range(2):
        k.op("dve", lambda hh=hh: nc.vector.bn_stats(out=stats[:, hh, :], in_=r[:, hh * 512:(hh + 1) * 512]), [stats[:]], [r[:]])
    k.op("dve", lambda: nc.vector.bn_aggr(out=mv[:], in_=stats[:].rearrange("p a b -> p (a b)")), [mv[:]], [stats[:]])
    k.act(rstd[:], mv[:, 1:2], AF.Sqrt, bias=epsc5)
    k.op("dve", lambda: nc.vector.reciprocal(out=rstd[:], in_=rstd[:]), [rstd[:]], [rstd[:]])
    k.ts("dve", out[:], r[:], mv[:, 0:1], rstd[:, 0:1], op0=ALU.subtract, op1=ALU.mult)
    k.tt("pool", out[:], out[:], gbc[:], ALU.mult)
    k.tt("pool", out[:], out[:], bbc[:], ALU.add)


def phase_D(k, g):
    nc = k.nc
    d = g.d
    To = g.To
    nqb = To // 512
    ps = g.ps
    psb = [p[:].bitcast(BF16) for p in ps]
    with contextlib.ExitStack() as es:
        sb = lambda name, shape, dt: k.sb(name, shape, dt, es)
        identb = sb("D_identb", [128, 128], BF16); k.dma("pool", identb[:], d["ident"][:, :], "c")
        wdn = sb("D_wdn", [128, 4, 1024], BF16)
        k.dma("pool", wdn[:], d["w_dn_o"].rearrange("(kk p) n -> p kk n", p=128), "w0")
        wda = sb("D_wda", [128, 4, 1024], BF16)
        k.dma("pool", wda[:], d["w_da_o"].rearrange("(kk p) n -> p kk n", p=128), "w1")
        wout = sb("D_wout", [128, 8, 1024], BF16)
        k.dma("pool", wout[:], d["w_out"].rearrange("(kk p) n -> p kk n", p=128), "w0")
        wr = sb("D_wr", [128, 8, 20], BF16)
        k.dma("pool", wr[:, :, 0:4], d["w_router_group"].rearrange("(kk p) n -> p kk n", p=128), "w1")
        k.dma("pool", wr[:, :, 4:20], d["w_router_expert"].rearrange("(kk p) n -> p kk n", p=128), "w1")
        rb = sb("D_rb", [128, 20], F32)
        k.dma("sp", rb[:, 0:4], d["b_router_group"].partition_broadcast(128), "c")
        k.dma("sp", rb[:, 4:20], d["b_router_expert"].partition_broadcast(128), "c")
        gbc = sb("D_gbc", [128, 1024], F32); k.dma("sp", gbc[:], d["ln1_g"].partition_broadcast(128), "c")
        bbc = sb("D_bbc", [128, 1024], F32); k.dma("sp", bbc[:], d["ln1_b"].partition_broadcast(128), "c")
        epsc = sb("D_epsc", [128, 1], F32); k.memset("dve", epsc[:], 1e-5)
        oTdn = [sb("D_oTdn%d" % i, [128, 4, 512], BF16) for i in range(2)]
        oTda = [sb("D_oTda%d" % i, [128, 4, 512], BF16) for i in range(2)]
        hTo = [sb("D_hTo%d" % i, [128, 8, 512], BF16) for i in range(2)]
        Wg = sb("D_Wg", [128, 8, 2048], BF16)
        for kk in range(8):
            k.dma("pool", (Wg[:, kk, :], kk), d["w_in"][kk * 128:(kk + 1) * 128, C_G:C_G + 2048], "w%d" % (kk % 2))
        gcolb = sb("D_gcolb", [128, 16], F32)
        colload(k, "sp", gcolb[:, :], d["b_in"][C_G:C_G + 2048], 16)
        g1 = [sb("D_g1_%d" % i, [128, 512], BF16) for i in range(2)]
        g2 = [sb("D_g2_%d" % i, [128, 512], BF16) for i in range(2)]
        hres = [sb("D_hres%d" % i, [128, 4, 1024], F32) for i in range(2)]
        t1 = [sb("D_t1_%d" % i, [128, 512], F32) for i in range(2)]
        t2 = [sb("D_t2_%d" % i, [128, 512], F32) for i in range(2)]
        mergeds = [sb("D_merged%d" % i, [128, 8, 512], BF16) for i in range(2)]
        r = [sb("D_r%d" % i, [128, 1024], F32) for i in range(2)]
        h1 = [sb("D_h1_%d" % i, [128, 1024], F32) for i in range(2)]
        h1bs = [sb("D_h1b%d" % i, [128, 1024], BF16) for i in range(2)]
        h1T = [sb("D_h1T%d" % i, [128, 8, 128], BF16) for i in range(2)]
        stats = sb("D_stats", [128, 2, 6], F32)
        mv = sb("D_mv", [128, 2], F32)
        rstd = sb("D_rstd", [128, 1], F32)
        lg = sb("D_lg", [128, 20], F32)
        sm = sb("D_sm", [128, 16], F32)
        ge = sb("D_ge", [128, 4], F32)
        ohg = sb("D_ohg", [128, 4], F32)
        tmp16 = sb("D_tmp16", [128, 4, 4], F32)
        ig = sb("D_ig", [128, 4], F32)
        ig2 = sb("D_ig2", [128, 4], F32)
        mk1 = sb("D_mk1", [128, 4], F32)
        mk2 = sb("D_mk2", [128, 4], F32)
        cwi = sb("D_cwi", [128, 4], F32)
        comb = [sb("D_comb%d" % i, [128, 4, 4], F32) for i in range(2)]

        def load(qb):
            s = qb % 2
            q0 = qb * 512
            k.dma("sp", oTdn[s][:], d["oT_dn"][:, :, q0:q0 + 512].rearrange("h d t -> d h t"), "dl%d" % s)
            k.dma("sp", oTda[s][:], d["oT_da"][:, :, q0:q0 + 512].rearrange("h d t -> d h t"), "dl%d" % s)
            k.dma("sp", hTo[s][:], d["hT_own"][:, :, q0:q0 + 512].rearrange("kk p t -> p kk t"), "dl%d" % s)
            k.dma("sp", hres[s][:], d["hres"][q0:q0 + 512, :].rearrange("(t p) f -> p t f", p=128), "dl%d" % s)

        load(0)
        ti = 0
        for qb in range(nqb):
            s = qb % 2
            q0 = qb * 512
            if qb + 1 < nqb:
                load(qb + 1)
            def cpart(qb_, cs):
                s_ = qb_ % 2
                mg = mergeds[qb_ % 2]
                for c in cs:
                    pg1, pg2, pa, pb_ = ps[0], ps[1], ps[2], ps[3]
                    for kk in range(8):
                        k.mm(pg1[:, :], (Wg[:, kk, c * 128:(c + 1) * 128], kk), hTo[s_][:, kk, :], start=(kk == 0), stop=(kk == 7))
                    k.act(g1[c % 2][:], pg1[:, :], AF.Sigmoid, bias=gcolb[:, c:c + 1])
                    for kk in range(8):
                        k.mm(pg2[:, :], (Wg[:, kk, 1024 + c * 128:1024 + (c + 1) * 128], kk), hTo[s_][:, kk, :], start=(kk == 0), stop=(kk == 7))
                    k.act(g2[c % 2][:], pg2[:, :], AF.Sigmoid, bias=gcolb[:, 8 + c:9 + c])
                    for kk in range(4):
                        k.mm(pa[:, :], wdn[:, kk, c * 128:(c + 1) * 128], oTdn[s_][:, kk, :], start=(kk == 0), stop=(kk == 3))
                    for kk in range(4):
                        k.mm(pb_[:, :], wda[:, kk, c * 128:(c + 1) * 128], oTda[s_][:, kk, :], start=(kk == 0), stop=(kk == 3))
                    a_, b_ = t1[c % 2], t2[c % 2]
                    k.tt("dve", a_[:], pa[:, :], g1[c % 2][:], ALU.mult)
                    k.tt("dve", b_[:], pb_[:, :], g2[c % 2][:], ALU.mult)
                    k.tt("pool", mg[:, c, :], a_[:], b_[:], ALU.add)

            merged = mergeds[qb % 2]
            if qb == 0:
                cpart(0, range(8))
            def bufs(i):
                return r[i % 2], h1[i % 2], h1T[i % 2], comb[i % 2], h1bs[i % 2]

            def S1(t, i):
                r_, h_, hT_, cb_, h1b = bufs(i)
                for n in range(2):
                    pm = ps[4 + n]
                    for kk in range(8):
                        k.mm(pm[:, :], merged[:, kk, t * 128:(t + 1) * 128], wout[:, kk, n * 512:(n + 1) * 512], start=(kk == 0), stop=(kk == 7))
                    k.stt(r_[:, n * 512:(n + 1) * 512], hres[s][:, t, n * 512:(n + 1) * 512], ALPHA, pm[:, :], ALU.mult, ALU.add)
                layer_norm_tile(k, nc, r_, gbc, bbc, h_, stats, mv, rstd, epsc[:, 0:1])
                k.dma("pool", d["h1"][q0 + t * 128:q0 + (t + 1) * 128, :], h_[:], "do0")
                k.copy("act", h1b[:], h_[:])

            def S2(t, i):
                r_, h_, hT_, cb_, h1b = bufs(i)
                for kk in range(8):
                    k.tr(psb[6][:, kk * 128:(kk + 1) * 128], h1b[:, kk * 128:(kk + 1) * 128], identb[:])
                k.copy("act", hT_[:].rearrange("p a b -> p (a b)"), psb[6][:, 0:1024])
                k.dma("pool", d["h1T"][:, :, q0 + t * 128:q0 + (t + 1) * 128].rearrange("kk p t -> p kk t"), hT_[:], "do1")
                for kk in range(8):
                    k.mm(ps[7][:, 0:20], hT_[:, kk, :], wr[:, kk, :], start=(kk == 0), stop=(kk == 7))
                k.tt("dve", lg[:], ps[7][:, 0:20], rb[:], ALU.add)
                k.op("dve", lambda: nc.vector.tensor_reduce(out=sm[:, 0:1], in_=lg[:, 0:4], axis=AX.X, op=ALU.max), [sm[:]], [lg[:]])
                k.ts("dve", sm[:, 1:2], sm[:, 0:1], -1.0)
                k.act(ge[:], lg[:, 0:4], AF.Exp, bias=sm[:, 1:2])
                k.op("dve", lambda: nc.vector.tensor_reduce(out=sm[:, 2:3], in_=ge[:], axis=AX.X, op=ALU.add), [sm[:]], [ge[:]])
                k.op("dve", lambda: nc.vector.reciprocal(out=sm[:, 3:4], in_=sm[:, 2:3]), [sm[:]], [sm[:]])
                k.ts("dve", ohg[:], lg[:, 0:4], sm[:, 0:1], op0=ALU.is_equal)
                k.tt("dve", tmp16[:], lg[:, 4:20].rearrange("p (g e) -> p g e", g=4), ohg[:, :].unsqueeze(2).broadcast_to([128, 4, 4]), ALU.mult)
                k.op("dve", lambda: nc.vector.tensor_reduce(out=ig[:], in_=tmp16[:].rearrange("p g e -> p e g"), axis=AX.X, op=ALU.add), [ig[:]], [tmp16[:]])
                k.op("dve", lambda: nc.vector.tensor_reduce(out=sm[:, 4:5], in_=ig[:], axis=AX.X, op=ALU.max), [sm[:]], [ig[:]])
                k.ts("dve", mk1[:], ig[:], sm[:, 4:5], op0=ALU.is_equal)
                k.stt(ig2[:], mk1[:], -1e9, ig[:], ALU.mult, ALU.add)
                k.op("dve", lambda: nc.vector.tensor_reduce(out=sm[:, 5:6], in_=ig2[:], axis=AX.X, op=ALU.max), [sm[:]], [ig2[:]])
                k.ts("dve", mk2[:], ig2[:], sm[:, 5:6], op0=ALU.is_equal)
                k.tt("dve", sm[:, 6:7], sm[:, 5:6], sm[:, 4:5], ALU.subtract)
                k.act(sm[:, 7:8], sm[:, 6:7], AF.Exp)
                k.ts("dve", sm[:, 8:9], sm[:, 7:8], 1.0, op0=ALU.add)
                k.op("dve", lambda: nc.vector.reciprocal(out=sm[:, 9:10], in_=sm[:, 8:9]), [sm[:]], [sm[:]])
                k.tt("dve", sm[:, 10:11], sm[:, 9:10], sm[:, 3:4], ALU.mult)
                k.tt("dve", sm[:, 11:12], sm[:, 10:11], sm[:, 7:8], ALU.mult)
                k.ts("dve", cwi[:], mk1[:], sm[:, 10:11])
                k.stt(cwi[:], mk2[:], sm[:, 11:12], cwi[:], ALU.mult, ALU.add)
                k.tt("dve", cb_[:], ohg[:, :].unsqueeze(2).broadcast_to([128, 4, 4]), cwi[:, :].unsqueeze(1).broadcast_to([128, 4, 4]), ALU.mult)
                k.dma("pool", d["comb"][q0 + t * 128:q0 + (t + 1) * 128, :], cb_[:].rearrange("p g e -> p (g e)"), "do2")

            S1(0, ti)
            for t in range(4):
                if t + 1 < 4:
                    S1(t + 1, ti + 1)
                if qb + 1 < nqb:
                    cpart(qb + 1, [2 * t, 2 * t + 1])
                S2(t, ti)
                ti += 1
        k.barrier()


def phase_E(k, g):
    nc = k.nc
    d = g.d
    To = g.To
    nt = To // 128
    ps = g.ps
    psb = [p[:].bitcast(BF16) for p in ps]
    with contextlib.ExitStack() as es:
        sb = lambda name, shape, dt: k.sb(name, shape, dt, es)
        identb = sb("E_identb", [128, 128], BF16); k.dma("pool", identb[:], d["ident"][:, :], "c")
        wgu = sb("E_wgu", [128, 8, 8, 512], BF16)
        wdn = sb("E_wdn", [128, 8, 2, 1024], BF16)
        wpg = sb("E_wpg", [128, 8, 1024], BF16)
        k.dma("pool", wpg[:], d["w_ple_gate"].rearrange("(kk p) n -> p kk n", p=128), "w0")
        wpp = sb("E_wpp", [128, 2, 1024], BF16)
        k.dma("pool", wpp[:], d["w_ple_proj"].rearrange("(kk p) n -> p kk n", p=128), "w1")
        pgb = sb("E_pgb", [128, 1024], F32); k.dma("sp", pgb[:], d["b_ple_gate"].partition_broadcast(128), "c")
        gbc = sb("E_gbc", [128, 1024], F32); k.dma("sp", gbc[:], d["ln2_g"].partition_broadcast(128), "c")
        bbc = sb("E_bbc", [128, 1024], F32); k.dma("sp", bbc[:], d["ln2_b"].partition_broadcast(128), "c")
        epsc = sb("E_epsc", [128, 1], F32); k.memset("dve", epsc[:], 1e-5)
        h1T = [sb("E_h1T%d" % i, [128, 8, 128], BF16) for i in range(2)]
        comb = [sb("E_comb%d" % i, [128, 16], F32) for i in range(2)]
        h1 = [sb("E_h1_%d" % i, [128, 1024], F32) for i in range(2)]
        f0 = [sb("E_f0_%d" % i, [128, 1024], F32) for i in range(2)]
        pt = [sb("E_pt%d" % i, [128, 256], F32) for i in range(2)]
        ptb = sb("E_ptb", [128, 256], BF16)
        pT = sb("E_pT", [128, 2, 128], BF16)
        sg = [sb("E_sg%d" % i, [128, 256], F32) for i in range(2)]
        hid = [sb("E_hid%d" % i, [128, 256], BF16) for i in range(2)]
        hidT = [sb("E_hidT%d" % i, [128, 2, 128], BF16) for i in range(2)]
        fo = [sb("E_fo%d" % i, [128, 1024], F32) for i in range(2)]
        gate = sb("E_gate", [128, 512], F32)
        r = sb("E_r", [128, 1024], F32)
        outt = [sb("E_out%d" % i, [128, 1024], F32) for i in range(2)]
        stats = sb("E_stats", [128, 2, 6], F32)
        mv = sb("E_mv", [128, 2], F32)
        rstd = sb("E_rstd", [128, 1], F32)
        st = {"ei": 0}
        for pas in range(2):
            for e in range(8):
                ge_ = pas * 8 + e
                k.dma("pool", wgu[:, e, :, 0:256], d["w_exp_gate"][ge_].rearrange("(kk p) f -> p kk f", p=128), "w%d" % (e % 2))
                k.dma("pool", wgu[:, e, :, 256:512], d["w_exp_up"][ge_].rearrange("(kk p) f -> p kk f", p=128), "w%d" % (e % 2))
                k.dma("pool", wdn[:, e, :, :], d["w_exp_down"][ge_].rearrange("(fk p) n -> p fk n", p=128), "w%d" % (e % 2))

            def load(t):
                s = t % 2
                k.dma("sp", h1T[s][:], d["h1T"][:, :, t * 128:(t + 1) * 128].rearrange("kk p t -> p kk t"), "el%d" % s)
                k.dma("sp", comb[s][:], d["comb"][t * 128:(t + 1) * 128, :], "el%d" % s)
                if pas == 1:
                    k.dma("sp", h1[s][:], d["h1"][t * 128:(t + 1) * 128, :], "el%d" % s)
                    k.dma("sp", f0[s][:], d["ffn0"][t * 128:(t + 1) * 128, :], "el%d" % s)
                    k.dma("sp", pt[s][:], d["pin"][t * 128:(t + 1) * 128, :], "el%d" % s)

            load(0)
            for t in range(nt):
                s = t % 2
                if t + 1 < nt:
                    load(t + 1)
                def emit_gu(e):
                    pg = ps[2 + st["ei"] % 2]
                    sg_, hid_ = sg[st["ei"] % 2], hid[st["ei"] % 2]
                    st["ei"] += 1
                    ge_ = pas * 8 + e
                    for kk in range(8):
                        k.mm(pg[:, :], h1T[s][:, kk, :], wgu[:, e, kk, :], start=(kk == 0), stop=(kk == 7))
                    k.act(sg_[:], pg[:, 0:256], AF.Silu)
                    k.stt(hid_[:], sg_[:], comb[s][:, ge_:ge_ + 1], pg[:, 256:512], ALU.mult, ALU.mult)
                    return hid_
                def emit_down(e, hidT_):
                    for n in range(2):
                        for fk in range(2):
                            k.mm(ps[n][:, :], hidT_[:, fk, :], wdn[:, e, fk, n * 512:(n + 1) * 512],
                                 start=(e == 0 and fk == 0), stop=(e == 7 and fk == 1))
                nxt = emit_gu(0)
                pend = None
                for e in range(8):
                    hid_ = nxt
                    if e + 1 < 8:
                        nxt = emit_gu(e + 1)
                    hidT_ = hidT[e % 2]
                    pt_ = psb[4 + e % 2]
                    for fk in range(2):
                        k.tr(pt_[:, fk * 128:(fk + 1) * 128], hid_[:, fk * 128:(fk + 1) * 128], identb[:])
                    k.copy("act", hidT_[:].rearrange("p a b -> p (a b)"), pt_[:, 0:256])
                    if pend is not None:
                        emit_down(*pend)
                    pend = (e, hidT_)
                emit_down(*pend)
                if pas == 0:
                    fo_ = fo[t % 2]
                    k.copy("act", fo_[:, 0:512], ps[0][:, :])
                    k.copy("dve", fo_[:, 512:1024], ps[1][:, :])
                    k.dma("pool", d["ffn0"][t * 128:(t + 1) * 128, :], fo_[:], "eo%d" % (t % 2))
                else:
                    k.copy("act", ptb[:], pt[s][:])
                    for kk in range(2):
                        k.tr(psb[6][:, kk * 128:(kk + 1) * 128], ptb[:, kk * 128:(kk + 1) * 128], identb[:])
                    k.copy("act", pT[:].rearrange("p a b -> p (a b)"), psb[6][:, 0:256])
                    for n in range(2):
                        sl = slice(n * 512, (n + 1) * 512)
                        for kk in range(8):
                            k.mm(ps[6][:, :], h1T[s][:, kk, :], wpg[:, kk, sl], start=(kk == 0), stop=(kk == 7))
                        k.tt("dve", gate[:], ps[6][:, :], pgb[:, sl], ALU.add)
                        k.act(gate[:], gate[:], AF.Sigmoid)
                        for kk in range(2):
                            k.mm(ps[7][:, :], pT[:, kk, :], wpp[:, kk, sl], start=(kk == 0), stop=(kk == 1))
                        k.tt("dve", gate[:], gate[:], ps[7][:, :], ALU.mult)
                        k.tt("dve", r[:, sl], f0[s][:, sl], ps[n][:, :], ALU.add)
                        k.tt("pool", r[:, sl], r[:, sl], gate[:], ALU.add)
                        k.stt(r[:, sl], h1[s][:, sl], ALPHA, r[:, sl], ALU.mult, ALU.add)
                    o_ = outt[t % 2]
                    layer_norm_tile(k, nc, r, gbc, bbc, o_, stats, mv, rstd, epsc[:, 0:1])
                    k.dma("pool", d["out"][t * 128:(t + 1) * 128, :], o_[:], "eo%d" % (t % 2))
            k.barrier()
        k.finish([d["out"]])


_CACHE = {}


def kernel(**inputs):
    from concourse.bass_utils import run_bass_kernel_spmd
    x = np.asarray(inputs["x"], dtype=np.float32)
    p = np.asarray(inputs["p"], dtype=np.float32)
    B, S, _ = x.shape
    n_cores = 8
    per = n_cores // B
    To = S // per
    Tc = S - To
    T = Tc + To
    if (Tc, To) not in _CACHE:
        _CACHE[(Tc, To)] = build(Tc, To, debug=False, phases="ABCDE")
    nc, g = _CACHE[(Tc, To)]
    consts = make_consts(Tc, To)
    w = {}
    for n in INPUT_NAMES:
        a = np.asarray(inputs[n], dtype=np.float32)
        if n not in ("emb_ln_g", "emb_ln_b"):
            a = a[0]
        w[n] = np.ascontiguousarray(a)
    in_maps = []
    for c in range(n_cores):
        b, half = c // per, c % per
        own = x[b, half * To:(half + 1) * To]
        if half == 0:
            ctx = x[b, 0:Tc]
            tmask = np.concatenate([np.zeros(Tc, np.float32), np.ones(To, np.float32)])
        else:
            ctx = x[b, 0:Tc]
            tmask = np.ones(T, np.float32)
        m = {"xin": np.ascontiguousarray(np.concatenate([ctx, own], axis=0)),
             "pin": np.ascontiguousarray(p[0, b, half * To:(half + 1) * To]),
             "tmask": tmask,
             "tm_tok": np.ascontiguousarray(tmask.reshape(T // 128, 128).T)}
        m.update(w)
        m.update(consts)
        in_maps.append(m)
    res = run_bass_kernel_spmd(nc, in_maps, core_ids=list(range(n_cores)))
    out = np.empty((B, S, D), np.float32)
    for c in range(n_cores):
        b, half = c // per, c % per
        out[b, half * To:(half + 1) * To] = np.asarray(res.results[c]["out"], dtype=np.float32)
    return out
```

```python
import contextlib
import numpy as np
import concourse.bass as bass
import concourse.mybir as mybir

F32 = mybir.dt.float32
BF16 = mybir.dt.bfloat16
AF = mybir.ActivationFunctionType
ALU = mybir.AluOpType
AX = mybir.AxisListType

EPOCH = 30000


class Res:
    __slots__ = ("w", "r")

    def __init__(self):
        self.w = None
        self.r = {}


class KB:
    def __init__(self, nc, es):
        self.nc = nc
        self.es = es
        self.E = {"pe": nc.tensor, "act": nc.scalar, "dve": nc.vector, "pool": nc.gpsimd, "sp": nc.sync}
        self.sems = {}
        self.cur = {}
        self.epoch = {e: 0 for e in self.E}
        self.waited = {e: {} for e in self.E}
        self.res = {}
        self.dcount = {}
        self.dtarget = {}
        self.ringpos = {}
        self.RINGS = {"c": 6, "w0": 2, "w1": 2}
        self.ninstr = 0
        for e in self.E:
            self._new_epoch(e)

    def _sem(self, key):
        if key not in self.sems:
            self.sems[key] = self.es.enter_context(self.nc.semaphore(key))
        return self.sems[key]

    def _new_epoch(self, e):
        key = f"e_{e}_{self.epoch[e]}"
        self.epoch[e] += 1
        self._sem(key)
        self.cur[e] = [key, 0]

    def R(self, x):
        if isinstance(x, tuple):
            ap, key = x
            k = (ap.tensor.name, key)
        else:
            ap = x
            k = ap.tensor.name
        r = self.res.get(k)
        if r is None:
            r = self.res[k] = Res()
        return ap, r

    def _need(self, eng, ev):
        if ev is None:
            return
        key, val = ev
        if eng == "pe" and key.startswith("e_pe_"):
            return
        if key.startswith("d_"):
            val = self.dcount[key]
            if self.dtarget.get(key, 0) < val:
                self.dtarget[key] = val
        w = self.waited[eng]
        if w.get(key, 0) >= val:
            return
        self.E[eng].wait_ge(self.sems[key], val)
        w[key] = val

    def _pre(self, eng, reads, writes):
        for r in reads:
            self._need(eng, r.w)
        for w in writes:
            self._need(eng, w.w)
            for key, val in w.r.items():
                self._need(eng, (key, val))

    def _post(self, ev, reads, writes):
        key, val = ev
        for r in reads:
            if r.r.get(key, 0) < val:
                r.r[key] = val
        for w in writes:
            w.w = ev
            w.r = {}

    def op(self, eng, fn, outs, ins):
        rs = [self.R(x)[1] for x in ins if x is not None]
        ws = [self.R(x)[1] for x in outs]
        self._pre(eng, rs, ws)
        ins_ = fn()
        c = self.cur[eng]
        ins_.then_inc(self.sems[c[0]], 1)
        c[1] += 1
        ev = (c[0], c[1])
        self._post(ev, rs, ws)
        if c[1] >= EPOCH:
            self._new_epoch(eng)
        self.ninstr += 1
        return ins_

    def dma(self, q, out, in_, stream, **kw):
        oap, ow = self.R(out)
        iap, ir = self.R(in_)
        self._pre(q, [ir], [ow])
        ring = self.RINGS.get(stream, 1)
        if ring > 1:
            i = self.ringpos.get((q, stream), 0)
            self.ringpos[(q, stream)] = i + 1
            stream = "%s%d" % (stream, i % ring)
        key = "d_" + q + "_" + stream
        sem = self._sem(key)
        issued = self.dcount.get(key, 0)
        if issued and self.dtarget.get(key, 0) >= issued:
            self._need(q, (key, issued))
        self.E[q].dma_start(out=oap, in_=iap, **kw).then_inc(sem, 16)
        self.dcount[key] = self.dcount.get(key, 0) + 16
        ev = (key, self.dcount[key])
        self._post(ev, [ir], [ow])
        self.ninstr += 1

    def barrier(self):
        evs = [(c[0], c[1]) for c in self.cur.values() if c[1] > 0]
        evs += [(k, v) for k, v in self.dcount.items()]
        for e in self.E:
            for ev in evs:
                if ev[0].startswith("e_" + e + "_"):
                    continue
                self._need(e, ev)
        self.res = {}

    def finish(self, outs):
        for x in outs:
            _, r = self.R(x)
            self._need("sp", r.w)

    def mm(self, out, lhsT, rhs, start=True, stop=True, **kw):
        o = out[0] if isinstance(out, tuple) else out
        l = lhsT[0] if isinstance(lhsT, tuple) else lhsT
        r = rhs[0] if isinstance(rhs, tuple) else rhs
        return self.op("pe", lambda: self.nc.tensor.matmul(o, l, r, start=start, stop=stop, **kw), [out], [lhsT, rhs])

    def tr(self, out, in_, ident):
        o = out[0] if isinstance(out, tuple) else out
        i = in_[0] if isinstance(in_, tuple) else in_
        return self.op("pe", lambda: self.nc.tensor.transpose(o, i, ident), [out], [in_, ident])

    def act(self, out, in_, func, bias=None, scale=None, accum_out=None, eng="act"):
        o = out[0] if isinstance(out, tuple) else out
        i = in_[0] if isinstance(in_, tuple) else in_
        kw = {}
        ins = [in_]
        if bias is not None:
            kw["bias"] = bias[0] if isinstance(bias, tuple) else bias
            if not isinstance(bias, (int, float)):
                ins.append(bias)
        if scale is not None:
            kw["scale"] = scale[0] if isinstance(scale, tuple) else scale
            if not isinstance(scale, (int, float)):
                ins.append(scale)
        outs = [out]
        if accum_out is not None:
            kw["accum_out"] = accum_out[0] if isinstance(accum_out, tuple) else accum_out
            outs.append(accum_out)
        return self.op("act", lambda: self.nc.scalar.activation(out=o, in_=i, func=func, **kw), outs, ins)

    def tt(self, eng, out, in0, in1, op):
        o = out[0] if isinstance(out, tuple) else out
        a = in0[0] if isinstance(in0, tuple) else in0
        b = in1[0] if isinstance(in1, tuple) else in1
        return self.op(eng, lambda: self.E[eng].tensor_tensor(out=o, in0=a, in1=b, op=op), [out], [in0, in1])

    def ts(self, eng, out, in0, s1, s2=None, op0=ALU.mult, op1=None, accum_out=None):
        o = out[0] if isinstance(out, tuple) else out
        a = in0[0] if isinstance(in0, tuple) else in0
        ins = [in0]
        kw = {}
        if not isinstance(s1, (int, float)):
            ins.append(s1)
            s1 = s1[0] if isinstance(s1, tuple) else s1
        if s2 is not None and not isinstance(s2, (int, float)):
            ins.append(s2)
            s2 = s2[0] if isinstance(s2, tuple) else s2
        if op1 is not None:
            kw["op1"] = op1
        outs = [out]
        if accum_out is not None:
            kw["accum_out"] = accum_out[0] if isinstance(accum_out, tuple) else accum_out
            outs.append(accum_out)
        return self.op(eng, lambda: self.E[eng].tensor_scalar(out=o, in0=a, scalar1=s1, scalar2=s2, op0=op0, **kw), outs, ins)

    def stt(self, out, in0, scalar, in1, op0, op1):
        o = out[0] if isinstance(out, tuple) else out
        a = in0[0] if isinstance(in0, tuple) else in0
        b = in1[0] if isinstance(in1, tuple) else in1
        ins = [in0, in1]
        if not isinstance(scalar, (int, float)):
            ins.append(scalar)
            scalar = scalar[0] if isinstance(scalar, tuple) else scalar
        return self.op("dve", lambda: self.nc.vector.scalar_tensor_tensor(out=o, in0=a, scalar=scalar, in1=b, op0=op0, op1=op1), [out], ins)

    def copy(self, eng, out, in_):
        o = out[0] if isinstance(out, tuple) else out
        i = in_[0] if isinstance(in_, tuple) else in_
        if eng == "act":
            return self.op("act", lambda: self.nc.scalar.copy(out=o, in_=i), [out], [in_])
        return self.op(eng, lambda: self.E[eng].tensor_copy(out=o, in_=i), [out], [in_])

    def memset(self, eng, out, val):
        o = out[0] if isinstance(out, tuple) else out
        return self.op(eng, lambda: self.E[eng].memset(o, val), [out], [])

    def sb(self, name, shape, dt, es=None):
        es = es or self.es
        t = es.enter_context(self.nc.sbuf_tensor(name, shape, dt))
        nbytes = int(np.prod(shape[1:])) * (2 if dt == BF16 else 4)
        rem = (-nbytes) % 128
        if rem:
            es.enter_context(self.nc.sbuf_tensor(name + "_pad", [shape[0], rem // 2], BF16))
        return t


D = 1024
IN_DIM = 5640
C_Z, C_B, C_A, C_DQ, C_DK, C_DV, C_G = 1536, 2048, 2052, 2056, 2568, 3080, 3592
SLOPES = [2.0 ** (-8.0 * (h + 1) / 4) for h in range(4)]


def dram(nc, name, shape, dt, kind="Internal"):
    return nc.dram_tensor(name, list(shape), dt, kind=kind).ap()


class Ctx:
    pass


def make_consts(Tc, To):
    T = Tc + To
    c = {}
    c["ident"] = np.eye(128, dtype=np.float32)
    i = np.arange(128)
    c["U"] = (i[:, None] <= i[None, :]).astype(np.float32)
    c["negU"] = -c["U"]
    c["ones"] = np.ones((128, 128), np.float32)
    c["MLs"] = np.tile(np.where(i[:, None] > i[None, :], 0.0, -1e9).astype(np.float32), (1, 4))
    c["MUi"] = np.tile(np.where(i[None, :] >= i[:, None], 0.0, -1e9).astype(np.float32), (1, 4))
    c["SU01"] = np.tile((i[None, :] > i[:, None]).astype(np.float32), (1, 4))
    c["catri"] = np.where(i[:, None] <= i[None, :], 0.0, -30000.0).astype(np.float32)
    kpos = np.arange(T)
    qpos = Tc + np.arange(To)
    augk = np.zeros((4, 5, T), np.float32)
    augq = np.zeros((4, 4, To), np.float32)
    for h in range(4):
        s = SLOPES[h]
        augk[h, 0] = 1.0
        augk[h, 1] = 1.0
        augk[h, 2] = s * 128.0 * (kpos // 128)
        augk[h, 3] = 1.0
        augk[h, 4] = s * (kpos % 128)
        augq[h, 0] = -s * 128.0 * (qpos // 128)
        augq[h, 1] = 1.0
        augq[h, 2] = -s * (qpos % 128)
        augq[h, 3] = 1.0
    c["augk"] = augk
    c["augq"] = augq
    return c


INPUT_NAMES = ['emb_ln_g', 'emb_ln_b', 'w_in', 'b_in', 'conv_w', 'dn_a_log', 'dn_dt_bias', 'dn_norm_w', 'w_dn_o',
               'da_lq1', 'da_lk1', 'da_lq2', 'da_lk2', 'da_subln_w', 'w_da_o', 'w_out', 'ln1_g', 'ln1_b',
               'w_router_group', 'b_router_group', 'w_router_expert', 'b_router_expert',
               'w_exp_gate', 'w_exp_up', 'w_exp_down', 'w_ple_gate', 'b_ple_gate', 'w_ple_proj', 'ln2_g', 'ln2_b']
W_SHAPES = {
    'emb_ln_g': (1024,), 'emb_ln_b': (1024,), 'w_in': (1024, 5640), 'b_in': (5640,), 'conv_w': (4, 1536),
    'dn_a_log': (4,), 'dn_dt_bias': (4,), 'dn_norm_w': (128,), 'w_dn_o': (512, 1024),
    'da_lq1': (64,), 'da_lk1': (64,), 'da_lq2': (64,), 'da_lk2': (64,), 'da_subln_w': (128,),
    'w_da_o': (512, 1024), 'w_out': (1024, 1024), 'ln1_g': (1024,), 'ln1_b': (1024,),
    'w_router_group': (1024, 4), 'b_router_group': (4,), 'w_router_expert': (1024, 16), 'b_router_expert': (16,),
    'w_exp_gate': (16, 1024, 256), 'w_exp_up': (16, 1024, 256), 'w_exp_down': (16, 256, 1024),
    'w_ple_gate': (1024, 1024), 'b_ple_gate': (1024,), 'w_ple_proj': (256, 1024), 'ln2_g': (1024,), 'ln2_b': (1024,),
}


def colload(k, q, dst, src1d, n, M=128, stream="c"):
    k.dma(q, dst, src1d.rearrange("(c p) -> p c", p=M), stream, allow_slow_non_contiguous=True)


def phase_A(k, g):
    nc = k.nc
    d = g.d
    Tc, To, T = g.Tc, g.To, g.T
    nb = T // 512
    nbc = Tc // 512
    NW = C_G
    with contextlib.ExitStack() as es:
        sb = lambda name, shape, dt: k.sb(name, shape, dt, es)
        Wb = sb("A_Wb", [128, 8, NW], BF16)
        for kk in range(8):
            k.dma("pool", (Wb[:, kk, :], kk), d["w_in"][kk * 128:(kk + 1) * 128, 0:NW], "w%d" % (kk % 2))
        ident = sb("A_ident", [128, 128], BF16)
        k.dma("pool", ident[:], d["ident"][:, :], "c")
        onesb = sb("A_ones", [128, 128], BF16)
        k.dma("pool", onesb[:], d["ones"][:, :], "c")
        bcol = sb("A_bcol", [128, 12], F32)
        colload(k, "sp", bcol[:, :], d["b_in"][0:1536], 12)
        qcol = sb("A_qcol", [128, 4], F32)
        colload(k, "sp", qcol[:, :], d["b_in"][C_DQ:C_DQ + 512], 4)
        kcol = sb("A_kcol", [128, 4], F32)
        colload(k, "sp", kcol[:, :], d["b_in"][C_DK:C_DK + 512], 4)
        egc = sb("A_egc", [128, 8], F32)
        colload(k, "sp", egc[:, :], d["emb_ln_g"], 8)
        ebc = sb("A_ebc", [128, 8], F32)
        colload(k, "sp", ebc[:, :], d["emb_ln_b"], 8)
        cw = sb("A_cw", [128, 12, 4], F32)
        for j in range(4):
            k.dma("sp", cw[:, :, j], d["conv_w"][j, :].rearrange("(c p) -> p c", p=128), "c", allow_slow_non_contiguous=True)
        eg_bc = sb("A_eg_bc", [128, 1024], F32)
        k.dma("sp", eg_bc[:], d["emb_ln_g"].partition_broadcast(128), "c")
        eb_bc = sb("A_eb_bc", [128, 1024], F32)
        k.dma("sp", eb_bc[:], d["emb_ln_b"].partition_broadcast(128), "c")
        zb_bc = sb("A_zb_bc", [128, 512], F32)
        k.dma("sp", zb_bc[:], d["b_in"][C_Z:C_Z + 512].partition_broadcast(128), "c")
        vb_bc = sb("A_vb_bc", [128, 512], F32)
        k.dma("sp", vb_bc[:], d["b_in"][C_DV:C_DV + 512].partition_broadcast(128), "c")
        bab_bc = sb("A_bab_bc", [128, 8], F32)
        k.dma("sp", bab_bc[:], d["b_in"][C_B:C_B + 8].partition_broadcast(128), "c")
        nw_bc = sb("A_nw_bc", [128, 4, 128], F32)
        for h in range(4):
            k.dma("sp", nw_bc[:, h, :], d["dn_norm_w"].partition_broadcast(128), "c")
        dtb_bc = sb("A_dtb_bc", [128, 4], F32)
        k.dma("sp", dtb_bc[:], d["dn_dt_bias"].partition_broadcast(128), "c")
        negA = sb("A_negA", [128, 4], F32)
        k.dma("sp", negA[:], d["dn_a_log"].partition_broadcast(128), "c")
        k.act(negA[:], negA[:], AF.Exp)
        k.ts("dve", negA[:], negA[:], -1.0)
        tm_tok = sb("A_tm_tok", [128, T // 128], F32)
        k.dma("sp", tm_tok[:], d["tm_tok"][:, :], "c")
        epsc = sb("A_epsc", [128, 3], F32)
        k.memset("dve", epsc[:, 0:1], 1e-6)
        k.memset("dve", epsc[:, 1:2], 1e-5)
        k.memset("dve", epsc[:, 2:3], 1.0)
        kmx = sb("A_kmx", [128, 4, nb], F32)

        xblk = sb("A_x", [128, 4, 1024], F32)
        xn = sb("A_xn", [128, 4, 1024], BF16)
        xn32s = [sb("A_xn32_%d" % i, [128, 1024], F32) for i in range(2)]
        stats = sb("A_stats", [128, 4, 2, 6], F32)
        mv = sb("A_mv", [128, 4, 2], F32)
        rstd = sb("A_rstd", [128, 4], F32)
        hTs = [sb("A_hT%d" % i, [128, 8, 512], BF16) for i in range(2)]
        pre = sb("A_pre", [128, 12, 515], BF16)
        identf = sb("A_identf", [128, 128], F32)
        k.dma("sp", identf[:], d["ident"][:, :], "c")
        diagw = sb("A_diagw", [128, 12, 4, 128], BF16)
        for c_ in range(12):
            for j_ in range(4):
                k.ts("dve" if (c_ + j_) % 2 == 0 else "pool", diagw[:, c_, j_, :], identf[:], cw[:, c_, j_:j_ + 1])
        for c_ in range(12):
            k.memset("pool", (pre[:, c_, 0:3], c_), 0.0)
        tmbcs = [sb("A_tmbc%d" % i, [128, 512], F32) for i in range(2)]
        NR = 4
        qa = [sb("A_qa%d" % i, [128, 512], F32) for i in range(NR)]
        sq = [sb("A_sq%d" % i, [128, 512], BF16) for i in range(NR)]
        rinv = [sb("A_rinv%d" % i, [128, 512], F32) for i in range(NR)]
        qn = [sb("A_qn%d" % i, [128, 512], BF16) for i in range(NR)]
        vn = [sb("A_vn%d" % i, [128, 512], BF16) for i in range(3)]
        ktok = sb("A_ktok", [128, 4, 512], BF16)
        vtok = sb("A_vtok", [128, 4, 512], BF16)
        zzb = sb("A_zzb", [128, 4, 512], BF16)
        tmp32 = [sb("A_tmp32%d" % i, [128, 512], F32) for i in range(2)]
        vaug = sb("A_vaug", [128, 4, 4, 130], BF16)
        k.memset("pool", vaug[:], 0.0)
        ba = sb("A_ba", [128, 4, 8], F32)
        bgb = sb("A_bgb", [128, 4, 8], F32)
        spx = sb("A_spx", [128, 4, 4], F32)
        dq = [sb("A_dq%d" % i, [128, 512], BF16) for i in range(2)]
        dk = [sb("A_dk%d" % i, [128, 512], BF16) for i in range(2)]
        ps = g.ps
        psb = [p[:].bitcast(BF16) for p in ps]

        st = {"bank": 0, "cnt": 0}
        dfq = []

        def defer(n, fn):
            dfq.append([n, fn])

        def tick():
            for it in dfq:
                it[0] -= 1
            for it in [it for it in dfq if it[0] <= 0]:
                dfq.remove(it)
                it[1]()

        def flush():
            while dfq:
                it = dfq.pop(0)
                it[1]()

        def nextbank():
            st["bank"] = (st["bank"] + 1) % 6
            return 2 + st["bank"]

        def proj_f(hT, col0, M):
            b_ = nextbank()
            for kk in range(8):
                k.mm(ps[b_][0:M, :], (Wb[:, kk, col0:col0 + M], kk), hT[:, kk, :], start=(kk == 0), stop=(kk == 7))
            tick()
            return ps[b_]

        def proj_t(hT, t, col0, N):
            b_ = nextbank()
            for kk in range(8):
                k.mm(ps[b_][:, 0:N], hT[:, kk, t * 128:(t + 1) * 128], (Wb[:, kk, col0:col0 + N], kk), start=(kk == 0), stop=(kk == 7))
            tick()
            return ps[b_]

        def ln_stats(b):
            for t in range(4):
                for hh in range(2):
                    k.op("dve", lambda t=t, hh=hh: nc.vector.bn_stats(out=stats[:, t, hh, :], in_=xblk[:, t, hh * 512:(hh + 1) * 512]),
                         [stats[:]], [xblk[:]])
                k.op("dve", lambda t=t: nc.vector.bn_aggr(out=mv[:, t, :], in_=stats[:, t, :, :].rearrange("p a b -> p (a b)")),
                     [mv[:]], [stats[:]])
            k.act(rstd[:], mv[:, :, 1], AF.Sqrt, bias=epsc[:, 1:2])
            k.op("dve", lambda: nc.vector.reciprocal(out=rstd[:], in_=rstd[:]), [rstd[:]], [rstd[:]])

        def ln_apply(b, t):
            own = b >= nbc
            o0 = b * 512 - Tc
            xn32 = xn32s[t % 2]
            if own:
                k.ts("dve", xn32[:], xblk[:, t, :], mv[:, t, 0:1], rstd[:, t:t + 1], op0=ALU.subtract, op1=ALU.mult)
                k.copy("act", xn[:, t, :], xn32[:])
                k.tt("pool", xn32[:], xn32[:], eg_bc[:], ALU.mult)
                k.tt("pool", xn32[:], xn32[:], eb_bc[:], ALU.add)
                k.dma("pool", d["hres"][o0 + t * 128:o0 + (t + 1) * 128, :], xn32[:], "hres")
            else:
                k.ts("dve", xn[:, t, :], xblk[:, t, :], mv[:, t, 0:1], rstd[:, t:t + 1], op0=ALU.subtract, op1=ALU.mult)
            if t == 3 and b + 1 < nb:
                k.dma("sp", xblk[:], d["xin"][(b + 1) * 512:(b + 2) * 512, :].rearrange("(t p) f -> p t f", p=128), "x")

        def layer_norm(b):
            ln_stats(b)
            for t in range(4):
                ln_apply(b, t)

        def transposes(b):
            hT = hTs[b % 2]
            for kk in range(8):
                pb = psb[kk % 2]
                for t in range(4):
                    k.tr(pb[:, t * 128:(t + 1) * 128], xn[:, t, kk * 128:(kk + 1) * 128], ident[:])
                k.act(hT[:, kk, :], pb[:, 0:512], AF.Identity, bias=ebc[:, kk:kk + 1], scale=egc[:, kk:kk + 1])
            if b >= nbc:
                o0 = b * 512 - Tc
                k.dma("pool", d["hT_own"][:, :, o0:o0 + 512].rearrange("kk p t -> p kk t"), hT[:], "hT")

        k.dma("sp", xblk[:], d["xin"][0:512, :].rearrange("(t p) f -> p t f", p=128), "x")
        k.dma("sp", tmbcs[0][:], d["tmask"][0:512].partition_broadcast(128), "tm")
        layer_norm(0)
        transposes(0)
        for b in range(nb):
            own = b >= nbc
            t0 = b * 512
            o0 = t0 - Tc
            hT = hTs[b % 2]
            tmbc = tmbcs[b % 2]
            if b + 1 < nb:
                k.dma("sp", tmbcs[(b + 1) % 2][:], d["tmask"][t0 + 512:t0 + 1024].partition_broadcast(128), "tm")
            for c in range(12):
                isq = c < 4
                if isq and not (own or b == nbc - 1):
                    continue
                p_ = proj_f(hT, c * 128, 128)
                k.stt((pre[:, c, 3:515], c), p_[:, :], bcol[:, c:c + 1], tmbc[:], ALU.add, ALU.mult)
                if isq and not own:
                    k.copy("pool", (pre[:, c, 0:3], c), (pre[:, c, 512:515], c))
                    continue
                i_ = st["cnt"]
                st["cnt"] += 1
                q_, s_, r_, n_ = qa[i_ % NR], sq[i_ % NR], rinv[i_ % NR], qn[i_ % NR]
                if c >= 8:
                    n_ = vn[i_ % 3]
                h = c % 4

                def stage2(c=c, h=h, n_=n_):
                    dst = ktok if c < 8 else vtok
                    pb = psb[nextbank()]
                    for t in range(4):
                        k.tr(pb[:, t * 128:(t + 1) * 128], n_[:, t * 128:(t + 1) * 128], ident[:])
                    k.copy("act", dst[:, :, h * 128:(h + 1) * 128], pb[:, 0:512].rearrange("p (t d) -> p t d", t=4))

                def stage1b(c=c, h=h, q_=q_, r_=r_, n_=n_, isq=isq, stage2=stage2, o0=o0, t0=t0):
                    k.op("dve", lambda: nc.vector.reciprocal(out=r_[:], in_=r_[:]), [r_[:]], [r_[:]])
                    if isq:
                        k.stt(n_[:], q_[:], 128.0 ** -0.5, r_[:], ALU.mult, ALU.mult)
                        k.dma("pool", d["dn_qT"][h, :, o0:o0 + 512], n_[:], "o0")
                    else:
                        k.tt("dve", n_[:], q_[:], r_[:], ALU.mult)
                        k.dma("pool", d["dn_kT"][h, :, t0:t0 + 512], n_[:], "o1")
                        defer(2, stage2)

                def stage1(c=c, s_=s_, r_=r_, stage1b=stage1b):
                    b2 = nextbank()
                    k.mm(ps[b2][:, :], onesb[:], s_[:])
                    k.act(r_[:], ps[b2][:, :], AF.Sqrt, bias=epsc[:, 0:1])
                    defer(1, stage1b)

                def stage0(c=c, q_=q_, s_=s_, n_=n_, stage1=stage1, stage2=stage2):
                    b3 = nextbank()
                    for j in range(4):
                        k.mm(ps[b3][:, :], diagw[:, c, j, :], (pre[:, c, j:j + 512], c), start=(j == 0), stop=(j == 3))
                    k.copy("pool", (pre[:, c, 0:3], c), (pre[:, c, 512:515], c))
                    if c < 8:
                        k.act(q_[:], ps[b3][:, :], AF.Silu)
                        k.act(s_[:], q_[:], AF.Square)
                        defer(2, stage1)
                    else:
                        k.act(n_[:], ps[b3][:, :], AF.Silu)
                        defer(2, stage2)

                defer(2, stage0)
            if b + 1 < nb:
                ln_stats(b + 1)
            for t in range(4):
                if b + 1 < nb:
                    ln_apply(b + 1, t)
                if own:
                    p_ = proj_t(hT, t, C_Z, 512)
                    k.tt("dve", tmp32[0][:], p_[:, :], zb_bc[:], ALU.add)
                    k.act(tmp32[0][:], tmp32[0][:], AF.Silu)
                    k.tt("pool", zzb[:, t, :], tmp32[0][:], nw_bc[:].rearrange("p h d -> p (h d)"), ALU.mult)
                p_ = proj_t(hT, t, C_B, 8)
                k.tt("dve", ba[:, t, :], p_[:, 0:8], bab_bc[:], ALU.add)
                p_ = proj_t(hT, t, C_DV, 512)
                k.tt("dve", tmp32[1][:], p_[:, :], vb_bc[:], ALU.add)
                tmc = tm_tok[:, b * 4 + t:b * 4 + t + 1]
                k.act(vaug[:, t, :, 0:128], tmp32[1][:].rearrange("p (h d) -> p h d", h=4), AF.Copy, scale=tmc)
                k.copy("pool", vaug[:, t, :, 128:129], tmc.unsqueeze(1).broadcast_to([128, 4, 1]))
            if own:
                k.dma("pool", d["dn_zz"][o0:o0 + 512, :].rearrange("(t p) f -> p t f", p=128), zzb[:], "o4")
            k.dma("pool", d["da_v"][t0:t0 + 512, :, :].rearrange("(t p) h e -> p t h e", p=128), vaug[:], "o5")
            k.act(bgb[:, :, 0:4], ba[:, :, 0:4], AF.Sigmoid)
            k.tt("dve", spx[:], ba[:, :, 4:8], dtb_bc[:].unsqueeze(1).broadcast_to([128, 4, 4]), ALU.add)
            k.act(spx[:], spx[:], AF.Exp)
            k.act(spx[:], spx[:], AF.Ln, bias=epsc[:, 2:3])
            k.tt("dve", bgb[:, :, 4:8], spx[:], negA[:].unsqueeze(1).broadcast_to([128, 4, 4]), ALU.mult)
            k.dma("pool", d["dn_bg"][t0:t0 + 512, 0:8].rearrange("(t p) f -> p t f", p=128), bgb[:], "o6")
            for h in range(4):
                if own:
                    p_ = proj_f(hT, C_DQ + h * 128, 128)
                    q_ = dq[h % 2]
                    k.ts("dve", q_[:], p_[:, :], qcol[:, h:h + 1], 0.125, op0=ALU.add, op1=ALU.mult)
                    k.dma("pool", d["da_qT"][h, :, :, o0:o0 + 512].rearrange("m d t -> (m d) t"), q_[:], "o7")
                p_ = proj_f(hT, C_DK + h * 128, 128)
                k_ = dk[h % 2]
                k.ts("dve", k_[:], p_[:, :], kcol[:, h:h + 1], None, op0=ALU.add)
                k.op("dve", lambda k_=k_, h=h, b=b: nc.vector.tensor_reduce(out=kmx[:, h, b:b + 1], in_=k_[:], axis=AX.X, op=ALU.max,
                                                                          apply_absolute_value=True), [kmx[:]], [k_[:]])
                k.dma("pool", d["da_kT"][h, :, :, t0:t0 + 512].rearrange("m d t -> (m d) t"), k_[:], "o8")
            flush()
            k.dma("pool", d["dn_ktok"][t0:t0 + 512, :].rearrange("(t p) f -> p t f", p=128), ktok[:], "o2")
            k.dma("pool", d["dn_vtok"][t0:t0 + 512, :].rearrange("(t p) f -> p t f", p=128), vtok[:], "o3")
            if b + 1 < nb:
                transposes(b + 1)
        kmxf = sb("A_kmxf", [128, 4], F32)
        k.op("dve", lambda: nc.vector.tensor_reduce(out=kmxf[:], in_=kmx[:], axis=AX.X, op=ALU.max), [kmxf[:]], [kmx[:]])
        for m in range(2):
            k.dma("pool", d["kmaxabs"][:, 0:8].rearrange("d (h m) -> d h m", m=2)[:, :, m], kmxf[m * 64:(m + 1) * 64, :], "o9",
                  allow_slow_non_contiguous=True)
        k.barrier()


def declare_dram(nc, g, debug):
    Tc, To, T = g.Tc, g.To, g.T
    d = {}
    kin = "ExternalInput"
    d["xin"] = dram(nc, "xin", [T, D], F32, kin)
    d["pin"] = dram(nc, "pin", [To, 256], F32, kin)
    d["tm_tok"] = dram(nc, "tm_tok", [128, T // 128], F32, kin)
    d["tmask"] = dram(nc, "tmask", [T], F32, kin)
    for n in INPUT_NAMES:
        d[n] = dram(nc, n, W_SHAPES[n], F32, kin)
    for n, a in make_consts(Tc, To).items():
        d[n] = dram(nc, n, a.shape, F32, kin)
    sk = "ExternalOutput" if debug else "Internal"
    g.scratch = {}

    def scr(name, shape, dt):
        d[name] = dram(nc, name, shape, dt, sk)
        g.scratch[name] = (shape, dt)
    scr("hres", [To, D], F32)
    scr("dn_qT", [4, 128, To], BF16)
    scr("dn_kT", [4, 128, T], BF16)
    scr("dn_ktok", [T, 512], BF16)
    scr("dn_vtok", [T, 512], BF16)
    scr("dn_bg", [T, 16], F32)
    scr("dn_zz", [To, 512], BF16)
    scr("da_qT", [4, 2, 64, To], BF16)
    scr("da_kT", [4, 2, 64, T], BF16)
    scr("da_v", [T, 4, 130], BF16)
    scr("hT_own", [8, 128, To], BF16)
    scr("kmaxabs", [64, 16], F32)
    scr("oT_dn", [4, 128, To], BF16)
    scr("oT_da", [4, 128, To], BF16)
    scr("h1", [To, D], F32)
    scr("h1T", [8, 128, To], BF16)
    scr("comb", [To, 16], F32)
    scr("ffn0", [To, D], F32)
    d["out"] = dram(nc, "out", [To, D], F32, "ExternalOutput")
    return d


def build(Tc, To, debug=False, phases="A"):
    nc = bass.Bass("TRN2", target_bir_lowering=False)
    g = Ctx()
    g.Tc, g.To, g.T = Tc, To, Tc + To
    g.d = declare_dram(nc, g, debug)
    es = contextlib.ExitStack()
    with es:
        k = KB(nc, es)
        g.ps = [es.enter_context(nc.psum_tensor("ps%d" % i, [128, 512], F32)) for i in range(8)]
        if "A" in phases:
            phase_A(k, g)
        if "B" in phases and "C" in phases and "S" not in phases:
            with contextlib.ExitStack() as esb:
                gen = gen_B(k, g, esb, (4, 5, 7))
                state = {"done": False}
                next(gen)

                def stepper():
                    if not state["done"]:
                        try:
                            next(gen)
                        except StopIteration:
                            state["done"] = True
                phase_C(k, g, stepper)
                while not state["done"]:
                    stepper()
                k.barrier()
        else:
            if "B" in phases:
                phase_B(k, g)
            if "C" in phases:
                phase_C(k, g)
        if "D" in phases:
            phase_D(k, g)
        if "E" in phases:
            phase_E(k, g)
        k.barrier()
        print("instructions:", k.ninstr, "sems:", len(k.sems))
    return nc, g


def gen_B(k, g, es, bk):
    nc = k.nc
    d = g.d
    Tc, To, T = g.Tc, g.To, g.T
    nch = T // 128
    ncc = Tc // 128
    ps = g.ps
    psb = [p[:].bitcast(BF16) for p in ps]
    X, Y, Z = [ps[i] for i in bk]
    XB = X[:].bitcast(BF16)
    if True:
        sb = lambda name, shape, dt: k.sb(name, shape, dt, es)
        U = sb("B_U", [128, 128], F32); k.dma("sp", U[:], d["U"][:, :], "c")
        negU = sb("B_negU", [128, 128], F32); k.dma("sp", negU[:], d["negU"][:, :], "c")
        ones = sb("B_ones", [128, 128], F32); k.dma("sp", ones[:], d["ones"][:, :], "c")
        identb = sb("B_identb", [128, 128], BF16); k.dma("pool", identb[:], d["ident"][:, :], "c")
        MLs = sb("B_MLs", [128, 512], F32); k.dma("sp", MLs[:], d["MLs"][:, :], "c")
        MUi = sb("B_MUi", [128, 512], F32); k.dma("sp", MUi[:], d["MUi"][:, :], "c")
        SU01 = sb("B_SU01", [128, 512], F32); k.dma("sp", SU01[:], d["SU01"][:, :], "c")
        epsc = sb("B_epsc", [128, 1], F32); k.memset("dve", epsc[:], 1e-6)
        S32 = sb("B_S32", [128, 4, 128], F32); k.memset("dve", S32[:], 0.0)
        Sb = sb("B_Sb", [128, 4, 128], BF16); k.memset("pool", Sb[:], 0.0)
        KT = [sb("B_KT%d" % i, [128, 4, 128], BF16) for i in range(2)]
        QT = [sb("B_QT%d" % i, [128, 4, 128], BF16) for i in range(2)]
        ktok = [sb("B_ktok%d" % i, [128, 4, 128], BF16) for i in range(2)]
        vtok = [sb("B_vtok%d" % i, [128, 4, 128], BF16) for i in range(2)]
        zz = [sb("B_zz%d" % i, [128, 4, 128], BF16) for i in range(2)]
        bg = [sb("B_bg%d" % i, [128, 8], F32) for i in range(2)]
        gB = sb("B_gB", [128, 4, 128], F32)
        Gc = sb("B_Gc", [128, 4], F32)
        cb = sb("B_cb", [128, 4], F32)
        cg = sb("B_cg", [128, 4], F32)
        egl = sb("B_egl", [128, 4], F32)
        nbeta = sb("B_nbeta", [128, 4], F32)
        argL = sb("B_argL", [128, 4, 128], F32)
        argU = sb("B_argU", [128, 4, 128], F32)
        Dst = sb("B_Dst", [128, 4, 128], F32)
        DTi = sb("B_DTi", [128, 4, 128], F32)
        kbg = sb("B_kbg", [128, 4, 128], BF16)
        kg = sb("B_kg", [128, 4, 128], BF16)
        vb = sb("B_vb", [128, 4, 128], BF16)
        P = [sb("B_P%d" % i, [128, 4, 128], BF16) for i in range(2)]
        PT = [sb("B_PT%d" % i, [128, 4, 128], BF16) for i in range(2)]
        AT = [sb("B_AT%d" % i, [128, 4, 128], BF16) for i in range(2)]
        u = sb("B_u", [128, 4, 128], F32)
        wT = sb("B_wT", [128, 4, 128], BF16)
        aT = sb("B_aT", [128, 4, 128], BF16)
        eGrow = sb("B_eGrow", [128, 4, 128], F32)
        QgT = sb("B_QgT", [128, 4, 128], BF16)
        vnew = sb("B_vnew", [128, 4, 128], BF16)
        ssq = sb("B_ssq", [128, 4], F32)
        junk = sb("B_junk", [128, 128], F32)
        og = sb("B_og", [128, 4, 128], BF16)
        oT = [sb("B_oT%d" % i, [128, 4, 128], BF16) for i in range(2)]

        def bc4(ap):
            return ap.unsqueeze(2).broadcast_to([128, 4, 128])

        def load(c):
            s = c % 2
            t0 = c * 128
            k.dma("sp", KT[s][:], d["dn_kT"][:, :, t0:t0 + 128].rearrange("h d t -> d h t"), "bl%d" % s)
            k.dma("sp", ktok[s][:].rearrange("p h d -> p (h d)"), d["dn_ktok"][t0:t0 + 128, :], "bl%d" % s)
            k.dma("sp", vtok[s][:].rearrange("p h d -> p (h d)"), d["dn_vtok"][t0:t0 + 128, :], "bl%d" % s)
            k.dma("sp", bg[s][:], d["dn_bg"][t0:t0 + 128, 0:8], "bl%d" % s)
            if c >= ncc:
                o0 = t0 - Tc
                k.dma("sp", QT[s][:], d["dn_qT"][:, :, o0:o0 + 128].rearrange("h d t -> d h t"), "bl%d" % s)
                k.dma("sp", zz[s][:].rearrange("p h d -> p (h d)"), d["dn_zz"][o0:o0 + 128, :], "bl%d" % s)

        yield
        load(0)
        for c in range(nch):
            s = c % 2
            own = c >= ncc
            o0 = c * 128 - Tc
            if c + 1 < nch:
                load(c + 1)
            beta = bg[s][:, 0:4]
            gg = bg[s][:, 4:8]
            k.copy("dve", gB[:], bc4(gg))
            k.mm(X[:, 0:4], U[:], gg)
            k.mm(X[:, 8:12], ones[:], gg)
            for h in range(4):
                k.mm(Y[:, h * 128:(h + 1) * 128], U[:], gB[:, h, :], start=True, stop=False)
                k.mm(Y[:, h * 128:(h + 1) * 128], gB[:, h, :], negU[:], start=False, stop=True)
            if own:
                for h in range(4):
                    k.mm(Z[:, h * 128:(h + 1) * 128], gB[:, h, :], U[:])
            yield
            k.copy("dve", Gc[:], X[:, 0:4])
            k.tt("dve", cg[:], X[:, 8:12], Gc[:], ALU.subtract)
            k.ts("dve", nbeta[:], beta, -1.0)
            k.tt("dve", argL[:].rearrange("p h d -> p (h d)"), Y[:, :], MLs[:], ALU.add)
            if own:
                k.stt(argU[:].rearrange("p h d -> p (h d)"), Y[:, :], -1.0, MUi[:], ALU.mult, ALU.add)
            yield
            k.act(cb[:], Gc[:], AF.Exp)
            k.act(cg[:], cg[:], AF.Exp)
            k.act(egl[:], X[:, 8:12], AF.Exp)
            k.act(Dst[:], argL[:], AF.Exp)
            if own:
                k.act(DTi[:], argU[:], AF.Exp)
                k.act(eGrow[:].rearrange("p h d -> p (h d)"), Z[:, :], AF.Exp)
            yield
            k.tt("dve", cb[:], cb[:], beta, ALU.mult)
            if own:
                k.tt("pool", QgT[:], QT[s][:], eGrow[:], ALU.mult)
            k.tt("pool", kbg[:], ktok[s][:], bc4(cb[:, :]), ALU.mult)
            k.tt("pool", kg[:], ktok[s][:], bc4(cg[:, :]), ALU.mult)
            k.tt("pool", vb[:], vtok[s][:], bc4(beta), ALU.mult)
            for h in range(4):
                k.mm(X[:, h * 128:(h + 1) * 128], KT[s][:, h, :], KT[s][:, h, :])
            yield
            for h in range(4):
                k.stt(P[0][:, h, :], X[:, h * 128:(h + 1) * 128], nbeta[:, h:h + 1], Dst[:, h, :], ALU.mult, ALU.mult)
            yield
            for h in range(4):
                k.mm(Y[:, h * 128:(h + 1) * 128], P[0][:, h, :], identb[:])
            yield
            k.copy("dve", PT[0][:].rearrange("p h d -> p (h d)"), Y[:, :])
            k.tt("pool", AT[0][:], PT[0][:], identb[:, :].unsqueeze(1).broadcast_to([128, 4, 128]), ALU.add)
            yield
            ci = 0
            ai = 0
            for it in range(1, 7):
                ni = 1 - ci
                for h in range(4):
                    k.mm(X[:, h * 128:(h + 1) * 128], PT[ci][:, h, :], P[ci][:, h, :])
                if it < 6:
                    for h in range(4):
                        k.mm(Y[:, h * 128:(h + 1) * 128], P[ci][:, h, :], PT[ci][:, h, :])
                yield
                k.copy("dve", P[ni][:].rearrange("p h d -> p (h d)"), X[:, :])
                if it < 6:
                    k.copy("dve", PT[ni][:].rearrange("p h d -> p (h d)"), Y[:, :])
                yield
                yield
                for h in range(4):
                    k.mm(Z[:, h * 128:(h + 1) * 128], P[ni][:, h, :], AT[ai][:, h, :])
                yield
                k.tt("dve", AT[1 - ai][:].rearrange("p h d -> p (h d)"), Z[:, :], AT[ai][:].rearrange("p h d -> p (h d)"), ALU.add)
                ai = 1 - ai
                ci = ni
            TT = AT[ai]
            yield
            for h in range(4):
                k.mm(X[:, h * 128:(h + 1) * 128], TT[:, h, :], vb[:, h, :])
            for h in range(4):
                k.mm(Y[:, h * 128:(h + 1) * 128], kbg[:, h, :], TT[:, h, :])
            if own:
                for h in range(4):
                    k.mm(Z[:, h * 128:(h + 1) * 128], KT[s][:, h, :], QT[s][:, h, :])
            yield
            k.copy("dve", u[:].rearrange("p h d -> p (h d)"), X[:, :])
            k.copy("dve", wT[:].rearrange("p h d -> p (h d)"), Y[:, :])
            if own:
                k.tt("dve", aT[:].rearrange("p h d -> p (h d)"), Z[:, :], DTi[:].rearrange("p h d -> p (h d)"), ALU.mult)
            yield
            yield
            for h in range(4):
                k.mm(X[:, h * 128:(h + 1) * 128], wT[:, h, :], Sb[:, h, :])
            yield
            k.tt("dve", vnew[:].rearrange("p h d -> p (h d)"), u[:].rearrange("p h d -> p (h d)"), X[:, :], ALU.subtract)
            yield
            yield
            if own:
                for h in range(4):
                    k.mm(Y[:, h * 128:(h + 1) * 128], QgT[:, h, :], Sb[:, h, :], start=True, stop=False)
                    k.mm(Y[:, h * 128:(h + 1) * 128], aT[:, h, :], vnew[:, h, :], start=False, stop=True)
            for h in range(4):
                k.mm(Z[:, h * 128:(h + 1) * 128], kg[:, h, :], vnew[:, h, :])
            yield
            k.tt("dve", S32[:], S32[:], bc4(egl[:, :]), ALU.mult)
            k.tt("dve", S32[:].rearrange("p h d -> p (h d)"), S32[:].rearrange("p h d -> p (h d)"), Z[:, :], ALU.add)
            k.copy("dve", Sb[:], S32[:])
            if own:
                yield
                for h in range(4):
                    k.act(junk[:], Y[:, h * 128:(h + 1) * 128], AF.Square, accum_out=ssq[:, h:h + 1])
                k.act(ssq[:], ssq[:], AF.Sqrt, bias=epsc[:, 0:1], scale=1.0 / 128.0)
                yield
                k.op("dve", lambda: nc.vector.reciprocal(out=ssq[:], in_=ssq[:]), [ssq[:]], [ssq[:]])
                for h in range(4):
                    k.stt(og[:, h, :], Y[:, h * 128:(h + 1) * 128], ssq[:, h:h + 1], zz[s][:, h, :], ALU.mult, ALU.mult)
                yield
                for h in range(4):
                    k.tr(XB[:, h * 128:(h + 1) * 128], og[:, h, :], identb[:])
                yield
                o_ = oT[c % 2]
                k.copy("dve", o_[:].rearrange("p h d -> p (h d)"), XB[:, 0:512])
                k.dma("pool", d["oT_dn"][:, :, o0:o0 + 128].rearrange("h d t -> d h t"), o_[:], "bo%d" % (c % 2))


def phase_B(k, g):
    with contextlib.ExitStack() as es:
        for _ in gen_B(k, g, es, (4, 5, 7)):
            pass
        k.barrier()


def phase_C(k, g, stepper=None):
    nc = k.nc
    d = g.d
    Tc, To, T = g.Tc, g.To, g.T
    nkt = T // 128
    nqb = To // 512
    ps = g.ps
    psb = [p[:].bitcast(BF16) for p in ps]
    lam_init = 0.8 - 0.6
    with contextlib.ExitStack() as es:
        sb = lambda name, shape, dt: k.sb(name, shape, dt, es)
        identb = sb("C_identb", [128, 128], BF16); k.dma("pool", identb[:], d["ident"][:, :], "c")
        catrib = sb("C_catrib", [128, 128], BF16); k.dma("pool", catrib[:], d["catri"][:, :], "c")
        epsc = sb("C_epsc", [128, 1], F32); k.memset("dve", epsc[:], 1e-6)
        lv = sb("C_lv", [128, 4, 64], F32)
        for i, n in enumerate(["da_lq1", "da_lk1", "da_lq2", "da_lk2"]):
            k.dma("sp", lv[:, i, :], d[n].partition_broadcast(128), "c")
        lp = sb("C_lp", [128, 2, 64], F32)
        k.tt("dve", lp[:, 0, :], lv[:, 0, :], lv[:, 1, :], ALU.mult)
        k.tt("dve", lp[:, 1, :], lv[:, 2, :], lv[:, 3, :], ALU.mult)
        ls = sb("C_ls", [128, 2], F32)
        k.op("dve", lambda: nc.vector.tensor_reduce(out=ls[:], in_=lp[:], axis=AX.X, op=ALU.add), [ls[:]], [lp[:]])
        k.act(ls[:], ls[:], AF.Exp)
        neglam = sb("C_neglam", [128, 1], F32)
        k.tt("dve", neglam[:], ls[:, 1:2], ls[:, 0:1], ALU.subtract)
        k.ts("dve", neglam[:], neglam[:], -lam_init, op0=ALU.add)
        wbc = sb("C_wbc", [128, 128], F32)
        k.dma("sp", wbc[:], d["da_subln_w"].partition_broadcast(128), "c")
        k.ts("dve", wbc[:], wbc[:], 1.0 - lam_init)
        kmx = sb("C_kmx", [64, 8], F32); k.dma("sp", kmx[:], d["kmaxabs"][:, 0:8], "c")
        kmxL = sb("C_kmxL", [64, 8, 65], BF16)
        k.memset("dve", kmxL[:], 0.0)
        k.copy("dve", kmxL[:, :, 64:65], kmx[:, :].unsqueeze(2))

        Va = [sb("C_Va%d" % i, [128, nkt, 130], BF16) for i in range(2)]
        kTa = [[sb("C_kTa%d_%d" % (i, m), [69, T], BF16) for m in range(2)] for i in range(2)]
        qTa = [[sb("C_qTa%d_%d" % (i, m), [69, To], BF16) for m in range(2)] for i in range(2)]
        absqs = [sb("C_absq%d" % i, [64, 512], BF16) for i in range(2)]
        PT = [sb("C_PT%d" % i, [128, 512], BF16) for i in range(4)]
        o1 = sb("C_o1", [128, 4, 128], F32)
        rden = sb("C_rden", [128, 4], F32)
        ssqs = [sb("C_ssq%d" % i, [128, 4], F32) for i in range(2)]
        o1s = [sb("C_o1s%d" % i, [128, 4, 128], F32) for i in range(2)]
        junk = sb("C_junk", [128, 128], F32)
        ons = [sb("C_on%d" % i, [128, 4, 128], BF16) for i in range(2)]
        oT = [sb("C_oT%d" % i, [128, 512], BF16) for i in range(2)]

        def load(h):
            s = h % 2
            k.dma("sp", Va[s][:], d["da_v"][:, h, :].rearrange("(kt p) e -> p kt e", p=128), "cl%d" % s)
            for m in range(2):
                k.dma("sp", kTa[s][m][0:64, :], d["da_kT"][h, m, :, :], "cl%d" % s)
                k.dma("pool", kTa[s][m][64:69, :], d["augk"][h, :, :], "cl%d" % s)
                k.dma("sp", qTa[s][m][0:64, :], d["da_qT"][h, m, :, :], "cl%d" % s)
                k.dma("pool", qTa[s][m][65:69, :], d["augq"][h, :, :], "cl%d" % s)

        accs = sb("C_accs", [128, 4, 129], F32)

        def accv(j):
            return ps[2 + j // 2][:, (j % 2) * 256:(j % 2) * 256 + 129]

        load(0)
        st = {"pti": 0, "si": 0, "pend": [], "epi": 0, "ab": 0}
        for h in range(4):
            s = h % 2
            if h + 1 < 4:
                load(h + 1)
            def bound(hh, m, qb):
                s2 = hh % 2
                hm = hh * 2 + m
                a_ = absqs[st["ab"] % 2]
                st["ab"] += 1
                k.act(a_[:], qTa[s2][m][0:64, qb * 512:(qb + 1) * 512], AF.Abs)
                k.mm(ps[6][0:65, :], kmxL[:, hm, :], a_[:])
                k.act(qTa[s2][m][64:65, qb * 512:(qb + 1) * 512], ps[6][64:65, :], AF.Copy, scale=-1.0)
                return lambda: None
            if h == 0:
                for m in range(2):
                    for qb in range(nqb):
                        bound(0, m, qb)()
            for qb in range(nqb):
                Q0 = Tc // 128 + 4 * qb
                for m in range(2):
                    def emit_S(kt):
                        c = max(kt - Q0, 0)
                        lo = 128 * c
                        pS = ps[(0, 1, 6)[st["si"] % 3]]
                        st["si"] += 1
                        dg = kt >= Q0
                        k.mm(pS[:, lo:512], kTa[s][m][0:69, kt * 128:(kt + 1) * 128], qTa[s][m][0:69, qb * 512 + lo:(qb + 1) * 512],
                             start=True, stop=not dg)
                        if dg:
                            k.mm(pS[:, lo:lo + 128], identb[:], catrib[:], start=False, stop=True)
                        P_ = PT[st["pti"] % 4]
                        st["pti"] += 1
                        k.act(P_[:, lo:512], pS[:, lo:512], AF.Exp)
                        return P_, c
                    nkt = Q0 + 4
                    ahead = [emit_S(0)]
                    if nkt > 1:
                        ahead.append(emit_S(1))
                    for kt in range(nkt):
                        P_, c = ahead.pop(0)
                        if kt + 2 < nkt:
                            ahead.append(emit_S(kt + 2))
                        if stepper is not None:
                            stepper()
                        for j in range(c, 4):
                            k.mm(accv(j), P_[:, j * 128:(j + 1) * 128], Va[s][:, kt, 0:129],
                                 start=(kt == 0 and j % 2 == 0), stop=(kt == Q0 + j), skip_group_check=True)
                        if st["pend"] and kt in (1, 3, 5):
                            st["pend"].pop(0)()
                        if kt == min(6, Q0 + 3) - 1 and h + 1 < 4:
                            st["bnd"] = bound(h + 1, m, qb)
                        elif st.get("bnd") is not None:
                            st["bnd"]()
                            st["bnd"] = None
                    for j in range(4):
                        k.copy("dve", accs[:, j, :], accv(j))
                    k.op("dve", lambda: nc.vector.reciprocal(out=rden[:], in_=accs[:, :, 128]), [rden[:]], [accs[:]])
                    if m == 0:
                        for j in range(4):
                            k.ts("dve", o1[:, j, :], accs[:, j, 0:128], rden[:, j:j + 1])
                    else:
                        k.ts("dve", rden[:], rden[:], neglam[:, 0:1])
                        o1_ = o1s[st["epi"] % 2]
                        for j in range(4):
                            k.stt(o1_[:, j, :], accs[:, j, 0:128], rden[:, j:j + 1], o1[:, j, :], ALU.mult, ALU.add)
                on_ = ons[st["epi"] % 2]
                ssq_ = ssqs[st["epi"] % 2]
                st["epi"] += 1

                def e_act(o1_=o1_, ssq_=ssq_):
                    for j in range(4):
                        k.act(junk[:], o1_[:, j, :], AF.Square, accum_out=ssq_[:, j:j + 1])
                    k.act(ssq_[:], ssq_[:], AF.Sqrt, bias=epsc[:, 0:1], scale=1.0 / 128.0)

                def e_dve(o1_=o1_, ssq_=ssq_, on_=on_):
                    k.op("dve", lambda: nc.vector.reciprocal(out=ssq_[:], in_=ssq_[:]), [ssq_[:]], [ssq_[:]])
                    for j in range(4):
                        k.stt(on_[:, j, :], o1_[:, j, :], ssq_[:, j:j + 1], wbc[:], ALU.mult, ALU.mult)

                def e_pe(h=h, qb=qb, on_=on_):
                    for j in range(4):
                        k.tr(psb[6][:, j * 128:(j + 1) * 128], on_[:, j, :], identb[:])
                    o_ = oT[qb % 2]
                    k.copy("dve", o_[:], psb[6][:, 0:512])
                    k.dma("pool", d["oT_da"][h, :, qb * 512:(qb + 1) * 512], o_[:], "co%d" % (qb % 2))
                while st["pend"]:
                    st["pend"].pop(0)()
                st["pend"] = [e_act, e_dve, e_pe]
        while st["pend"]:
            st["pend"].pop(0)()
        k.barrier()


ALPHA = 2.0 ** 0.25


def layer_norm_tile(k, nc, r, gbc, bbc, out, stats, mv, rstd, epsc5):
    for hh in range(2):
        k.op("dve", lambda hh=hh: nc.vector.bn_stats(out=stats[:, hh, :], in_=r[:, hh * 512:(hh + 1) * 512]), [stats[:]], [r[:]])
    k.op("dve", lambda: nc.vector.bn_aggr(out=mv[:], in_=stats[:].rearrange("p a b -> p (a b)")), [mv[:]], [stats[:]])
    k.act(rstd[:], mv[:, 1:2], AF.Sqrt, bias=epsc5)
    k.op("dve", lambda: nc.vector.reciprocal(out=rstd[:], in_=rstd[:]), [rstd[:]], [rstd[:]])
    k.ts("dve", out[:], r[:], mv[:, 0:1], rstd[:, 0:1], op0=ALU.subtract, op1=ALU.mult)
    k.tt("pool", out[:], out[:], gbc[:], ALU.mult)
    k.tt("pool", out[:], out[:], bbc[:], ALU.add)


def phase_D(k, g):
    nc = k.nc
    d = g.d
    To = g.To
    nqb = To // 512
    ps = g.ps
    psb = [p[:].bitcast(BF16) for p in ps]
    with contextlib.ExitStack() as es:
        sb = lambda name, shape, dt: k.sb(name, shape, dt, es)
        identb = sb("D_identb", [128, 128], BF16); k.dma("pool", identb[:], d["ident"][:, :], "c")
        wdn = sb("D_wdn", [128, 4, 1024], BF16)
        k.dma("pool", wdn[:], d["w_dn_o"].rearrange("(kk p) n -> p kk n", p=128), "w0")
        wda = sb("D_wda", [128, 4, 1024], BF16)
        k.dma("pool", wda[:], d["w_da_o"].rearrange("(kk p) n -> p kk n", p=128), "w1")
        wout = sb("D_wout", [128, 8, 1024], BF16)
        k.dma("pool", wout[:], d["w_out"].rearrange("(kk p) n -> p kk n", p=128), "w0")
        wr = sb("D_wr", [128, 8, 20], BF16)
        k.dma("pool", wr[:, :, 0:4], d["w_router_group"].rearrange("(kk p) n -> p kk n", p=128), "w1")
        k.dma("pool", wr[:, :, 4:20], d["w_router_expert"].rearrange("(kk p) n -> p kk n", p=128), "w1")
        rb = sb("D_rb", [128, 20], F32)
        k.dma("sp", rb[:, 0:4], d["b_router_group"].partition_broadcast(128), "c")
        k.dma("sp", rb[:, 4:20], d["b_router_expert"].partition_broadcast(128), "c")
        gbc = sb("D_gbc", [128, 1024], F32); k.dma("sp", gbc[:], d["ln1_g"].partition_broadcast(128), "c")
        bbc = sb("D_bbc", [128, 1024], F32); k.dma("sp", bbc[:], d["ln1_b"].partition_broadcast(128), "c")
        epsc = sb("D_epsc", [128, 1], F32); k.memset("dve", epsc[:], 1e-5)
        oTdn = [sb("D_oTdn%d" % i, [128, 4, 512], BF16) for i in range(2)]
        oTda = [sb("D_oTda%d" % i, [128, 4, 512], BF16) for i in range(2)]
        hTo = [sb("D_hTo%d" % i, [128, 8, 512], BF16) for i in range(2)]
        Wg = sb("D_Wg", [128, 8, 2048], BF16)
        for kk in range(8):
            k.dma("pool", (Wg[:, kk, :], kk), d["w_in"][kk * 128:(kk + 1) * 128, C_G:C_G + 2048], "w%d" % (kk % 2))
        gcolb = sb("D_gcolb", [128, 16], F32)
        colload(k, "sp", gcolb[:, :], d["b_in"][C_G:C_G + 2048], 16)
        g1 = [sb("D_g1_%d" % i, [128, 512], BF16) for i in range(2)]
        g2 = [sb("D_g2_%d" % i, [128, 512], BF16) for i in range(2)]
        hres = [sb("D_hres%d" % i, [128, 4, 1024], F32) for i in range(2)]
        t1 = [sb("D_t1_%d" % i, [128, 512], F32) for i in range(2)]
        t2 = [sb("D_t2_%d" % i, [128, 512], F32) for i in range(2)]
        mergeds = [sb("D_merged%d" % i, [128, 8, 512], BF16) for i in range(2)]
        r = [sb("D_r%d" % i, [128, 1024], F32) for i in range(2)]
        h1 = [sb("D_h1_%d" % i, [128, 1024], F32) for i in range(2)]
        h1bs = [sb("D_h1b%d" % i, [128, 1024], BF16) for i in range(2)]
        h1T = [sb("D_h1T%d" % i, [128, 8, 128], BF16) for i in range(2)]
        stats = sb("D_stats", [128, 2, 6], F32)
        mv = sb("D_mv", [128, 2], F32)
        rstd = sb("D_rstd", [128, 1], F32)
        lg = sb("D_lg", [128, 20], F32)
        sm = sb("D_sm", [128, 16], F32)
        ge = sb("D_ge", [128, 4], F32)
        ohg = sb("D_ohg", [128, 4], F32)
        tmp16 = sb("D_tmp16", [128, 4, 4], F32)
        ig = sb("D_ig", [128, 4], F32)
        ig2 = sb("D_ig2", [128, 4], F32)
        mk1 = sb("D_mk1", [128, 4], F32)
        mk2 = sb("D_mk2", [128, 4], F32)
        cwi = sb("D_cwi", [128, 4], F32)
        comb = [sb("D_comb%d" % i, [128, 4, 4], F32) for i in range(2)]

        def load(qb):
            s = qb % 2
            q0 = qb * 512
            k.dma("sp", oTdn[s][:], d["oT_dn"][:, :, q0:q0 + 512].rearrange("h d t -> d h t"), "dl%d" % s)
            k.dma("sp", oTda[s][:], d["oT_da"][:, :, q0:q0 + 512].rearrange("h d t -> d h t"), "dl%d" % s)
            k.dma("sp", hTo[s][:], d["hT_own"][:, :, q0:q0 + 512].rearrange("kk p t -> p kk t"), "dl%d" % s)
            k.dma("sp", hres[s][:], d["hres"][q0:q0 + 512, :].rearrange("(t p) f -> p t f", p=128), "dl%d" % s)

        load(0)
        ti = 0
        for qb in range(nqb):
            s = qb % 2
            q0 = qb * 512
            if qb + 1 < nqb:
                load(qb + 1)
            def cpart(qb_, cs):
                s_ = qb_ % 2
                mg = mergeds[qb_ % 2]
                for c in cs:
                    pg1, pg2, pa, pb_ = ps[0], ps[1], ps[2], ps[3]
                    for kk in range(8):
                        k.mm(pg1[:, :], (Wg[:, kk, c * 128:(c + 1) * 128], kk), hTo[s_][:, kk, :], start=(kk == 0), stop=(kk == 7))
                    k.act(g1[c % 2][:], pg1[:, :], AF.Sigmoid, bias=gcolb[:, c:c + 1])
                    for kk in range(8):
                        k.mm(pg2[:, :], (Wg[:, kk, 1024 + c * 128:1024 + (c + 1) * 128], kk), hTo[s_][:, kk, :], start=(kk == 0), stop=(kk == 7))
                    k.act(g2[c % 2][:], pg2[:, :], AF.Sigmoid, bias=gcolb[:, 8 + c:9 + c])
                    for kk in range(4):
                        k.mm(pa[:, :], wdn[:, kk, c * 128:(c + 1) * 128], oTdn[s_][:, kk, :], start=(kk == 0), stop=(kk == 3))
                    for kk in range(4):
                        k.mm(pb_[:, :], wda[:, kk, c * 128:(c + 1) * 128], oTda[s_][:, kk, :], start=(kk == 0), stop=(kk == 3))
                    a_, b_ = t1[c % 2], t2[c % 2]
                    k.tt("dve", a_[:], pa[:, :], g1[c % 2][:], ALU.mult)
                    k.tt("dve", b_[:], pb_[:, :], g2[c % 2][:], ALU.mult)
                    k.tt("pool", mg[:, c, :], a_[:], b_[:], ALU.add)

            merged = mergeds[qb % 2]
            if qb == 0:
                cpart(0, range(8))
            def bufs(i):
                return r[i % 2], h1[i % 2], h1T[i % 2], comb[i % 2], h1bs[i % 2]

            def S1(t, i):
                r_, h_, hT_, cb_, h1b = bufs(i)
                for n in range(2):
                    pm = ps[4 + n]
                    for kk in range(8):
                        k.mm(pm[:, :], merged[:, kk, t * 128:(t + 1) * 128], wout[:, kk, n * 512:(n + 1) * 512], start=(kk == 0), stop=(kk == 7))
                    k.stt(r_[:, n * 512:(n + 1) * 512], hres[s][:, t, n * 512:(n + 1) * 512], ALPHA, pm[:, :], ALU.mult, ALU.add)
                layer_norm_tile(k, nc, r_, gbc, bbc, h_, stats, mv, rstd, epsc[:, 0:1])
                k.dma("pool", d["h1"][q0 + t * 128:q0 + (t + 1) * 128, :], h_[:], "do0")
                k.copy("act", h1b[:], h_[:])

            def S2(t, i):
                r_, h_, hT_, cb_, h1b = bufs(i)
                for kk in range(8):
                    k.tr(psb[6][:, kk * 128:(kk + 1) * 128], h1b[:, kk * 128:(kk + 1) * 128], identb[:])
                k.copy("act", hT_[:].rearrange("p a b -> p (a b)"), psb[6][:, 0:1024])
                k.dma("pool", d["h1T"][:, :, q0 + t * 128:q0 + (t + 1) * 128].rearrange("kk p t -> p kk t"), hT_[:], "do1")
                for kk in range(8):
                    k.mm(ps[7][:, 0:20], hT_[:, kk, :], wr[:, kk, :], start=(kk == 0), stop=(kk == 7))
                k.tt("dve", lg[:], ps[7][:, 0:20], rb[:], ALU.add)
                k.op("dve", lambda: nc.vector.tensor_reduce(out=sm[:, 0:1], in_=lg[:, 0:4], axis=AX.X, op=ALU.max), [sm[:]], [lg[:]])
                k.ts("dve", sm[:, 1:2], sm[:, 0:1], -1.0)
                k.act(ge[:], lg[:, 0:4], AF.Exp, bias=sm[:, 1:2])
                k.op("dve", lambda: nc.vector.tensor_reduce(out=sm[:, 2:3], in_=ge[:], axis=AX.X, op=ALU.add), [sm[:]], [ge[:]])
                k.op("dve", lambda: nc.vector.reciprocal(out=sm[:, 3:4], in_=sm[:, 2:3]), [sm[:]], [sm[:]])
                k.ts("dve", ohg[:], lg[:, 0:4], sm[:, 0:1], op0=ALU.is_equal)
                k.tt("dve", tmp16[:], lg[:, 4:20].rearrange("p (g e) -> p g e", g=4), ohg[:, :].unsqueeze(2).broadcast_to([128, 4, 4]), ALU.mult)
                k.op("dve", lambda: nc.vector.tensor_reduce(out=ig[:], in_=tmp16[:].rearrange("p g e -> p e g"), axis=AX.X, op=ALU.add), [ig[:]], [tmp16[:]])
                k.op("dve", lambda: nc.vector.tensor_reduce(out=sm[:, 4:5], in_=ig[:], axis=AX.X, op=ALU.max), [sm[:]], [ig[:]])
                k.ts("dve", mk1[:], ig[:], sm[:, 4:5], op0=ALU.is_equal)
                k.stt(ig2[:], mk1[:], -1e9, ig[:], ALU.mult, ALU.add)
                k.op("dve", lambda: nc.vector.tensor_reduce(out=sm[:, 5:6], in_=ig2[:], axis=AX.X, op=ALU.max), [sm[:]], [ig2[:]])
                k.ts("dve", mk2[:], ig2[:], sm[:, 5:6], op0=ALU.is_equal)
                k.tt("dve", sm[:, 6:7], sm[:, 5:6], sm[:, 4:5], ALU.subtract)
                k.act(sm[:, 7:8], sm[:, 6:7], AF.Exp)
                k.ts("dve", sm[:, 8:9], sm[:, 7:8], 1.0, op0=ALU.add)
                k.op("dve", lambda: nc.vector.reciprocal(out=sm[:, 9:10], in_=sm[:, 8:9]), [sm[:]], [sm[:]])
                k.tt("dve", sm[:, 10:11], sm[:, 9:10], sm[:, 3:4], ALU.mult)
                k.tt("dve", sm[:, 11:12], sm[:, 10:11], sm[:, 7:8], ALU.mult)
                k.ts("dve", cwi[:], mk1[:], sm[:, 10:11])
                k.stt(cwi[:], mk2[:], sm[:, 11:12], cwi[:], ALU.mult, ALU.add)
                k.tt("dve", cb_[:], ohg[:, :].unsqueeze(2).broadcast_to([128, 4, 4]), cwi[:, :].unsqueeze(1).broadcast_to([128, 4, 4]), ALU.mult)
                k.dma("pool", d["comb"][q0 + t * 128:q0 + (t + 1) * 128, :], cb_[:].rearrange("p g e -> p (g e)"), "do2")

            S1(0, ti)
            for t in range(4):
                if t + 1 < 4:
                    S1(t + 1, ti + 1)
                if qb + 1 < nqb:
                    cpart(qb + 1, [2 * t, 2 * t + 1])
                S2(t, ti)
                ti += 1
        k.barrier()


def phase_E(k, g):
    nc = k.nc
    d = g.d
    To = g.To
    nt = To // 128
    ps = g.ps
    psb = [p[:].bitcast(BF16) for p in ps]
    with contextlib.ExitStack() as es:
        sb = lambda name, shape, dt: k.sb(name, shape, dt, es)
        identb = sb("E_identb", [128, 128], BF16); k.dma("pool", identb[:], d["ident"][:, :], "c")
        wgu = sb("E_wgu", [128, 8, 8, 512], BF16)
        wdn = sb("E_wdn", [128, 8, 2, 1024], BF16)
        wpg = sb("E_wpg", [128, 8, 1024], BF16)
        k.dma("pool", wpg[:], d["w_ple_gate"].rearrange("(kk p) n -> p kk n", p=128), "w0")
        wpp = sb("E_wpp", [128, 2, 1024], BF16)
        k.dma("pool", wpp[:], d["w_ple_proj"].rearrange("(kk p) n -> p kk n", p=128), "w1")
        pgb = sb("E_pgb", [128, 1024], F32); k.dma("sp", pgb[:], d["b_ple_gate"].partition_broadcast(128), "c")
        gbc = sb("E_gbc", [128, 1024], F32); k.dma("sp", gbc[:], d["ln2_g"].partition_broadcast(128), "c")
        bbc = sb("E_bbc", [128, 1024], F32); k.dma("sp", bbc[:], d["ln2_b"].partition_broadcast(128), "c")
        epsc = sb("E_epsc", [128, 1], F32); k.memset("dve", epsc[:], 1e-5)
        h1T = [sb("E_h1T%d" % i, [128, 8, 128], BF16) for i in range(2)]
        comb = [sb("E_comb%d" % i, [128, 16], F32) for i in range(2)]
        h1 = [sb("E_h1_%d" % i, [128, 1024], F32) for i in range(2)]
        f0 = [sb("E_f0_%d" % i, [128, 1024], F32) for i in range(2)]
        pt = [sb("E_pt%d" % i, [128, 256], F32) for i in range(2)]
        ptb = sb("E_ptb", [128, 256], BF16)
        pT = sb("E_pT", [128, 2, 128], BF16)
        sg = [sb("E_sg%d" % i, [128, 256], F32) for i in range(2)]
        hid = [sb("E_hid%d" % i, [128, 256], BF16) for i in range(2)]
        hidT = [sb("E_hidT%d" % i, [128, 2, 128], BF16) for i in range(2)]
        fo = [sb("E_fo%d" % i, [128, 1024], F32) for i in range(2)]
        gate = sb("E_gate", [128, 512], F32)
        r = sb("E_r", [128, 1024], F32)
        outt = [sb("E_out%d" % i, [128, 1024], F32) for i in range(2)]
        stats = sb("E_stats", [128, 2, 6], F32)
        mv = sb("E_mv", [128, 2], F32)
        rstd = sb("E_rstd", [128, 1], F32)
        st = {"ei": 0}
        for pas in range(2):
            for e in range(8):
                ge_ = pas * 8 + e
                k.dma("pool", wgu[:, e, :, 0:256], d["w_exp_gate"][ge_].rearrange("(kk p) f -> p kk f", p=128), "w%d" % (e % 2))
                k.dma("pool", wgu[:, e, :, 256:512], d["w_exp_up"][ge_].rearrange("(kk p) f -> p kk f", p=128), "w%d" % (e % 2))
                k.dma("pool", wdn[:, e, :, :], d["w_exp_down"][ge_].rearrange("(fk p) n -> p fk n", p=128), "w%d" % (e % 2))

            def load(t):
                s = t % 2
                k.dma("sp", h1T[s][:], d["h1T"][:, :, t * 128:(t + 1) * 128].rearrange("kk p t -> p kk t"), "el%d" % s)
                k.dma("sp", comb[s][:], d["comb"][t * 128:(t + 1) * 128, :], "el%d" % s)
                if pas == 1:
                    k.dma("sp", h1[s][:], d["h1"][t * 128:(t + 1) * 128, :], "el%d" % s)
                    k.dma("sp", f0[s][:], d["ffn0"][t * 128:(t + 1) * 128, :], "el%d" % s)
                    k.dma("sp", pt[s][:], d["pin"][t * 128:(t + 1) * 128, :], "el%d" % s)

            load(0)
            for t in range(nt):
                s = t % 2
                if t + 1 < nt:
                    load(t + 1)
                def emit_gu(e):
                    pg = ps[2 + st["ei"] % 2]
                    sg_, hid_ = sg[st["ei"] % 2], hid[st["ei"] % 2]
                    st["ei"] += 1
                    ge_ = pas * 8 + e
                    for kk in range(8):
                        k.mm(pg[:, :], h1T[s][:, kk, :], wgu[:, e, kk, :], start=(kk == 0), stop=(kk == 7))
                    k.act(sg_[:], pg[:, 0:256], AF.Silu)
                    k.stt(hid_[:], sg_[:], comb[s][:, ge_:ge_ + 1], pg[:, 256:512], ALU.mult, ALU.mult)
                    return hid_
                def emit_down(e, hidT_):
                    for n in range(2):
                        for fk in range(2):
                            k.mm(ps[n][:, :], hidT_[:, fk, :], wdn[:, e, fk, n * 512:(n + 1) * 512],
                                 start=(e == 0 and fk == 0), stop=(e == 7 and fk == 1))
                nxt = emit_gu(0)
                pend = None
                for e in range(8):
                    hid_ = nxt
                    if e + 1 < 8:
                        nxt = emit_gu(e + 1)
                    hidT_ = hidT[e % 2]
                    pt_ = psb[4 + e % 2]
                    for fk in range(2):
                        k.tr(pt_[:, fk * 128:(fk + 1) * 128], hid_[:, fk * 128:(fk + 1) * 128], identb[:])
                    k.copy("act", hidT_[:].rearrange("p a b -> p (a b)"), pt_[:, 0:256])
                    if pend is not None:
                        emit_down(*pend)
                    pend = (e, hidT_)
                emit_down(*pend)
                if pas == 0:
                    fo_ = fo[t % 2]
                    k.copy("act", fo_[:, 0:512], ps[0][:, :])
                    k.copy("dve", fo_[:, 512:1024], ps[1][:, :])
                    k.dma("pool", d["ffn0"][t * 128:(t + 1) * 128, :], fo_[:], "eo%d" % (t % 2))
                else:
                    k.copy("act", ptb[:], pt[s][:])
                    for kk in range(2):
                        k.tr(psb[6][:, kk * 128:(kk + 1) * 128], ptb[:, kk * 128:(kk + 1) * 128], identb[:])
                    k.copy("act", pT[:].rearrange("p a b -> p (a b)"), psb[6][:, 0:256])
                    for n in range(2):
                        sl = slice(n * 512, (n + 1) * 512)
                        for kk in range(8):
                            k.mm(ps[6][:, :], h1T[s][:, kk, :], wpg[:, kk, sl], start=(kk == 0), stop=(kk == 7))
                        k.tt("dve", gate[:], ps[6][:, :], pgb[:, sl], ALU.add)
                        k.act(gate[:], gate[:], AF.Sigmoid)
                        for kk in range(2):
                            k.mm(ps[7][:, :], pT[:, kk, :], wpp[:, kk, sl], start=(kk == 0), stop=(kk == 1))
                        k.tt("dve", gate[:], gate[:], ps[7][:, :], ALU.mult)
                        k.tt("dve", r[:, sl], f0[s][:, sl], ps[n][:, :], ALU.add)
                        k.tt("pool", r[:, sl], r[:, sl], gate[:], ALU.add)
                        k.stt(r[:, sl], h1[s][:, sl], ALPHA, r[:, sl], ALU.mult, ALU.add)
                    o_ = outt[t % 2]
                    layer_norm_tile(k, nc, r, gbc, bbc, o_, stats, mv, rstd, epsc[:, 0:1])
                    k.dma("pool", d["out"][t * 128:(t + 1) * 128, :], o_[:], "eo%d" % (t % 2))
            k.barrier()
        k.finish([d["out"]])


_CACHE = {}


def kernel(**inputs):
    from concourse.bass_utils import run_bass_kernel_spmd
    x = np.asarray(inputs["x"], dtype=np.float32)
    p = np.asarray(inputs["p"], dtype=np.float32)
    B, S, _ = x.shape
    n_cores = 8
    per = n_cores // B
    To = S // per
    Tc = S - To
    T = Tc + To
    if (Tc, To) not in _CACHE:
        _CACHE[(Tc, To)] = build(Tc, To, debug=False, phases="ABCDE")
    nc, g = _CACHE[(Tc, To)]
    consts = make_consts(Tc, To)
    w = {}
    for n in INPUT_NAMES:
        a = np.asarray(inputs[n], dtype=np.float32)
        if n not in ("emb_ln_g", "emb_ln_b"):
            a = a[0]
        w[n] = np.ascontiguousarray(a)
    in_maps = []
    for c in range(n_cores):
        b, half = c // per, c % per
        own = x[b, half * To:(half + 1) * To]
        if half == 0:
            ctx = x[b, 0:Tc]
            tmask = np.concatenate([np.zeros(Tc, np.float32), np.ones(To, np.float32)])
        else:
            ctx = x[b, 0:Tc]
            tmask = np.ones(T, np.float32)
        m = {"xin": np.ascontiguousarray(np.concatenate([ctx, own], axis=0)),
             "pin": np.ascontiguousarray(p[0, b, half * To:(half + 1) * To]),
             "tmask": tmask,
             "tm_tok": np.ascontiguousarray(tmask.reshape(T // 128, 128).T)}
        m.update(w)
        m.update(consts)
        in_maps.append(m)
    res = run_bass_kernel_spmd(nc, in_maps, core_ids=list(range(n_cores)))
    out = np.empty((B, S, D), np.float32)
    for c in range(n_cores):
        b, half = c // per, c % per
        out[b, half * To:(half + 1) * To] = np.asarray(res.results[c]["out"], dtype=np.float32)
    return out
```

```python
import contextlib
import numpy as np
import concourse.bass as bass
import concourse.mybir as mybir

F32 = mybir.dt.float32
BF16 = mybir.dt.bfloat16
AF = mybir.ActivationFunctionType
ALU = mybir.AluOpType
AX = mybir.AxisListType

EPOCH = 30000


class Res:
    __slots__ = ("w", "r")

    def __init__(self):
        self.w = None
        self.r = {}


class KB:
    def __init__(self, nc, es):
        self.nc = nc
        self.es = es
        self.E = {"pe": nc.tensor, "act": nc.scalar, "dve": nc.vector, "pool": nc.gpsimd, "sp": nc.sync}
        self.sems = {}
        self.cur = {}
        self.epoch = {e: 0 for e in self.E}
        self.waited = {e: {} for e in self.E}
        self.res = {}
        self.dcount = {}
        self.dtarget = {}
        self.ringpos = {}
        self.RINGS = {"c": 6, "w0": 2, "w1": 2}
        self.ninstr = 0
        for e in self.E:
            self._new_epoch(e)

    def _sem(self, key):
        if key not in self.sems:
            self.sems[key] = self.es.enter_context(self.nc.semaphore(key))
        return self.sems[key]

    def _new_epoch(self, e):
        key = f"e_{e}_{self.epoch[e]}"
        self.epoch[e] += 1
        self._sem(key)
        self.cur[e] = [key, 0]

    def R(self, x):
        if isinstance(x, tuple):
            ap, key = x
            k = (ap.tensor.name, key)
        else:
            ap = x
            k = ap.tensor.name
        r = self.res.get(k)
        if r is None:
            r = self.res[k] = Res()
        return ap, r

    def _need(self, eng, ev):
        if ev is None:
            return
        key, val = ev
        if eng == "pe" and key.startswith("e_pe_"):
            return
        if key.startswith("d_"):
            val = self.dcount[key]
            if self.dtarget.get(key, 0) < val:
                self.dtarget[key] = val
        w = self.waited[eng]
        if w.get(key, 0) >= val:
            return
        self.E[eng].wait_ge(self.sems[key], val)
        w[key] = val

    def _pre(self, eng, reads, writes):
        for r in reads:
            self._need(eng, r.w)
        for w in writes:
            self._need(eng, w.w)
            for key, val in w.r.items():
                self._need(eng, (key, val))

    def _post(self, ev, reads, writes):
        key, val = ev
        for r in reads:
            if r.r.get(key, 0) < val:
                r.r[key] = val
        for w in writes:
            w.w = ev
            w.r = {}

    def op(self, eng, fn, outs, ins):
        rs = [self.R(x)[1] for x in ins if x is not None]
        ws = [self.R(x)[1] for x in outs]
        self._pre(eng, rs, ws)
        ins_ = fn()
        c = self.cur[eng]
        ins_.then_inc(self.sems[c[0]], 1)
        c[1] += 1
        ev = (c[0], c[1])
        self._post(ev, rs, ws)
        if c[1] >= EPOCH:
            self._new_epoch(eng)
        self.ninstr += 1
        return ins_

    def dma(self, q, out, in_, stream, **kw):
        oap, ow = self.R(out)
        iap, ir = self.R(in_)
        self._pre(q, [ir], [ow])
        ring = self.RINGS.get(stream, 1)
        if ring > 1:
            i = self.ringpos.get((q, stream), 0)
            self.ringpos[(q, stream)] = i + 1
            stream = "%s%d" % (stream, i % ring)
        key = "d_" + q + "_" + stream
        sem = self._sem(key)
        issued = self.dcount.get(key, 0)
        if issued and self.dtarget.get(key, 0) >= issued:
            self._need(q, (key, issued))
        self.E[q].dma_start(out=oap, in_=iap, **kw).then_inc(sem, 16)
        self.dcount[key] = self.dcount.get(key, 0) + 16
        ev = (key, self.dcount[key])
        self._post(ev, [ir], [ow])
        self.ninstr += 1

    def barrier(self):
        evs = [(c[0], c[1]) for c in self.cur.values() if c[1] > 0]
        evs += [(k, v) for k, v in self.dcount.items()]
        for e in self.E:
            for ev in evs:
                if ev[0].startswith("e_" + e + "_"):
                    continue
                self._need(e, ev)
        self.res = {}

    def finish(self, outs):
        for x in outs:
            _, r = self.R(x)
            self._need("sp", r.w)

    def mm(self, out, lhsT, rhs, start=True, stop=True, **kw):
        o = out[0] if isinstance(out, tuple) else out
        l = lhsT[0] if isinstance(lhsT, tuple) else lhsT
        r = rhs[0] if isinstance(rhs, tuple) else rhs
        return self.op("pe", lambda: self.nc.tensor.matmul(o, l, r, start=start, stop=stop, **kw), [out], [lhsT, rhs])

    def tr(self, out, in_, ident):
        o = out[0] if isinstance(out, tuple) else out
        i = in_[0] if isinstance(in_, tuple) else in_
        return self.op("pe", lambda: self.nc.tensor.transpose(o, i, ident), [out], [in_, ident])

    def act(self, out, in_, func, bias=None, scale=None, accum_out=None, eng="act"):
        o = out[0] if isinstance(out, tuple) else out
        i = in_[0] if isinstance(in_, tuple) else in_
        kw = {}
        ins = [in_]
        if bias is not None:
            kw["bias"] = bias[0] if isinstance(bias, tuple) else bias
            if not isinstance(bias, (int, float)):
                ins.append(bias)
        if scale is not None:
            kw["scale"] = scale[0] if isinstance(scale, tuple) else scale
            if not isinstance(scale, (int, float)):
                ins.append(scale)
        outs = [out]
        if accum_out is not None:
            kw["accum_out"] = accum_out[0] if isinstance(accum_out, tuple) else accum_out
            outs.append(accum_out)
        return self.op("act", lambda: self.nc.scalar.activation(out=o, in_=i, func=func, **kw), outs, ins)

    def tt(self, eng, out, in0, in1, op):
        o = out[0] if isinstance(out, tuple) else out
        a = in0[0] if isinstance(in0, tuple) else in0
        b = in1[0] if isinstance(in1, tuple) else in1
        return self.op(eng, lambda: self.E[eng].tensor_tensor(out=o, in0=a, in1=b, op=op), [out], [in0, in1])

    def ts(self, eng, out, in0, s1, s2=None, op0=ALU.mult, op1=None, accum_out=None):
        o = out[0] if isinstance(out, tuple) else out
        a = in0[0] if isinstance(in0, tuple) else in0
        ins = [in0]
        kw = {}
        if not isinstance(s1, (int, float)):
            ins.append(s1)
            s1 = s1[0] if isinstance(s1, tuple) else s1
        if s2 is not None and not isinstance(s2, (int, float)):
            ins.append(s2)
            s2 = s2[0] if isinstance(s2, tuple) else s2
        if op1 is not None:
            kw["op1"] = op1
        outs = [out]
        if accum_out is not None:
            kw["accum_out"] = accum_out[0] if isinstance(accum_out, tuple) else accum_out
            outs.append(accum_out)
        return self.op(eng, lambda: self.E[eng].tensor_scalar(out=o, in0=a, scalar1=s1, scalar2=s2, op0=op0, **kw), outs, ins)

    def stt(self, out, in0, scalar, in1, op0, op1):
        o = out[0] if isinstance(out, tuple) else out
        a = in0[0] if isinstance(in0, tuple) else in0
        b = in1[0] if isinstance(in1, tuple) else in1
        ins = [in0, in1]
        if not isinstance(scalar, (int, float)):
            ins.append(scalar)
            scalar = scalar[0] if isinstance(scalar, tuple) else scalar
        return self.op("dve", lambda: self.nc.vector.scalar_tensor_tensor(out=o, in0=a, scalar=scalar, in1=b, op0=op0, op1=op1), [out], ins)

    def copy(self, eng, out, in_):
        o = out[0] if isinstance(out, tuple) else out
        i = in_[0] if isinstance(in_, tuple) else in_
        if eng == "act":
            return self.op("act", lambda: self.nc.scalar.copy(out=o, in_=i), [out], [in_])
        return self.op(eng, lambda: self.E[eng].tensor_copy(out=o, in_=i), [out], [in_])

    def memset(self, eng, out, val):
        o = out[0] if isinstance(out, tuple) else out
        return self.op(eng, lambda: self.E[eng].memset(o, val), [out], [])

    def sb(self, name, shape, dt, es=None):
        es = es or self.es
        t = es.enter_context(self.nc.sbuf_tensor(name, shape, dt))
        nbytes = int(np.prod(shape[1:])) * (2 if dt == BF16 else 4)
        rem = (-nbytes) % 128
        if rem:
            es.enter_context(self.nc.sbuf_tensor(name + "_pad", [shape[0], rem // 2], BF16))
        return t


D = 1024
IN_DIM = 5640
C_Z, C_B, C_A, C_DQ, C_DK, C_DV, C_G = 1536, 2048, 2052, 2056, 2568, 3080, 3592
SLOPES = [2.0 ** (-8.0 * (h + 1) / 4) for h in range(4)]


def dram(nc, name, shape, dt, kind="Internal"):
    return nc.dram_tensor(name, list(shape), dt, kind=kind).ap()


class Ctx:
    pass


def make_consts(Tc, To):
    T = Tc + To
    c = {}
    c["ident"] = np.eye(128, dtype=np.float32)
    i = np.arange(128)
    c["U"] = (i[:, None] <= i[None, :]).astype(np.float32)
    c["negU"] = -c["U"]
    c["ones"] = np.ones((128, 128), np.float32)
    c["MLs"] = np.tile(np.where(i[:, None] > i[None, :], 0.0, -1e9).astype(np.float32), (1, 4))
    c["MUi"] = np.tile(np.where(i[None, :] >= i[:, None], 0.0, -1e9).astype(np.float32), (1, 4))
    c["SU01"] = np.tile((i[None, :] > i[:, None]).astype(np.float32), (1, 4))
    c["catri"] = np.where(i[:, None] <= i[None, :], 0.0, -30000.0).astype(np.float32)
    kpos = np.arange(T)
    qpos = Tc + np.arange(To)
    augk = np.zeros((4, 5, T), np.float32)
    augq = np.zeros((4, 4, To), np.float32)
    for h in range(4):
        s = SLOPES[h]
        augk[h, 0] = 1.0
        augk[h, 1] = 1.0
        augk[h, 2] = s * 128.0 * (kpos // 128)
        augk[h, 3] = 1.0
        augk[h, 4] = s * (kpos % 128)
        augq[h, 0] = -s * 128.0 * (qpos // 128)
        augq[h, 1] = 1.0
        augq[h, 2] = -s * (qpos % 128)
        augq[h, 3] = 1.0
    c["augk"] = augk
    c["augq"] = augq
    return c


INPUT_NAMES = ['emb_ln_g', 'emb_ln_b', 'w_in', 'b_in', 'conv_w', 'dn_a_log', 'dn_dt_bias', 'dn_norm_w', 'w_dn_o',
               'da_lq1', 'da_lk1', 'da_lq2', 'da_lk2', 'da_subln_w', 'w_da_o', 'w_out', 'ln1_g', 'ln1_b',
               'w_router_group', 'b_router_group', 'w_router_expert', 'b_router_expert',
               'w_exp_gate', 'w_exp_up', 'w_exp_down', 'w_ple_gate', 'b_ple_gate', 'w_ple_proj', 'ln2_g', 'ln2_b']
W_SHAPES = {
    'emb_ln_g': (1024,), 'emb_ln_b': (1024,), 'w_in': (1024, 5640), 'b_in': (5640,), 'conv_w': (4, 1536),
    'dn_a_log': (4,), 'dn_dt_bias': (4,), 'dn_norm_w': (128,), 'w_dn_o': (512, 1024),
    'da_lq1': (64,), 'da_lk1': (64,), 'da_lq2': (64,), 'da_lk2': (64,), 'da_subln_w': (128,),
    'w_da_o': (512, 1024), 'w_out': (1024, 1024), 'ln1_g': (1024,), 'ln1_b': (1024,),
    'w_router_group': (1024, 4), 'b_router_group': (4,), 'w_router_expert': (1024, 16), 'b_router_expert': (16,),
    'w_exp_gate': (16, 1024, 256), 'w_exp_up': (16, 1024, 256), 'w_exp_down': (16, 256, 1024),
    'w_ple_gate': (1024, 1024), 'b_ple_gate': (1024,), 'w_ple_proj': (256, 1024), 'ln2_g': (1024,), 'ln2_b': (1024,),
}


def colload(k, q, dst, src1d, n, M=128, stream="c"):
    k.dma(q, dst, src1d.rearrange("(c p) -> p c", p=M), stream, allow_slow_non_contiguous=True)


def phase_A(k, g):
    nc = k.nc
    d = g.d
    Tc, To, T = g.Tc, g.To, g.T
    nb = T // 512
    nbc = Tc // 512
    NW = C_G
    with contextlib.ExitStack() as es:
        sb = lambda name, shape, dt: k.sb(name, shape, dt, es)
        Wb = sb("A_Wb", [128, 8, NW], BF16)
        for kk in range(8):
            k.dma("pool", (Wb[:, kk, :], kk), d["w_in"][kk * 128:(kk + 1) * 128, 0:NW], "w%d" % (kk % 2))
        ident = sb("A_ident", [128, 128], BF16)
        k.dma("pool", ident[:], d["ident"][:, :], "c")
        onesb = sb("A_ones", [128, 128], BF16)
        k.dma("pool", onesb[:], d["ones"][:, :], "c")
        bcol = sb("A_bcol", [128, 12], F32)
        colload(k, "sp", bcol[:, :], d["b_in"][0:1536], 12)
        qcol = sb("A_qcol", [128, 4], F32)
        colload(k, "sp", qcol[:, :], d["b_in"][C_DQ:C_DQ + 512], 4)
        kcol = sb("A_kcol", [128, 4], F32)
        colload(k, "sp", kcol[:, :], d["b_in"][C_DK:C_DK + 512], 4)
        egc = sb("A_egc", [128, 8], F32)
        colload(k, "sp", egc[:, :], d["emb_ln_g"], 8)
        ebc = sb("A_ebc", [128, 8], F32)
        colload(k, "sp", ebc[:, :], d["emb_ln_b"], 8)
        cw = sb("A_cw", [128, 12, 4], F32)
        for j in range(4):
            k.dma("sp", cw[:, :, j], d["conv_w"][j, :].rearrange("(c p) -> p c", p=128), "c", allow_slow_non_contiguous=True)
        eg_bc = sb("A_eg_bc", [128, 1024], F32)
        k.dma("sp", eg_bc[:], d["emb_ln_g"].partition_broadcast(128), "c")
        eb_bc = sb("A_eb_bc", [128, 1024], F32)
        k.dma("sp", eb_bc[:], d["emb_ln_b"].partition_broadcast(128), "c")
        zb_bc = sb("A_zb_bc", [128, 512], F32)
        k.dma("sp", zb_bc[:], d["b_in"][C_Z:C_Z + 512].partition_broadcast(128), "c")
        vb_bc = sb("A_vb_bc", [128, 512], F32)
        k.dma("sp", vb_bc[:], d["b_in"][C_DV:C_DV + 512].partition_broadcast(128), "c")
        bab_bc = sb("A_bab_bc", [128, 8], F32)
        k.dma("sp", bab_bc[:], d["b_in"][C_B:C_B + 8].partition_broadcast(128), "c")
        nw_bc = sb("A_nw_bc", [128, 4, 128], F32)
        for h in range(4):
            k.dma("sp", nw_bc[:, h, :], d["dn_norm_w"].partition_broadcast(128), "c")
        dtb_bc = sb("A_dtb_bc", [128, 4], F32)
        k.dma("sp", dtb_bc[:], d["dn_dt_bias"].partition_broadcast(128), "c")
        negA = sb("A_negA", [128, 4], F32)
        k.dma("sp", negA[:], d["dn_a_log"].partition_broadcast(128), "c")
        k.act(negA[:], negA[:], AF.Exp)
        k.ts("dve", negA[:], negA[:], -1.0)
        tm_tok = sb("A_tm_tok", [128, T // 128], F32)
        k.dma("sp", tm_tok[:], d["tm_tok"][:, :], "c")
        epsc = sb("A_epsc", [128, 3], F32)
        k.memset("dve", epsc[:, 0:1], 1e-6)
        k.memset("dve", epsc[:, 1:2], 1e-5)
        k.memset("dve", epsc[:, 2:3], 1.0)
        kmx = sb("A_kmx", [128, 4, nb], F32)

        xblk = sb("A_x", [128, 4, 1024], F32)
        xn = sb("A_xn", [128, 4, 1024], BF16)
        xn32s = [sb("A_xn32_%d" % i, [128, 1024], F32) for i in range(2)]
        stats = sb("A_stats", [128, 4, 2, 6], F32)
        mv = sb("A_mv", [128, 4, 2], F32)
        rstd = sb("A_rstd", [128, 4], F32)
        hTs = [sb("A_hT%d" % i, [128, 8, 512], BF16) for i in range(2)]
        pre = sb("A_pre", [128, 12, 515], BF16)
        identf = sb("A_identf", [128, 128], F32)
        k.dma("sp", identf[:], d["ident"][:, :], "c")
        diagw = sb("A_diagw", [128, 12, 4, 128], BF16)
        for c_ in range(12):
            for j_ in range(4):
                k.ts("dve" if (c_ + j_) % 2 == 0 else "pool", diagw[:, c_, j_, :], identf[:], cw[:, c_, j_:j_ + 1])
        for c_ in range(12):
            k.memset("pool", (pre[:, c_, 0:3], c_), 0.0)
        tmbcs = [sb("A_tmbc%d" % i, [128, 512], F32) for i in range(2)]
        NR = 4
        qa = [sb("A_qa%d" % i, [128, 512], F32) for i in range(NR)]
        sq = [sb("A_sq%d" % i, [128, 512], BF16) for i in range(NR)]
        rinv = [sb("A_rinv%d" % i, [128, 512], F32) for i in range(NR)]
        qn = [sb("A_qn%d" % i, [128, 512], BF16) for i in range(NR)]
        vn = [sb("A_vn%d" % i, [128, 512], BF16) for i in range(3)]
        ktok = sb("A_ktok", [128, 4, 512], BF16)
        vtok = sb("A_vtok", [128, 4, 512], BF16)
        zzb = sb("A_zzb", [128, 4, 512], BF16)
        tmp32 = [sb("A_tmp32%d" % i, [128, 512], F32) for i in range(2)]
        vaug = sb("A_vaug", [128, 4, 4, 130], BF16)
        k.memset("pool", vaug[:], 0.0)
        ba = sb("A_ba", [128, 4, 8], F32)
        bgb = sb("A_bgb", [128, 4, 8], F32)
        spx = sb("A_spx", [128, 4, 4], F32)
        dq = [sb("A_dq%d" % i, [128, 512], BF16) for i in range(2)]
        dk = [sb("A_dk%d" % i, [128, 512], BF16) for i in range(2)]
        ps = g.ps
        psb = [p[:].bitcast(BF16) for p in ps]

        st = {"bank": 0, "cnt": 0}
        dfq = []

        def defer(n, fn):
            dfq.append([n, fn])

        def tick():
            for it in dfq:
                it[0] -= 1
            for it in [it for it in dfq if it[0] <= 0]:
                dfq.remove(it)
                it[1]()

        def flush():
            while dfq:
                it = dfq.pop(0)
                it[1]()

        def nextbank():
            st["bank"] = (st["bank"] + 1) % 6
            return 2 + st["bank"]

        def proj_f(hT, col0, M):
            b_ = nextbank()
            for kk in range(8):
                k.mm(ps[b_][0:M, :], (Wb[:, kk, col0:col0 + M], kk), hT[:, kk, :], start=(kk == 0), stop=(kk == 7))
            tick()
            return ps[b_]

        def proj_t(hT, t, col0, N):
            b_ = nextbank()
            for kk in range(8):
                k.mm(ps[b_][:, 0:N], hT[:, kk, t * 128:(t + 1) * 128], (Wb[:, kk, col0:col0 + N], kk), start=(kk == 0), stop=(kk == 7))
            tick()
            return ps[b_]

        def ln_stats(b):
            for t in range(4):
                for hh in range(2):
                    k.op("dve", lambda t=t, hh=hh: nc.vector.bn_stats(out=stats[:, t, hh, :], in_=xblk[:, t, hh * 512:(hh + 1) * 512]),
                         [stats[:]], [xblk[:]])
                k.op("dve", lambda t=t: nc.vector.bn_aggr(out=mv[:, t, :], in_=stats[:, t, :, :].rearrange("p a b -> p (a b)")),
                     [mv[:]], [stats[:]])
            k.act(rstd[:], mv[:, :, 1], AF.Sqrt, bias=epsc[:, 1:2])
            k.op("dve", lambda: nc.vector.reciprocal(out=rstd[:], in_=rstd[:]), [rstd[:]], [rstd[:]])

        def ln_apply(b, t):
            own = b >= nbc
            o0 = b * 512 - Tc
            xn32 = xn32s[t % 2]
            if own:
                k.ts("dve", xn32[:], xblk[:, t, :], mv[:, t, 0:1], rstd[:, t:t + 1], op0=ALU.subtract, op1=ALU.mult)
                k.copy("act", xn[:, t, :], xn32[:])
                k.tt("pool", xn32[:], xn32[:], eg_bc[:], ALU.mult)
                k.tt("pool", xn32[:], xn32[:], eb_bc[:], ALU.add)
                k.dma("pool", d["hres"][o0 + t * 128:o0 + (t + 1) * 128, :], xn32[:], "hres")
            else:
                k.ts("dve", xn[:, t, :], xblk[:, t, :], mv[:, t, 0:1], rstd[:, t:t + 1], op0=ALU.subtract, op1=ALU.mult)
            if t == 3 and b + 1 < nb:
                k.dma("sp", xblk[:], d["xin"][(b + 1) * 512:(b + 2) * 512, :].rearrange("(t p) f -> p t f", p=128), "x")

        def layer_norm(b):
            ln_stats(b)
            for t in range(4):
                ln_apply(b, t)

        def transposes(b):
            hT = hTs[b % 2]
            for kk in range(8):
                pb = psb[kk % 2]
                for t in range(4):
                    k.tr(pb[:, t * 128:(t + 1) * 128], xn[:, t, kk * 128:(kk + 1) * 128], ident[:])
                k.act(hT[:, kk, :], pb[:, 0:512], AF.Identity, bias=ebc[:, kk:kk + 1], scale=egc[:, kk:kk + 1])
            if b >= nbc:
                o0 = b * 512 - Tc
                k.dma("pool", d["hT_own"][:, :, o0:o0 + 512].rearrange("kk p t -> p kk t"), hT[:], "hT")

        k.dma("sp", xblk[:], d["xin"][0:512, :].rearrange("(t p) f -> p t f", p=128), "x")
        k.dma("sp", tmbcs[0][:], d["tmask"][0:512].partition_broadcast(128), "tm")
        layer_norm(0)
        transposes(0)
        for b in range(nb):
            own = b >= nbc
            t0 = b * 512
            o0 = t0 - Tc
            hT = hTs[b % 2]
            tmbc = tmbcs[b % 2]
            if b + 1 < nb:
                k.dma("sp", tmbcs[(b + 1) % 2][:], d["tmask"][t0 + 512:t0 + 1024].partition_broadcast(128), "tm")
            for c in range(12):
                isq = c < 4
                if isq and not (own or b == nbc - 1):
                    continue
                p_ = proj_f(hT, c * 128, 128)
                k.stt((pre[:, c, 3:515], c), p_[:, :], bcol[:, c:c + 1], tmbc[:], ALU.add, ALU.mult)
                if isq and not own:
                    k.copy("pool", (pre[:, c, 0:3], c), (pre[:, c, 512:515], c))
                    continue
                i_ = st["cnt"]
                st["cnt"] += 1
                q_, s_, r_, n_ = qa[i_ % NR], sq[i_ % NR], rinv[i_ % NR], qn[i_ % NR]
                if c >= 8:
                    n_ = vn[i_ % 3]
                h = c % 4

                def stage2(c=c, h=h, n_=n_):
                    dst = ktok if c < 8 else vtok
                    pb = psb[nextbank()]
                    for t in range(4):
                        k.tr(pb[:, t * 128:(t + 1) * 128], n_[:, t * 128:(t + 1) * 128], ident[:])
                    k.copy("act", dst[:, :, h * 128:(h + 1) * 128], pb[:, 0:512].rearrange("p (t d) -> p t d", t=4))

                def stage1b(c=c, h=h, q_=q_, r_=r_, n_=n_, isq=isq, stage2=stage2, o0=o0, t0=t0):
                    k.op("dve", lambda: nc.vector.reciprocal(out=r_[:], in_=r_[:]), [r_[:]], [r_[:]])
                    if isq:
                        k.stt(n_[:], q_[:], 128.0 ** -0.5, r_[:], ALU.mult, ALU.mult)
                        k.dma("pool", d["dn_qT"][h, :, o0:o0 + 512], n_[:], "o0")
                    else:
                        k.tt("dve", n_[:], q_[:], r_[:], ALU.mult)
                        k.dma("pool", d["dn_kT"][h, :, t0:t0 + 512], n_[:], "o1")
                        defer(2, stage2)

                def stage1(c=c, s_=s_, r_=r_, stage1b=stage1b):
                    b2 = nextbank()
                    k.mm(ps[b2][:, :], onesb[:], s_[:])
                    k.act(r_[:], ps[b2][:, :], AF.Sqrt, bias=epsc[:, 0:1])
                    defer(1, stage1b)

                def stage0(c=c, q_=q_, s_=s_, n_=n_, stage1=stage1, stage2=stage2):
                    b3 = nextbank()
                    for j in range(4):
                        k.mm(ps[b3][:, :], diagw[:, c, j, :], (pre[:, c, j:j + 512], c), start=(j == 0), stop=(j == 3))
                    k.copy("pool", (pre[:, c, 0:3], c), (pre[:, c, 512:515], c))
                    if c < 8:
                        k.act(q_[:], ps[b3][:, :], AF.Silu)
                        k.act(s_[:], q_[:], AF.Square)
                        defer(2, stage1)
                    else:
                        k.act(n_[:], ps[b3][:, :], AF.Silu)
                        defer(2, stage2)

                defer(2, stage0)
            if b + 1 < nb:
                ln_stats(b + 1)
            for t in range(4):
                if b + 1 < nb:
                    ln_apply(b + 1, t)
                if own:
                    p_ = proj_t(hT, t, C_Z, 512)
                    k.tt("dve", tmp32[0][:], p_[:, :], zb_bc[:], ALU.add)
                    k.act(tmp32[0][:], tmp32[0][:], AF.Silu)
                    k.tt("pool", zzb[:, t, :], tmp32[0][:], nw_bc[:].rearrange("p h d -> p (h d)"), ALU.mult)
                p_ = proj_t(hT, t, C_B, 8)
                k.tt("dve", ba[:, t, :], p_[:, 0:8], bab_bc[:], ALU.add)
                p_ = proj_t(hT, t, C_DV, 512)
                k.tt("dve", tmp32[1][:], p_[:, :], vb_bc[:], ALU.add)
                tmc = tm_tok[:, b * 4 + t:b * 4 + t + 1]
                k.act(vaug[:, t, :, 0:128], tmp32[1][:].rearrange("p (h d) -> p h d", h=4), AF.Copy, scale=tmc)
                k.copy("pool", vaug[:, t, :, 128:129], tmc.unsqueeze(1).broadcast_to([128, 4, 1]))
            if own:
                k.dma("pool", d["dn_zz"][o0:o0 + 512, :].rearrange("(t p) f -> p t f", p=128), zzb[:], "o4")
            k.dma("pool", d["da_v"][t0:t0 + 512, :, :].rearrange("(t p) h e -> p t h e", p=128), vaug[:], "o5")
            k.act(bgb[:, :, 0:4], ba[:, :, 0:4], AF.Sigmoid)
            k.tt("dve", spx[:], ba[:, :, 4:8], dtb_bc[:].unsqueeze(1).broadcast_to([128, 4, 4]), ALU.add)
            k.act(spx[:], spx[:], AF.Exp)
            k.act(spx[:], spx[:], AF.Ln, bias=epsc[:, 2:3])
            k.tt("dve", bgb[:, :, 4:8], spx[:], negA[:].unsqueeze(1).broadcast_to([128, 4, 4]), ALU.mult)
            k.dma("pool", d["dn_bg"][t0:t0 + 512, 0:8].rearrange("(t p) f -> p t f", p=128), bgb[:], "o6")
            for h in range(4):
                if own:
                    p_ = proj_f(hT, C_DQ + h * 128, 128)
                    q_ = dq[h % 2]
                    k.ts("dve", q_[:], p_[:, :], qcol[:, h:h + 1], 0.125, op0=ALU.add, op1=ALU.mult)
                    k.dma("pool", d["da_qT"][h, :, :, o0:o0 + 512].rearrange("m d t -> (m d) t"), q_[:], "o7")
                p_ = proj_f(hT, C_DK + h * 128, 128)
                k_ = dk[h % 2]
                k.ts("dve", k_[:], p_[:, :], kcol[:, h:h + 1], None, op0=ALU.add)
                k.op("dve", lambda k_=k_, h=h, b=b: nc.vector.tensor_reduce(out=kmx[:, h, b:b + 1], in_=k_[:], axis=AX.X, op=ALU.max,
                                                                          apply_absolute_value=True), [kmx[:]], [k_[:]])
                k.dma("pool", d["da_kT"][h, :, :, t0:t0 + 512].rearrange("m d t -> (m d) t"), k_[:], "o8")
            flush()
            k.dma("pool", d["dn_ktok"][t0:t0 + 512, :].rearrange("(t p) f -> p t f", p=128), ktok[:], "o2")
            k.dma("pool", d["dn_vtok"][t0:t0 + 512, :].rearrange("(t p) f -> p t f", p=128), vtok[:], "o3")
            if b + 1 < nb:
                transposes(b + 1)
        kmxf = sb("A_kmxf", [128, 4], F32)
        k.op("dve", lambda: nc.vector.tensor_reduce(out=kmxf[:], in_=kmx[:], axis=AX.X, op=ALU.max), [kmxf[:]], [kmx[:]])
        for m in range(2):
            k.dma("pool", d["kmaxabs"][:, 0:8].rearrange("d (h m) -> d h m", m=2)[:, :, m], kmxf[m * 64:(m + 1) * 64, :], "o9",
                  allow_slow_non_contiguous=True)
        k.barrier()


def declare_dram(nc, g, debug):
    Tc, To, T = g.Tc, g.To, g.T
    d = {}
    kin = "ExternalInput"
    d["xin"] = dram(nc, "xin", [T, D], F32, kin)
    d["pin"] = dram(nc, "pin", [To, 256], F32, kin)
    d["tm_tok"] = dram(nc, "tm_tok", [128, T // 128], F32, kin)
    d["tmask"] = dram(nc, "tmask", [T], F32, kin)
    for n in INPUT_NAMES:
        d[n] = dram(nc, n, W_SHAPES[n], F32, kin)
    for n, a in make_consts(Tc, To).items():
        d[n] = dram(nc, n, a.shape, F32, kin)
    sk = "ExternalOutput" if debug else "Internal"
    g.scratch = {}

    def scr(name, shape, dt):
        d[name] = dram(nc, name, shape, dt, sk)
        g.scratch[name] = (shape, dt)
    scr("hres", [To, D], F32)
    scr("dn_qT", [4, 128, To], BF16)
    scr("dn_kT", [4, 128, T], BF16)
    scr("dn_ktok", [T, 512], BF16)
    scr("dn_vtok", [T, 512], BF16)
    scr("dn_bg", [T, 16], F32)
    scr("dn_zz", [To, 512], BF16)
    scr("da_qT", [4, 2, 64, To], BF16)
    scr("da_kT", [4, 2, 64, T], BF16)
    scr("da_v", [T, 4, 130], BF16)
    scr("hT_own", [8, 128, To], BF16)
    scr("kmaxabs", [64, 16], F32)
    scr("oT_dn", [4, 128, To], BF16)
    scr("oT_da", [4, 128, To], BF16)
    scr("h1", [To, D], F32)
    scr("h1T", [8, 128, To], BF16)
    scr("comb", [To, 16], F32)
    scr("ffn0", [To, D], F32)
    d["out"] = dram(nc, "out", [To, D], F32, "ExternalOutput")
    return d


def build(Tc, To, debug=False, phases="A"):
    nc = bass.Bass("TRN2", target_bir_lowering=False)
    g = Ctx()
    g.Tc, g.To, g.T = Tc, To, Tc + To
    g.d = declare_dram(nc, g, debug)
    es = contextlib.ExitStack()
    with es:
        k = KB(nc, es)
        g.ps = [es.enter_context(nc.psum_tensor("ps%d" % i, [128, 512], F32)) for i in range(8)]
        if "A" in phases:
            phase_A(k, g)
        if "B" in phases and "C" in phases and "S" not in phases:
            with contextlib.ExitStack() as esb:
                gen = gen_B(k, g, esb, (4, 5, 7))
                state = {"done": False}
                next(gen)

                def stepper():
                    if not state["done"]:
                        try:
                            next(gen)
                        except StopIteration:
                            state["done"] = True
                phase_C(k, g, stepper)
                while not state["done"]:
                    stepper()
                k.barrier()
        else:
            if "B" in phases:
                phase_B(k, g)
            if "C" in phases:
                phase_C(k, g)
        if "D" in phases:
            phase_D(k, g)
        if "E" in phases:
            phase_E(k, g)
        k.barrier()
        print("instructions:", k.ninstr, "sems:", len(k.sems))
    return nc, g


def gen_B(k, g, es, bk):
    nc = k.nc
    d = g.d
    Tc, To, T = g.Tc, g.To, g.T
    nch = T // 128
    ncc = Tc // 128
    ps = g.ps
    psb = [p[:].bitcast(BF16) for p in ps]
    X, Y, Z = [ps[i] for i in bk]
    XB = X[:].bitcast(BF16)
    if True:
        sb = lambda name, shape, dt: k.sb(name, shape, dt, es)
        U = sb("B_U", [128, 128], F32); k.dma("sp", U[:], d["U"][:, :], "c")
        negU = sb("B_negU", [128, 128], F32); k.dma("sp", negU[:], d["negU"][:, :], "c")
        ones = sb("B_ones", [128, 128], F32); k.dma("sp", ones[:], d["ones"][:, :], "c")
        identb = sb("B_identb", [128, 128], BF16); k.dma("pool", identb[:], d["ident"][:, :], "c")
        MLs = sb("B_MLs", [128, 512], F32); k.dma("sp", MLs[:], d["MLs"][:, :], "c")
        MUi = sb("B_MUi", [128, 512], F32); k.dma("sp", MUi[:], d["MUi"][:, :], "c")
        SU01 = sb("B_SU01", [128, 512], F32); k.dma("sp", SU01[:], d["SU01"][:, :], "c")
        epsc = sb("B_epsc", [128, 1], F32); k.memset("dve", epsc[:], 1e-6)
        S32 = sb("B_S32", [128, 4, 128], F32); k.memset("dve", S32[:], 0.0)
        Sb = sb("B_Sb", [128, 4, 128], BF16); k.memset("pool", Sb[:], 0.0)
        KT = [sb("B_KT%d" % i, [128, 4, 128], BF16) for i in range(2)]
        QT = [sb("B_QT%d" % i, [128, 4, 128], BF16) for i in range(2)]
        ktok = [sb("B_ktok%d" % i, [128, 4, 128], BF16) for i in range(2)]
        vtok = [sb("B_vtok%d" % i, [128, 4, 128], BF16) for i in range(2)]
        zz = [sb("B_zz%d" % i, [128, 4, 128], BF16) for i in range(2)]
        bg = [sb("B_bg%d" % i, [128, 8], F32) for i in range(2)]
        gB = sb("B_gB", [128, 4, 128], F32)
        Gc = sb("B_Gc", [128, 4], F32)
        cb = sb("B_cb", [128, 4], F32)
        cg = sb("B_cg", [128, 4], F32)
        egl = sb("B_egl", [128, 4], F32)
        nbeta = sb("B_nbeta", [128, 4], F32)
        argL = sb("B_argL", [128, 4, 128], F32)
        argU = sb("B_argU", [128, 4, 128], F32)
        Dst = sb("B_Dst", [128, 4, 128], F32)
        DTi = sb("B_DTi", [128, 4, 128], F32)
        kbg = sb("B_kbg", [128, 4, 128], BF16)
        kg = sb("B_kg", [128, 4, 128], BF16)
        vb = sb("B_vb", [128, 4, 128], BF16)
        P = [sb("B_P%d" % i, [128, 4, 128], BF16) for i in range(2)]
        PT = [sb("B_PT%d" % i, [128, 4, 128], BF16) for i in range(2)]
        AT = [sb("B_AT%d" % i, [128, 4, 128], BF16) for i in range(2)]
        u = sb("B_u", [128, 4, 128], F32)
        wT = sb("B_wT", [128, 4, 128], BF16)
        aT = sb("B_aT", [128, 4, 128], BF16)
        eGrow = sb("B_eGrow", [128, 4, 128], F32)
        QgT = sb("B_QgT", [128, 4, 128], BF16)
        vnew = sb("B_vnew", [128, 4, 128], BF16)
        ssq = sb("B_ssq", [128, 4], F32)
        junk = sb("B_junk", [128, 128], F32)
        og = sb("B_og", [128, 4, 128], BF16)
        oT = [sb("B_oT%d" % i, [128, 4, 128], BF16) for i in range(2)]

        def bc4(ap):
            return ap.unsqueeze(2).broadcast_to([128, 4, 128])

        def load(c):
            s = c % 2
            t0 = c * 128
            k.dma("sp", KT[s][:], d["dn_kT"][:, :, t0:t0 + 128].rearrange("h d t -> d h t"), "bl%d" % s)
            k.dma("sp", ktok[s][:].rearrange("p h d -> p (h d)"), d["dn_ktok"][t0:t0 + 128, :], "bl%d" % s)
            k.dma("sp", vtok[s][:].rearrange("p h d -> p (h d)"), d["dn_vtok"][t0:t0 + 128, :], "bl%d" % s)
            k.dma("sp", bg[s][:], d["dn_bg"][t0:t0 + 128, 0:8], "bl%d" % s)
            if c >= ncc:
                o0 = t0 - Tc
                k.dma("sp", QT[s][:], d["dn_qT"][:, :, o0:o0 + 128].rearrange("h d t -> d h t"), "bl%d" % s)
                k.dma("sp", zz[s][:].rearrange("p h d -> p (h d)"), d["dn_zz"][o0:o0 + 128, :], "bl%d" % s)

        yield
        load(0)
        for c in range(nch):
            s = c % 2
            own = c >= ncc
            o0 = c * 128 - Tc
            if c + 1 < nch:
                load(c + 1)
            beta = bg[s][:, 0:4]
            gg = bg[s][:, 4:8]
            k.copy("dve", gB[:], bc4(gg))
            k.mm(X[:, 0:4], U[:], gg)
            k.mm(X[:, 8:12], ones[:], gg)
            for h in range(4):
                k.mm(Y[:, h * 128:(h + 1) * 128], U[:], gB[:, h, :], start=True, stop=False)
                k.mm(Y[:, h * 128:(h + 1) * 128], gB[:, h, :], negU[:], start=False, stop=True)
            if own:
                for h in range(4):
                    k.mm(Z[:, h * 128:(h + 1) * 128], gB[:, h, :], U[:])
            yield
            k.copy("dve", Gc[:], X[:, 0:4])
            k.tt("dve", cg[:], X[:, 8:12], Gc[:], ALU.subtract)
            k.ts("dve", nbeta[:], beta, -1.0)
            k.tt("dve", argL[:].rearrange("p h d -> p (h d)"), Y[:, :], MLs[:], ALU.add)
            if own:
                k.stt(argU[:].rearrange("p h d -> p (h d)"), Y[:, :], -1.0, MUi[:], ALU.mult, ALU.add)
            yield
            k.act(cb[:], Gc[:], AF.Exp)
            k.act(cg[:], cg[:], AF.Exp)
            k.act(egl[:], X[:, 8:12], AF.Exp)
            k.act(Dst[:], argL[:], AF.Exp)
            if own:
                k.act(DTi[:], argU[:], AF.Exp)
                k.act(eGrow[:].rearrange("p h d -> p (h d)"), Z[:, :], AF.Exp)
            yield
            k.tt("dve", cb[:], cb[:], beta, ALU.mult)
            if own:
                k.tt("pool", QgT[:], QT[s][:], eGrow[:], ALU.mult)
            k.tt("pool", kbg[:], ktok[s][:], bc4(cb[:, :]), ALU.mult)
            k.tt("pool", kg[:], ktok[s][:], bc4(cg[:, :]), ALU.mult)
            k.tt("pool", vb[:], vtok[s][:], bc4(beta), ALU.mult)
            for h in range(4):
                k.mm(X[:, h * 128:(h + 1) * 128], KT[s][:, h, :], KT[s][:, h, :])
            yield
            for h in range(4):
                k.stt(P[0][:, h, :], X[:, h * 128:(h + 1) * 128], nbeta[:, h:h + 1], Dst[:, h, :], ALU.mult, ALU.mult)
            yield
            for h in range(4):
                k.mm(Y[:, h * 128:(h + 1) * 128], P[0][:, h, :], identb[:])
            yield
            k.copy("dve", PT[0][:].rearrange("p h d -> p (h d)"), Y[:, :])
            k.tt("pool", AT[0][:], PT[0][:], identb[:, :].unsqueeze(1).broadcast_to([128, 4, 128]), ALU.add)
            yield
            ci = 0
            ai = 0
            for it in range(1, 7):
                ni = 1 - ci
                for h in range(4):
                    k.mm(X[:, h * 128:(h + 1) * 128], PT[ci][:, h, :], P[ci][:, h, :])
                if it < 6:
                    for h in range(4):
                        k.mm(Y[:, h * 128:(h + 1) * 128], P[ci][:, h, :], PT[ci][:, h, :])
                yield
                k.copy("dve", P[ni][:].rearrange("p h d -> p (h d)"), X[:, :])
                if it < 6:
                    k.copy("dve", PT[ni][:].rearrange("p h d -> p (h d)"), Y[:, :])
                yield
                yield
                for h in range(4):
                    k.mm(Z[:, h * 128:(h + 1) * 128], P[ni][:, h, :], AT[ai][:, h, :])
                yield
                k.tt("dve", AT[1 - ai][:].rearrange("p h d -> p (h d)"), Z[:, :], AT[ai][:].rearrange("p h d -> p (h d)"), ALU.add)
                ai = 1 - ai
                ci = ni
            TT = AT[ai]
            yield
            for h in range(4):
                k.mm(X[:, h * 128:(h + 1) * 128], TT[:, h, :], vb[:, h, :])
            for h in range(4):
                k.mm(Y[:, h * 128:(h + 1) * 128], kbg[:, h, :], TT[:, h, :])
            if own:
                for h in range(4):
                    k.mm(Z[:, h * 128:(h + 1) * 128], KT[s][:, h, :], QT[s][:, h, :])
            yield
            k.copy("dve", u[:].rearrange("p h d -> p (h d)"), X[:, :])
            k.copy("dve", wT[:].rearrange("p h d -> p (h d)"), Y[:, :])
            if own:
                k.tt("dve", aT[:].rearrange("p h d -> p (h d)"), Z[:, :], DTi[:].rearrange("p h d -> p (h d)"), ALU.mult)
            yield
            yield
            for h in range(4):
                k.mm(X[:, h * 128:(h + 1) * 128], wT[:, h, :], Sb[:, h, :])
            yield
            k.tt("dve", vnew[:].rearrange("p h d -> p (h d)"), u[:].rearrange("p h d -> p (h d)"), X[:, :], ALU.subtract)
            yield
            yield
            if own:
                for h in range(4):
                    k.mm(Y[:, h * 128:(h + 1) * 128], QgT[:, h, :], Sb[:, h, :], start=True, stop=False)
                    k.mm(Y[:, h * 128:(h + 1) * 128], aT[:, h, :], vnew[:, h, :], start=False, stop=True)
            for h in range(4):
                k.mm(Z[:, h * 128:(h + 1) * 128], kg[:, h, :], vnew[:, h, :])
            yield
            k.tt("dve", S32[:], S32[:], bc4(egl[:, :]), ALU.mult)
            k.tt("dve", S32[:].rearrange("p h d -> p (h d)"), S32[:].rearrange("p h d -> p (h d)"), Z[:, :], ALU.add)
            k.copy("dve", Sb[:], S32[:])
            if own:
                yield
                for h in range(4):
                    k.act(junk[:], Y[:, h * 128:(h + 1) * 128], AF.Square, accum_out=ssq[:, h:h + 1])
                k.act(ssq[:], ssq[:], AF.Sqrt, bias=epsc[:, 0:1], scale=1.0 / 128.0)
                yield
                k.op("dve", lambda: nc.vector.reciprocal(out=ssq[:], in_=ssq[:]), [ssq[:]], [ssq[:]])
                for h in range(4):
                    k.stt(og[:, h, :], Y[:, h * 128:(h + 1) * 128], ssq[:, h:h + 1], zz[s][:, h, :], ALU.mult, ALU.mult)
                yield
                for h in range(4):
                    k.tr(XB[:, h * 128:(h + 1) * 128], og[:, h, :], identb[:])
                yield
                o_ = oT[c % 2]
                k.copy("dve", o_[:].rearrange("p h d -> p (h d)"), XB[:, 0:512])
                k.dma("pool", d["oT_dn"][:, :, o0:o0 + 128].rearrange("h d t -> d h t"), o_[:], "bo%d" % (c % 2))


def phase_B(k, g):
    with contextlib.ExitStack() as es:
        for _ in gen_B(k, g, es, (4, 5, 7)):
            pass
        k.barrier()


def phase_C(k, g, stepper=None):
    nc = k.nc
    d = g.d
    Tc, To, T = g.Tc, g.To, g.T
    nkt = T // 128
    nqb = To // 512
    ps = g.ps
    psb = [p[:].bitcast(BF16) for p in ps]
    lam_init = 0.8 - 0.6
    with contextlib.ExitStack() as es:
        sb = lambda name, shape, dt: k.sb(name, shape, dt, es)
        identb = sb("C_identb", [128, 128], BF16); k.dma("pool", identb[:], d["ident"][:, :], "c")
        catrib = sb("C_catrib", [128, 128], BF16); k.dma("pool", catrib[:], d["catri"][:, :], "c")
        epsc = sb("C_epsc", [128, 1], F32); k.memset("dve", epsc[:], 1e-6)
        lv = sb("C_lv", [128, 4, 64], F32)
        for i, n in enumerate(["da_lq1", "da_lk1", "da_lq2", "da_lk2"]):
            k.dma("sp", lv[:, i, :], d[n].partition_broadcast(128), "c")
        lp = sb("C_lp", [128, 2, 64], F32)
        k.tt("dve", lp[:, 0, :], lv[:, 0, :], lv[:, 1, :], ALU.mult)
        k.tt("dve", lp[:, 1, :], lv[:, 2, :], lv[:, 3, :], ALU.mult)
        ls = sb("C_ls", [128, 2], F32)
        k.op("dve", lambda: nc.vector.tensor_reduce(out=ls[:], in_=lp[:], axis=AX.X, op=ALU.add), [ls[:]], [lp[:]])
        k.act(ls[:], ls[:], AF.Exp)
        neglam = sb("C_neglam", [128, 1], F32)
        k.tt("dve", neglam[:], ls[:, 1:2], ls[:, 0:1], ALU.subtract)
        k.ts("dve", neglam[:], neglam[:], -lam_init, op0=ALU.add)
        wbc = sb("C_wbc", [128, 128], F32)
        k.dma("sp", wbc[:], d["da_subln_w"].partition_broadcast(128), "c")
        k.ts("dve", wbc[:], wbc[:], 1.0 - lam_init)
        kmx = sb("C_kmx", [64, 8], F32); k.dma("sp", kmx[:], d["kmaxabs"][:, 0:8], "c")
        kmxL = sb("C_kmxL", [64, 8, 65], BF16)
        k.memset("dve", kmxL[:], 0.0)
        k.copy("dve", kmxL[:, :, 64:65], kmx[:, :].unsqueeze(2))

        Va = [sb("C_Va%d" % i, [128, nkt, 130], BF16) for i in range(2)]
        kTa = [[sb("C_kTa%d_%d" % (i, m), [69, T], BF16) for m in range(2)] for i in range(2)]
        qTa = [[sb("C_qTa%d_%d" % (i, m), [69, To], BF16) for m in range(2)] for i in range(2)]
        absqs = [sb("C_absq%d" % i, [64, 512], BF16) for i in range(2)]
        PT = [sb("C_PT%d" % i, [128, 512], BF16) for i in range(4)]
        o1 = sb("C_o1", [128, 4, 128], F32)
        rden = sb("C_rden", [128, 4], F32)
        ssqs = [sb("C_ssq%d" % i, [128, 4], F32) for i in range(2)]
        o1s = [sb("C_o1s%d" % i, [128, 4, 128], F32) for i in range(2)]
        junk = sb("C_junk", [128, 128], F32)
        ons = [sb("C_on%d" % i, [128, 4, 128], BF16) for i in range(2)]
        oT = [sb("C_oT%d" % i, [128, 512], BF16) for i in range(2)]

        def load(h):
            s = h % 2
            k.dma("sp", Va[s][:], d["da_v"][:, h, :].rearrange("(kt p) e -> p kt e", p=128), "cl%d" % s)
            for m in range(2):
                k.dma("sp", kTa[s][m][0:64, :], d["da_kT"][h, m, :, :], "cl%d" % s)
                k.dma("pool", kTa[s][m][64:69, :], d["augk"][h, :, :], "cl%d" % s)
                k.dma("sp", qTa[s][m][0:64, :], d["da_qT"][h, m, :, :], "cl%d" % s)
                k.dma("pool", qTa[s][m][65:69, :], d["augq"][h, :, :], "cl%d" % s)

        accs = sb("C_accs", [128, 4, 129], F32)

        def accv(j):
            return ps[2 + j // 2][:, (j % 2) * 256:(j % 2) * 256 + 129]

        load(0)
        st = {"pti": 0, "si": 0, "pend": [], "epi": 0, "ab": 0}
        for h in range(4):
            s = h % 2
            if h + 1 < 4:
                load(h + 1)
            def bound(hh, m, qb):
                s2 = hh % 2
                hm = hh * 2 + m
                a_ = absqs[st["ab"] % 2]
                st["ab"] += 1
                k.act(a_[:], qTa[s2][m][0:64, qb * 512:(qb + 1) * 512], AF.Abs)
                k.mm(ps[6][0:65, :], kmxL[:, hm, :], a_[:])
                k.act(qTa[s2][m][64:65, qb * 512:(qb + 1) * 512], ps[6][64:65, :], AF.Copy, scale=-1.0)
                return lambda: None
            if h == 0:
                for m in range(2):
                    for qb in range(nqb):
                        bound(0, m, qb)()
            for qb in range(nqb):
                Q0 = Tc // 128 + 4 * qb
                for m in range(2):
                    def emit_S(kt):
                        c = max(kt - Q0, 0)
                        lo = 128 * c
                        pS = ps[(0, 1, 6)[st["si"] % 3]]
                        st["si"] += 1
                        dg = kt >= Q0
                        k.mm(pS[:, lo:512], kTa[s][m][0:69, kt * 128:(kt + 1) * 128], qTa[s][m][0:69, qb * 512 + lo:(qb + 1) * 512],
                             start=True, stop=not dg)
                        if dg:
                            k.mm(pS[:, lo:lo + 128], identb[:], catrib[:], start=False, stop=True)
                        P_ = PT[st["pti"] % 4]
                        st["pti"] += 1
                        k.act(P_[:, lo:512], pS[:, lo:512], AF.Exp)
                        return P_, c
                    nkt = Q0 + 4
                    ahead = [emit_S(0)]
                    if nkt > 1:
                        ahead.append(emit_S(1))
                    for kt in range(nkt):
                        P_, c = ahead.pop(0)
                        if kt + 2 < nkt:
                            ahead.append(emit_S(kt + 2))
                        if stepper is not None:
                            stepper()
                        if st["pend"] and kt in (1, 3, 5):
                            st["pend"].pop(0)()
                        if kt == min(6, Q0 + 3) - 1 and h + 1 < 4:
                            st["bnd"] = bound(h + 1, m, qb)
                        elif st.get("bnd") is not None:
                            st["bnd"]()
                            st["bnd"] = None
                        for j in range(c, 4):
                            k.mm(accv(j), P_[:, j * 128:(j + 1) * 128], Va[s][:, kt, 0:129],
                                 start=(kt == 0 and j % 2 == 0), stop=(kt == Q0 + j), skip_group_check=True)
                    for j in range(4):
                        k.copy("dve", accs[:, j, :], accv(j))
                    k.op("dve", lambda: nc.vector.reciprocal(out=rden[:], in_=accs[:, :, 128]), [rden[:]], [accs[:]])
                    if m == 0:
                        for j in range(4):
                            k.ts("dve", o1[:, j, :], accs[:, j, 0:128], rden[:, j:j + 1])
                    else:
                        k.ts("dve", rden[:], rden[:], neglam[:, 0:1])
                        o1_ = o1s[st["epi"] % 2]
                        for j in range(4):
                            k.stt(o1_[:, j, :], accs[:, j, 0:128], rden[:, j:j + 1], o1[:, j, :], ALU.mult, ALU.add)
                on_ = ons[st["epi"] % 2]
                ssq_ = ssqs[st["epi"] % 2]
                st["epi"] += 1

                def e_act(o1_=o1_, ssq_=ssq_):
                    for j in range(4):
                        k.act(junk[:], o1_[:, j, :], AF.Square, accum_out=ssq_[:, j:j + 1])
                    k.act(ssq_[:], ssq_[:], AF.Sqrt, bias=epsc[:, 0:1], scale=1.0 / 128.0)

                def e_dve(o1_=o1_, ssq_=ssq_, on_=on_):
                    k.op("dve", lambda: nc.vector.reciprocal(out=ssq_[:], in_=ssq_[:]), [ssq_[:]], [ssq_[:]])
                    for j in range(4):
                        k.stt(on_[:, j, :], o1_[:, j, :], ssq_[:, j:j + 1], wbc[:], ALU.mult, ALU.mult)

                def e_pe(h=h, qb=qb, on_=on_):
                    for j in range(4):
                        k.tr(psb[6][:, j * 128:(j + 1) * 128], on_[:, j, :], identb[:])
                    o_ = oT[qb % 2]
                    k.copy("dve", o_[:], psb[6][:, 0:512])
                    k.dma("pool", d["oT_da"][h, :, qb * 512:(qb + 1) * 512], o_[:], "co%d" % (qb % 2))
                while st["pend"]:
                    st["pend"].pop(0)()
                st["pend"] = [e_act, e_dve, e_pe]
        while st["pend"]:
            st["pend"].pop(0)()
        k.barrier()


ALPHA = 2.0 ** 0.25


def layer_norm_tile(k, nc, r, gbc, bbc, out, stats, mv, rstd, epsc5):
    for hh in range(2):
        k.op("dve", lambda hh=hh: nc.vector.bn_stats(out=stats[:, hh, :], in_=r[:, hh * 512:(hh + 1) * 512]), [stats[:]], [r[:]])
    k.op("dve", lambda: nc.vector.bn_aggr(out=mv[:], in_=stats[:].rearrange("p a b -> p (a b)")), [mv[:]], [stats[:]])
    k.act(rstd[:], mv[:, 1:2], AF.Sqrt, bias=epsc5)
    k.op("dve", lambda: nc.vector.reciprocal(out=rstd[:], in_=rstd[:]), [rstd[:]], [rstd[:]])
    k.ts("dve", out[:], r[:], mv[:, 0:1], rstd[:, 0:1], op0=ALU.subtract, op1=ALU.mult)
    k.tt("pool", out[:], out[:], gbc[:], ALU.mult)
    k.tt("pool", out[:], out[:], bbc[:], ALU.add)


def phase_D(k, g):
    nc = k.nc
    d = g.d
    To = g.To
    nqb = To // 512
    ps = g.ps
    psb = [p[:].bitcast(BF16) for p in ps]
    with contextlib.ExitStack() as es:
        sb = lambda name, shape, dt: k.sb(name, shape, dt, es)
        identb = sb("D_identb", [128, 128], BF16); k.dma("pool", identb[:], d["ident"][:, :], "c")
        wdn = sb("D_wdn", [128, 4, 1024], BF16)
        k.dma("pool", wdn[:], d["w_dn_o"].rearrange("(kk p) n -> p kk n", p=128), "w0")
        wda = sb("D_wda", [128, 4, 1024], BF16)
        k.dma("pool", wda[:], d["w_da_o"].rearrange("(kk p) n -> p kk n", p=128), "w1")
        wout = sb("D_wout", [128, 8, 1024], BF16)
        k.dma("pool", wout[:], d["w_out"].rearrange("(kk p) n -> p kk n", p=128), "w0")
        wr = sb("D_wr", [128, 8, 20], BF16)
        k.dma("pool", wr[:, :, 0:4], d["w_router_group"].rearrange("(kk p) n -> p kk n", p=128), "w1")
        k.dma("pool", wr[:, :, 4:20], d["w_router_expert"].rearrange("(kk p) n -> p kk n", p=128), "w1")
        rb = sb("D_rb", [128, 20], F32)
        k.dma("sp", rb[:, 0:4], d["b_router_group"].partition_broadcast(128), "c")
        k.dma("sp", rb[:, 4:20], d["b_router_expert"].partition_broadcast(128), "c")
        gbc = sb("D_gbc", [128, 1024], F32); k.dma("sp", gbc[:], d["ln1_g"].partition_broadcast(128), "c")
        bbc = sb("D_bbc", [128, 1024], F32); k.dma("sp", bbc[:], d["ln1_b"].partition_broadcast(128), "c")
        epsc = sb("D_epsc", [128, 1], F32); k.memset("dve", epsc[:], 1e-5)
        oTdn = [sb("D_oTdn%d" % i, [128, 4, 512], BF16) for i in range(2)]
        oTda = [sb("D_oTda%d" % i, [128, 4, 512], BF16) for i in range(2)]
        hTo = [sb("D_hTo%d" % i, [128, 8, 512], BF16) for i in range(2)]
        Wg = sb("D_Wg", [128, 8, 2048], BF16)
        for kk in range(8):
            k.dma("pool", (Wg[:, kk, :], kk), d["w_in"][kk * 128:(kk + 1) * 128, C_G:C_G + 2048], "w%d" % (kk % 2))
        gcolb = sb("D_gcolb", [128, 16], F32)
        colload(k, "sp", gcolb[:, :], d["b_in"][C_G:C_G + 2048], 16)
        g1 = [sb("D_g1_%d" % i, [128, 512], BF16) for i in range(2)]
        g2 = [sb("D_g2_%d" % i, [128, 512], BF16) for i in range(2)]
        hres = [sb("D_hres%d" % i, [128, 4, 1024], F32) for i in range(2)]
        t1 = [sb("D_t1_%d" % i, [128, 512], F32) for i in range(2)]
        t2 = [sb("D_t2_%d" % i, [128, 512], F32) for i in range(2)]
        mergeds = [sb("D_merged%d" % i, [128, 8, 512], BF16) for i in range(2)]
        r = [sb("D_r%d" % i, [128, 1024], F32) for i in range(2)]
        h1 = [sb("D_h1_%d" % i, [128, 1024], F32) for i in range(2)]
        h1bs = [sb("D_h1b%d" % i, [128, 1024], BF16) for i in range(2)]
        h1T = [sb("D_h1T%d" % i, [128, 8, 128], BF16) for i in range(2)]
        stats = sb("D_stats", [128, 2, 6], F32)
        mv = sb("D_mv", [128, 2], F32)
        rstd = sb("D_rstd", [128, 1], F32)
        lg = sb("D_lg", [128, 20], F32)
        sm = sb("D_sm", [128, 16], F32)
        ge = sb("D_ge", [128, 4], F32)
        ohg = sb("D_ohg", [128, 4], F32)
        tmp16 = sb("D_tmp16", [128, 4, 4], F32)
        ig = sb("D_ig", [128, 4], F32)
        ig2 = sb("D_ig2", [128, 4], F32)
        mk1 = sb("D_mk1", [128, 4], F32)
        mk2 = sb("D_mk2", [128, 4], F32)
        cwi = sb("D_cwi", [128, 4], F32)
        comb = [sb("D_comb%d" % i, [128, 4, 4], F32) for i in range(2)]

        def load(qb):
            s = qb % 2
            q0 = qb * 512
            k.dma("sp", oTdn[s][:], d["oT_dn"][:, :, q0:q0 + 512].rearrange("h d t -> d h t"), "dl%d" % s)
            k.dma("sp", oTda[s][:], d["oT_da"][:, :, q0:q0 + 512].rearrange("h d t -> d h t"), "dl%d" % s)
            k.dma("sp", hTo[s][:], d["hT_own"][:, :, q0:q0 + 512].rearrange("kk p t -> p kk t"), "dl%d" % s)
            k.dma("sp", hres[s][:], d["hres"][q0:q0 + 512, :].rearrange("(t p) f -> p t f", p=128), "dl%d" % s)

        load(0)
        ti = 0
        for qb in range(nqb):
            s = qb % 2
            q0 = qb * 512
            if qb + 1 < nqb:
                load(qb + 1)
            def cpart(qb_, cs):
                s_ = qb_ % 2
                mg = mergeds[qb_ % 2]
                for c in cs:
                    pg1, pg2, pa, pb_ = ps[0], ps[1], ps[2], ps[3]
                    for kk in range(8):
                        k.mm(pg1[:, :], (Wg[:, kk, c * 128:(c + 1) * 128], kk), hTo[s_][:, kk, :], start=(kk == 0), stop=(kk == 7))
                    k.act(g1[c % 2][:], pg1[:, :], AF.Sigmoid, bias=gcolb[:, c:c + 1])
                    for kk in range(8):
                        k.mm(pg2[:, :], (Wg[:, kk, 1024 + c * 128:1024 + (c + 1) * 128], kk), hTo[s_][:, kk, :], start=(kk == 0), stop=(kk == 7))
                    k.act(g2[c % 2][:], pg2[:, :], AF.Sigmoid, bias=gcolb[:, 8 + c:9 + c])
                    for kk in range(4):
                        k.mm(pa[:, :], wdn[:, kk, c * 128:(c + 1) * 128], oTdn[s_][:, kk, :], start=(kk == 0), stop=(kk == 3))
                    for kk in range(4):
                        k.mm(pb_[:, :], wda[:, kk, c * 128:(c + 1) * 128], oTda[s_][:, kk, :], start=(kk == 0), stop=(kk == 3))
                    a_, b_ = t1[c % 2], t2[c % 2]
                    k.tt("dve", a_[:], pa[:, :], g1[c % 2][:], ALU.mult)
                    k.tt("dve", b_[:], pb_[:, :], g2[c % 2][:], ALU.mult)
                    k.tt("pool", mg[:, c, :], a_[:], b_[:], ALU.add)

            merged = mergeds[qb % 2]
            if qb == 0:
                cpart(0, range(8))
            def bufs(i):
                return r[i % 2], h1[i % 2], h1T[i % 2], comb[i % 2], h1bs[i % 2]

            def S1(t, i):
                r_, h_, hT_, cb_, h1b = bufs(i)
                for n in range(2):
                    pm = ps[4 + n]
                    for kk in range(8):
                        k.mm(pm[:, :], merged[:, kk, t * 128:(t + 1) * 128], wout[:, kk, n * 512:(n + 1) * 512], start=(kk == 0), stop=(kk == 7))
                    k.stt(r_[:, n * 512:(n + 1) * 512], hres[s][:, t, n * 512:(n + 1) * 512], ALPHA, pm[:, :], ALU.mult, ALU.add)
                layer_norm_tile(k, nc, r_, gbc, bbc, h_, stats, mv, rstd, epsc[:, 0:1])
                k.dma("pool", d["h1"][q0 + t * 128:q0 + (t + 1) * 128, :], h_[:], "do0")
                k.copy("act", h1b[:], h_[:])

            def S2(t, i):
                r_, h_, hT_, cb_, h1b = bufs(i)
                for kk in range(8):
                    k.tr(psb[6][:, kk * 128:(kk + 1) * 128], h1b[:, kk * 128:(kk + 1) * 128], identb[:])
                k.copy("act", hT_[:].rearrange("p a b -> p (a b)"), psb[6][:, 0:1024])
                k.dma("pool", d["h1T"][:, :, q0 + t * 128:q0 + (t + 1) * 128].rearrange("kk p t -> p kk t"), hT_[:], "do1")
                for kk in range(8):
                    k.mm(ps[7][:, 0:20], hT_[:, kk, :], wr[:, kk, :], start=(kk == 0), stop=(kk == 7))
                k.tt("dve", lg[:], ps[7][:, 0:20], rb[:], ALU.add)
                k.op("dve", lambda: nc.vector.tensor_reduce(out=sm[:, 0:1], in_=lg[:, 0:4], axis=AX.X, op=ALU.max), [sm[:]], [lg[:]])
                k.ts("dve", sm[:, 1:2], sm[:, 0:1], -1.0)
                k.act(ge[:], lg[:, 0:4], AF.Exp, bias=sm[:, 1:2])
                k.op("dve", lambda: nc.vector.tensor_reduce(out=sm[:, 2:3], in_=ge[:], axis=AX.X, op=ALU.add), [sm[:]], [ge[:]])
                k.op("dve", lambda: nc.vector.reciprocal(out=sm[:, 3:4], in_=sm[:, 2:3]), [sm[:]], [sm[:]])
                k.ts("dve", ohg[:], lg[:, 0:4], sm[:, 0:1], op0=ALU.is_equal)
                k.tt("dve", tmp16[:], lg[:, 4:20].rearrange("p (g e) -> p g e", g=4), ohg[:, :].unsqueeze(2).broadcast_to([128, 4, 4]), ALU.mult)
                k.op("dve", lambda: nc.vector.tensor_reduce(out=ig[:], in_=tmp16[:].rearrange("p g e -> p e g"), axis=AX.X, op=ALU.add), [ig[:]], [tmp16[:]])
                k.op("dve", lambda: nc.vector.tensor_reduce(out=sm[:, 4:5], in_=ig[:], axis=AX.X, op=ALU.max), [sm[:]], [ig[:]])
                k.ts("dve", mk1[:], ig[:], sm[:, 4:5], op0=ALU.is_equal)
                k.stt(ig2[:], mk1[:], -1e9, ig[:], ALU.mult, ALU.add)
                k.op("dve", lambda: nc.vector.tensor_reduce(out=sm[:, 5:6], in_=ig2[:], axis=AX.X, op=ALU.max), [sm[:]], [ig2[:]])
                k.ts("dve", mk2[:], ig2[:], sm[:, 5:6], op0=ALU.is_equal)
                k.tt("dve", sm[:, 6:7], sm[:, 5:6], sm[:, 4:5], ALU.subtract)
                k.act(sm[:, 7:8], sm[:, 6:7], AF.Exp)
                k.ts("dve", sm[:, 8:9], sm[:, 7:8], 1.0, op0=ALU.add)
                k.op("dve", lambda: nc.vector.reciprocal(out=sm[:, 9:10], in_=sm[:, 8:9]), [sm[:]], [sm[:]])
                k.tt("dve", sm[:, 10:11], sm[:, 9:10], sm[:, 3:4], ALU.mult)
                k.tt("dve", sm[:, 11:12], sm[:, 10:11], sm[:, 7:8], ALU.mult)
                k.ts("dve", cwi[:], mk1[:], sm[:, 10:11])
                k.stt(cwi[:], mk2[:], sm[:, 11:12], cwi[:], ALU.mult, ALU.add)
                k.tt("dve", cb_[:], ohg[:, :].unsqueeze(2).broadcast_to([128, 4, 4]), cwi[:, :].unsqueeze(1).broadcast_to([128, 4, 4]), ALU.mult)
                k.dma("pool", d["comb"][q0 + t * 128:q0 + (t + 1) * 128, :], cb_[:].rearrange("p g e -> p (g e)"), "do2")

            S1(0, ti)
            for t in range(4):
                if t + 1 < 4:
                    S1(t + 1, ti + 1)
                if qb + 1 < nqb:
                    cpart(qb + 1, [2 * t, 2 * t + 1])
                S2(t, ti)
                ti += 1
        k.barrier()


def phase_E(k, g):
    nc = k.nc
    d = g.d
    To = g.To
    nt = To // 128
    ps = g.ps
    psb = [p[:].bitcast(BF16) for p in ps]
    with contextlib.ExitStack() as es:
        sb = lambda name, shape, dt: k.sb(name, shape, dt, es)
        identb = sb("E_identb", [128, 128], BF16); k.dma("pool", identb[:], d["ident"][:, :], "c")
        wgu = sb("E_wgu", [128, 8, 8, 512], BF16)
        wdn = sb("E_wdn", [128, 8, 2, 1024], BF16)
        wpg = sb("E_wpg", [128, 8, 1024], BF16)
        k.dma("pool", wpg[:], d["w_ple_gate"].rearrange("(kk p) n -> p kk n", p=128), "w0")
        wpp = sb("E_wpp", [128, 2, 1024], BF16)
        k.dma("pool", wpp[:], d["w_ple_proj"].rearrange("(kk p) n -> p kk n", p=128), "w1")
        pgb = sb("E_pgb", [128, 1024], F32); k.dma("sp", pgb[:], d["b_ple_gate"].partition_broadcast(128), "c")
        gbc = sb("E_gbc", [128, 1024], F32); k.dma("sp", gbc[:], d["ln2_g"].partition_broadcast(128), "c")
        bbc = sb("E_bbc", [128, 1024], F32); k.dma("sp", bbc[:], d["ln2_b"].partition_broadcast(128), "c")
        epsc = sb("E_epsc", [128, 1], F32); k.memset("dve", epsc[:], 1e-5)
        h1T = [sb("E_h1T%d" % i, [128, 8, 128], BF16) for i in range(2)]
        comb = [sb("E_comb%d" % i, [128, 16], F32) for i in range(2)]
        h1 = [sb("E_h1_%d" % i, [128, 1024], F32) for i in range(2)]
        f0 = [sb("E_f0_%d" % i, [128, 1024], F32) for i in range(2)]
        pt = [sb("E_pt%d" % i, [128, 256], F32) for i in range(2)]
        ptb = sb("E_ptb", [128, 256], BF16)
        pT = sb("E_pT", [128, 2, 128], BF16)
        sg = [sb("E_sg%d" % i, [128, 256], F32) for i in range(2)]
        hid = [sb("E_hid%d" % i, [128, 256], BF16) for i in range(2)]
        hidT = [sb("E_hidT%d" % i, [128, 2, 128], BF16) for i in range(2)]
        fo = [sb("E_fo%d" % i, [128, 1024], F32) for i in range(2)]
        gate = sb("E_gate", [128, 512], F32)
        r = sb("E_r", [128, 1024], F32)
        outt = [sb("E_out%d" % i, [128, 1024], F32) for i in range(2)]
        stats = sb("E_stats", [128, 2, 6], F32)
        mv = sb("E_mv", [128, 2], F32)
        rstd = sb("E_rstd", [128, 1], F32)
        st = {"ei": 0}
        for pas in range(2):
            for e in range(8):
                ge_ = pas * 8 + e
                k.dma("pool", wgu[:, e, :, 0:256], d["w_exp_gate"][ge_].rearrange("(kk p) f -> p kk f", p=128), "w%d" % (e % 2))
                k.dma("pool", wgu[:, e, :, 256:512], d["w_exp_up"][ge_].rearrange("(kk p) f -> p kk f", p=128), "w%d" % (e % 2))
                k.dma("pool", wdn[:, e, :, :], d["w_exp_down"][ge_].rearrange("(fk p) n -> p fk n", p=128), "w%d" % (e % 2))

            def load(t):
                s = t % 2
                k.dma("sp", h1T[s][:], d["h1T"][:, :, t * 128:(t + 1) * 128].rearrange("kk p t -> p kk t"), "el%d" % s)
                k.dma("sp", comb[s][:], d["comb"][t * 128:(t + 1) * 128, :], "el%d" % s)
                if pas == 1:
                    k.dma("sp", h1[s][:], d["h1"][t * 128:(t + 1) * 128, :], "el%d" % s)
                    k.dma("sp", f0[s][:], d["ffn0"][t * 128:(t + 1) * 128, :], "el%d" % s)
                    k.dma("sp", pt[s][:], d["pin"][t * 128:(t + 1) * 128, :], "el%d" % s)

            load(0)
            for t in range(nt):
                s = t % 2
                if t + 1 < nt:
                    load(t + 1)
                def emit_gu(e):
                    pg = ps[2 + st["ei"] % 2]
                    sg_, hid_ = sg[st["ei"] % 2], hid[st["ei"] % 2]
                    st["ei"] += 1
                    ge_ = pas * 8 + e
                    for kk in range(8):
                        k.mm(pg[:, :], h1T[s][:, kk, :], wgu[:, e, kk, :], start=(kk == 0), stop=(kk == 7))
                    k.act(sg_[:], pg[:, 0:256], AF.Silu)
                    k.stt(hid_[:], sg_[:], comb[s][:, ge_:ge_ + 1], pg[:, 256:512], ALU.mult, ALU.mult)
                    return hid_
                def emit_down(e, hidT_):
                    for n in range(2):
                        for fk in range(2):
                            k.mm(ps[n][:, :], hidT_[:, fk, :], wdn[:, e, fk, n * 512:(n + 1) * 512],
                                 start=(e == 0 and fk == 0), stop=(e == 7 and fk == 1))
                nxt = emit_gu(0)
                pend = None
                for e in range(8):
                    hid_ = nxt
                    if e + 1 < 8:
                        nxt = emit_gu(e + 1)
                    hidT_ = hidT[e % 2]
                    pt_ = psb[4 + e % 2]
                    for fk in range(2):
                        k.tr(pt_[:, fk * 128:(fk + 1) * 128], hid_[:, fk * 128:(fk + 1) * 128], identb[:])
                    k.copy("act", hidT_[:].rearrange("p a b -> p (a b)"), pt_[:, 0:256])
                    if pend is not None:
                        emit_down(*pend)
                    pend = (e, hidT_)
                emit_down(*pend)
                if pas == 0:
                    fo_ = fo[t % 2]
                    k.copy("act", fo_[:, 0:512], ps[0][:, :])
                    k.copy("dve", fo_[:, 512:1024], ps[1][:, :])
                    k.dma("pool", d["ffn0"][t * 128:(t + 1) * 128, :], fo_[:], "eo%d" % (t % 2))
                else:
                    k.copy("act", ptb[:], pt[s][:])
                    for kk in range(2):
                        k.tr(psb[6][:, kk * 128:(kk + 1) * 128], ptb[:, kk * 128:(kk + 1) * 128], identb[:])
                    k.copy("act", pT[:].rearrange("p a b -> p (a b)"), psb[6][:, 0:256])
                    for n in range(2):
                        sl = slice(n * 512, (n + 1) * 512)
                        for kk in range(8):
                            k.mm(ps[6][:, :], h1T[s][:, kk, :], wpg[:, kk, sl], start=(kk == 0), stop=(kk == 7))
                        k.tt("dve", gate[:], ps[6][:, :], pgb[:, sl], ALU.add)
                        k.act(gate[:], gate[:], AF.Sigmoid)
                        for kk in range(2):
                            k.mm(ps[7][:, :], pT[:, kk, :], wpp[:, kk, sl], start=(kk == 0), stop=(kk == 1))
                        k.tt("dve", gate[:], gate[:], ps[7][:, :], ALU.mult)
                        k.tt("dve", r[:, sl], f0[s][:, sl], ps[n][:, :], ALU.add)
                        k.tt("pool", r[:, sl], r[:, sl], gate[:], ALU.add)
                        k.stt(r[:, sl], h1[s][:, sl], ALPHA, r[:, sl], ALU.mult, ALU.add)
                    o_ = outt[t % 2]
                    layer_norm_tile(k, nc, r, gbc, bbc, o_, stats, mv, rstd, epsc[:, 0:1])
                    k.dma("pool", d["out"][t * 128:(t + 1) * 128, :], o_[:], "eo%d" % (t % 2))
            k.barrier()
        k.finish([d["out"]])


_CACHE = {}


def kernel(**inputs):
    from concourse.bass_utils import run_bass_kernel_spmd
    x = np.asarray(inputs["x"], dtype=np.float32)
    p = np.asarray(inputs["p"], dtype=np.float32)
    B, S, _ = x.shape
    n_cores = 8
    per = n_cores // B
    To = S // per
    Tc = S - To
    T = Tc + To
    if (Tc, To) not in _CACHE:
        _CACHE[(Tc, To)] = build(Tc, To, debug=False, phases="ABCDE")
    nc, g = _CACHE[(Tc, To)]
    consts = make_consts(Tc, To)
    w = {}
    for n in INPUT_NAMES:
        a = np.asarray(inputs[n], dtype=np.float32)
        if n not in ("emb_ln_g", "emb_ln_b"):
            a = a[0]
        w[n] = np.ascontiguousarray(a)
    in_maps = []
    for c in range(n_cores):
        b, half = c // per, c % per
        own = x[b, half * To:(half + 1) * To]
        if half == 0:
            ctx = x[b, 0:Tc]
            tmask = np.concatenate([np.zeros(Tc, np.float32), np.ones(To, np.float32)])
        else:
            ctx = x[b, 0:Tc]
            tmask = np.ones(T, np.float32)
        m = {"xin": np.ascontiguousarray(np.concatenate([ctx, own], axis=0)),
             "pin": np.ascontiguousarray(p[0, b, half * To:(half + 1) * To]),
             "tmask": tmask,
             "tm_tok": np.ascontiguousarray(tmask.reshape(T // 128, 128).T)}
        m.update(w)
        m.update(consts)
        in_maps.append(m)
    res = run_bass_kernel_spmd(nc, in_maps, core_ids=list(range(n_cores)))
    out = np.empty((B, S, D), np.float32)
    for c in range(n_cores):
        b, half = c // per, c % per
        out[b, half * To:(half + 1) * To] = np.asarray(res.results[c]["out"], dtype=np.float32)
    return out
```
